# Optimizing a Trainium2 kernel written in Bass

```python
import math
import jax
import jax.numpy as jnp
from jax import lax
import numpy as np

D_MODEL = 1024
BATCH = 1
SEQ = 16384
DEPTH = 4

GRID_W = 64
CTX_LEN = 256
N_MIXERS = 3
NORM_EPS = 1e-6
N_MOD = 6

DA_HEADS = 8
DA_HEAD_DIM = D_MODEL // (2 * DA_HEADS)
DA_SCALE = DA_HEAD_DIM ** -0.5
Q_BLOCK = 128
ROPE_BASE = 10000.0
ROPE_AXIS_DIM = DA_HEAD_DIM // 2

CHUNK = 128
GM_DIM = D_MODEL
GM_GROUPS = 8
GM_GROUP_CH = GM_DIM // GM_GROUPS

RW_HEAD = 64
RW_HEADS = D_MODEL // RW_HEAD
RW_DECAY_LORA = 64
RW_A_LORA = 64
RW_GATE_LORA = 160
RW_GN_EPS = 64e-5

FFN_DIM = 2816
N_EXPERTS = 8
TOP_K = 2

kernel_name = 'hybrid_diffattn_gmlp_rwkv7_moe_dit'


def rms_norm(x, g, eps=NORM_EPS):
    xf = x.astype(jnp.float32)
    y = xf * lax.rsqrt(jnp.mean(xf * xf, axis=-1, keepdims=True) + eps)
    return (y * g.astype(jnp.float32)).astype(x.dtype)


def layer_norm(x, g, b, eps=1e-5):
    xf = x.astype(jnp.float32)
    xc = xf - jnp.mean(xf, axis=-1, keepdims=True)
    var = jnp.mean(xc * xc, axis=-1, keepdims=True)
    return (xc * lax.rsqrt(var + eps) * g.astype(jnp.float32) + b.astype(jnp.float32)).astype(x.dtype)


def swiglu(h, w1, w3, w2):
    return (jax.nn.silu(h @ w1) * (h @ w3)) @ w2


def moe_swiglu(h, router, w1, w3, w2):
    logits = (h @ router).astype(jnp.float32)
    top_v, top_i = lax.top_k(logits, TOP_K)
    gate = jnp.einsum('bnk,bnke->bne', jax.nn.softmax(top_v, axis=-1),
                      jax.nn.one_hot(top_i, N_EXPERTS, dtype=jnp.float32)).astype(h.dtype)
    y = jnp.zeros_like(h)
    for e in range(N_EXPERTS):
        y = y + gate[..., e:e + 1] * swiglu(h, w1[e], w3[e], w2[e])
    return y


def axial_rope_tables(n_tokens):
    rows = n_tokens // GRID_W
    row = jnp.repeat(jnp.arange(rows, dtype=jnp.float32), GRID_W)
    col = jnp.tile(jnp.arange(GRID_W, dtype=jnp.float32), rows)
    inv_freq = ROPE_BASE ** (-jnp.arange(0, ROPE_AXIS_DIM, 2, dtype=jnp.float32) / ROPE_AXIS_DIM)
    ang_r = row[:, None] * inv_freq
    ang_c = col[:, None] * inv_freq
    ang = jnp.concatenate([ang_r, ang_r, ang_c, ang_c], axis=-1)
    return jnp.cos(ang), jnp.sin(ang)


def apply_axial_rope(x, cos, sin):
    x1, x2, x3, x4 = jnp.split(x, 4, axis=-1)
    rot = jnp.concatenate([-x2, x1, -x4, x3], axis=-1)
    return (x * cos[:, None, None, :] + rot * sin[:, None, None, :]).astype(x.dtype)


def diff_softmax_attend(q, k, v, lam):
    s = jnp.einsum('bqhmd,bkhmd->bhmqk', q, k).astype(jnp.float32) * DA_SCALE
    p = jax.nn.softmax(s, axis=-1)
    a = p[:, :, 0] - lam * p[:, :, 1]
    return jnp.einsum('bhqk,bkhe->bqhe', a.astype(v.dtype), v)


def diff_attention(h_lat, h_ctx, w_in, lam_p, subln, w_out, lam_init, need_ctx):
    B, T, _ = h_lat.shape

    def project(h):
        n = h.shape[1]
        q, k, v = jnp.split(h @ w_in, 3, axis=-1)
        return (q.reshape(B, n, DA_HEADS, 2, DA_HEAD_DIM),
                k.reshape(B, n, DA_HEADS, 2, DA_HEAD_DIM),
                v.reshape(B, n, DA_HEADS, 2 * DA_HEAD_DIM))

    def finish(o):
        o = rms_norm(o, subln, 1e-5) * (1.0 - lam_init)
        return o.reshape(B, o.shape[1], D_MODEL) @ w_out

    cos, sin = axial_rope_tables(T)
    q_l, k_l, v_l = project(h_lat)
    q_l = apply_axial_rope(q_l, cos, sin)
    k_l = apply_axial_rope(k_l, cos, sin)
    q_c, k_c, v_c = project(h_ctx)
    lp = lam_p.astype(jnp.float32)
    lam = jnp.exp(jnp.sum(lp[0] * lp[1])) - jnp.exp(jnp.sum(lp[2] * lp[3])) + lam_init
    k_all = jnp.concatenate([k_c, k_l], axis=1)
    v_all = jnp.concatenate([v_c, v_l], axis=1)
    n_blk = T // Q_BLOCK
    q_blk = jnp.moveaxis(q_l.reshape(B, n_blk, Q_BLOCK, DA_HEADS, 2, DA_HEAD_DIM), 1, 0)
    o_blk = lax.map(lambda qb: diff_softmax_attend(qb, k_all, v_all, lam), q_blk)
    o_lat = jnp.moveaxis(o_blk, 0, 1).reshape(B, T, DA_HEADS, 2 * DA_HEAD_DIM)
    y_lat = finish(o_lat)
    y_ctx = finish(diff_softmax_attend(q_c, k_c, v_c, lam)) if need_ctx else None
    return y_lat, y_ctx


def chunk_gmlp(h_lat, h_ctx, w_in, ln_g, ln_b, w_s, b_s, w_out, need_ctx):
    def mix(h):
        B, n, _ = h.shape
        u, v = jnp.split(jax.nn.gelu(h @ w_in, approximate=False), 2, axis=-1)
        v = layer_norm(v, ln_g, ln_b).reshape(B, n // CHUNK, CHUNK, GM_GROUPS, GM_GROUP_CH)
        s = jnp.einsum('gpq,bcqgk->bcpgk', w_s, v) + b_s.T[:, :, None]
        return (u * s.reshape(B, n, GM_DIM)) @ w_out

    return mix(h_lat), (mix(h_ctx) if need_ctx else None)


def token_shift(x):
    prev = jnp.pad(x, ((0, 0), (1, 0), (0, 0)))[:, :-1]
    nxt = jnp.pad(x, ((0, 0), (0, 1), (0, 0)))[:, 1:]
    return 0.5 * (prev + nxt) - x


def to_heads(t):
    return t.reshape(t.shape[:-1] + (RW_HEADS, RW_HEAD)).astype(jnp.float32)


def rwkv_prepare(h, mix, w_rkv, w0, w1, w2, a0, a1, a2, g1, g2, k_k, k_a):
    xm = h[None] + token_shift(h)[None] * mix[:, None, None, :]
    xr, xw, xk, xv, xa, xg = xm
    r, k, v = jnp.einsum('pbnd,pde->pbne', jnp.stack([xr, xk, xv]), w_rkv)
    w_log = -jax.nn.softplus(-(w0[:, None, None, :] + jnp.einsum(
        'zbnl,zld->zbnd', jnp.tanh(jnp.einsum('bnd,zdl->zbnl', xw, w1)), w2))) - 0.5
    a = jax.nn.sigmoid(a0[:, None, None, :] + jnp.einsum(
        'zbnl,zld->zbnd', jnp.einsum('bnd,zdl->zbnl', xa, a1), a2))
    g = jax.nn.sigmoid(xg @ g1) @ g2
    kk = to_heads(k * k_k)
    kk = kk / jnp.maximum(jnp.sqrt(jnp.sum(kk * kk, axis=-1, keepdims=True)), 1e-12)
    k_dir = to_heads(k[None] * (1.0 + (a - 1.0) * k_a))
    r_h, v_h = to_heads(r), to_heads(v)
    decay = jnp.exp(-jnp.exp(to_heads(w_log)))
    pair = lambda t: jnp.broadcast_to(t[None], (2,) + t.shape)
    scan_in = (pair(r_h), decay, k_dir, pair(v_h), pair(-kk), kk[None] * to_heads(a))
    return scan_in, r_h, k_dir, v_h, g


def wkv7_bidir(scan_in, state0):
    def to_time_major(t):
        return jnp.moveaxis(jnp.stack([t[0], jnp.flip(t[1], axis=1)]), 2, 0)

    def step(S, inp):
        r, w, k, v, a, b = inp
        sa = jnp.einsum('zbhij,zbhj->zbhi', S, a)
        S = S * w[..., None, :] + sa[..., :, None] * b[..., None, :] + v[..., :, None] * k[..., None, :]
        return S, jnp.einsum('zbhij,zbhj->zbhi', S, r)

    s_fin, y = lax.scan(step, state0, tuple(to_time_major(t) for t in scan_in))
    y = jnp.moveaxis(y, 0, 2)
    return s_fin, y[0] + jnp.flip(y[1], axis=1)


def rwkv_output(y, r_h, k_dir, v_h, g, r_k, ln_w, ln_b, w_out, dtype):
    B, n = y.shape[:2]
    yc = y - jnp.mean(y, axis=-1, keepdims=True)
    yn = yc * lax.rsqrt(jnp.mean(yc * yc, axis=-1, keepdims=True) + RW_GN_EPS)
    yn = yn.reshape(B, n, D_MODEL) * ln_w.astype(jnp.float32) + ln_b.astype(jnp.float32)
    bonus = jnp.sum(jnp.sum(r_h[None] * k_dir * r_k.astype(jnp.float32), axis=-1, keepdims=True), axis=0) * v_h
    o = (yn + bonus.reshape(B, n, D_MODEL)).astype(dtype)
    return (o * g) @ w_out


def rwkv7_bidir(h_lat, h_ctx, mix, w_rkv, w0, w1, w2, a0, a1, a2, g1, g2, k_k, k_a,
                r_k, ln_w, ln_b, w_out, need_ctx):
    pp = (mix, w_rkv, w0, w1, w2, a0, a1, a2, g1, g2, k_k, k_a)
    scan_c, r_c, kd_c, v_c, g_c = rwkv_prepare(h_ctx, *pp)
    scan_l, r_l, kd_l, v_l, g_l = rwkv_prepare(h_lat, *pp)
    s0 = jnp.zeros((2, h_lat.shape[0], RW_HEADS, RW_HEAD, RW_HEAD), jnp.float32)
    s_ctx, y_c = wkv7_bidir(scan_c, s0)
    _, y_l = wkv7_bidir(scan_l, s_ctx)
    y_lat = rwkv_output(y_l, r_l, kd_l, v_l, g_l, r_k, ln_w, ln_b, w_out, h_lat.dtype)
    y_ctx = rwkv_output(y_c, r_c, kd_c, v_c, g_c, r_k, ln_w, ln_b, w_out, h_ctx.dtype) if need_ctx else None
    return y_lat, y_ctx


def setup_inputs(seed: int = 0) -> dict:
    key = jax.random.key(seed)
    ks = iter(jax.random.split(key, 64))
    f32 = jnp.float32
    D = D_MODEL
    nrm = lambda shape, scale: jax.random.normal(next(ks), shape, f32) * scale
    gain = lambda shape: 1.0 + jax.random.normal(next(ks), shape, f32) * 0.05
    unif = lambda shape, lo, hi: jax.random.uniform(next(ks), shape, f32, lo, hi)
    na, ng, nr = (DEPTH + 2) // 3, (DEPTH + 1) // 3, DEPTH // 3
    nd, nm = (DEPTH + 1) // 2, DEPTH // 2
    return {
        'x': nrm((BATCH, SEQ, D), 1.0),
        'c': nrm((BATCH, D), 1.0),
        'ctx': nrm((BATCH, CTX_LEN, D), 1.0),
        'c_ctx': nrm((D,), 1.0),
        'norm_g': gain((DEPTH, 2, D)),
        'mod_w': nrm((DEPTH, D, N_MOD * D), 0.5 * D ** -0.5),
        'mod_b': nrm((DEPTH, N_MOD * D), 0.02),
        'final_g': gain((D,)),
        'attn_w_in': nrm((na, D, 3 * D), D ** -0.5),
        'attn_lambda': nrm((na, 4, DA_HEAD_DIM), 0.1),
        'attn_subln': gain((na, 2 * DA_HEAD_DIM)),
        'attn_w_out': nrm((na, D, D), D ** -0.5),
        'gmlp_w_in': nrm((ng, D, 2 * GM_DIM), D ** -0.5),
        'gmlp_ln_g': gain((ng, GM_DIM)),
        'gmlp_ln_b': nrm((ng, GM_DIM), 0.02),
        'gmlp_w_s': nrm((ng, GM_GROUPS, CHUNK, CHUNK), CHUNK ** -0.5),
        'gmlp_b_s': gain((ng, GM_GROUPS, CHUNK)),
        'gmlp_w_out': nrm((ng, GM_DIM, D), GM_DIM ** -0.5),
        'rwkv_mix': unif((nr, 6, D), 0.0, 1.0),
        'rwkv_w_rkv': nrm((nr, 3, D, D), D ** -0.5),
        'rwkv_w0': unif((nr, 2, D), -4.0, -0.5),
        'rwkv_w1': nrm((nr, 2, D, RW_DECAY_LORA), D ** -0.5),
        'rwkv_w2': nrm((nr, 2, RW_DECAY_LORA, D), 0.1 * RW_DECAY_LORA ** -0.5),
        'rwkv_a0': nrm((nr, 2, D), 0.1),
        'rwkv_a1': nrm((nr, 2, D, RW_A_LORA), D ** -0.5),
        'rwkv_a2': nrm((nr, 2, RW_A_LORA, D), RW_A_LORA ** -0.5),
        'rwkv_g1': nrm((nr, D, RW_GATE_LORA), D ** -0.5),
        'rwkv_g2': nrm((nr, RW_GATE_LORA, D), RW_GATE_LORA ** -0.5),
        'rwkv_k_k': 0.85 + nrm((nr, D), 0.05),
        'rwkv_k_a': gain((nr, D)),
        'rwkv_r_k': nrm((nr, RW_HEADS, RW_HEAD), 0.1),
        'rwkv_ln_w': gain((nr, D)),
        'rwkv_ln_b': nrm((nr, D), 0.02),
        'rwkv_w_out': nrm((nr, D, D), D ** -0.5),
        'ffn_w1': nrm((nd, D, FFN_DIM), D ** -0.5),
        'ffn_w3': nrm((nd, D, FFN_DIM), D ** -0.5),
        'ffn_w2': nrm((nd, FFN_DIM, D), FFN_DIM ** -0.5),
        'moe_router': nrm((nm, D, N_EXPERTS), D ** -0.5),
        'moe_w1': nrm((nm, N_EXPERTS, D, FFN_DIM), D ** -0.5),
        'moe_w3': nrm((nm, N_EXPERTS, D, FFN_DIM), D ** -0.5),
        'moe_w2': nrm((nm, N_EXPERTS, FFN_DIM, D), FFN_DIM ** -0.5),
    }


def reference(x, c, ctx, c_ctx, norm_g, mod_w, mod_b, final_g,
              attn_w_in, attn_lambda, attn_subln, attn_w_out,
              gmlp_w_in, gmlp_ln_g, gmlp_ln_b, gmlp_w_s, gmlp_b_s, gmlp_w_out,
              rwkv_mix, rwkv_w_rkv, rwkv_w0, rwkv_w1, rwkv_w2, rwkv_a0, rwkv_a1, rwkv_a2,
              rwkv_g1, rwkv_g2, rwkv_k_k, rwkv_k_a, rwkv_r_k, rwkv_ln_w, rwkv_ln_b, rwkv_w_out,
              ffn_w1, ffn_w3, ffn_w2, moe_router, moe_w1, moe_w3, moe_w2):
    n_ctx = ctx.shape[1]
    xl, xc = x, ctx
    s_lat = jax.nn.silu(c)[:, None, :]
    s_ctx = jax.nn.silu(c_ctx)
    for i in range(DEPTH):
        last = i == DEPTH - 1
        mod_l = jnp.split(s_lat @ mod_w[i] + mod_b[i], N_MOD, axis=-1)
        mod_c = jnp.split(s_ctx @ mod_w[i] + mod_b[i], N_MOD, axis=-1)
        h_l = rms_norm(xl, norm_g[i, 0]) * (1.0 + mod_l[1]) + mod_l[0]
        h_c = rms_norm(xc, norm_g[i, 0]) * (1.0 + mod_c[1]) + mod_c[0]
        kind, j = i % N_MIXERS, i // N_MIXERS
        if kind == 0:
            lam_init = 0.8 - 0.6 * math.exp(-0.3 * i)
            y_l, y_c = diff_attention(h_l, h_c, attn_w_in[j], attn_lambda[j], attn_subln[j],
                                      attn_w_out[j], lam_init, not last)
        elif kind == 1:
            y_l, y_c = chunk_gmlp(h_l, h_c, gmlp_w_in[j], gmlp_ln_g[j], gmlp_ln_b[j], gmlp_w_s[j],
                                  gmlp_b_s[j], gmlp_w_out[j], not last)
        else:
            y_l, y_c = rwkv7_bidir(h_l, h_c, rwkv_mix[j], rwkv_w_rkv[j], rwkv_w0[j], rwkv_w1[j],
                                   rwkv_w2[j], rwkv_a0[j], rwkv_a1[j], rwkv_a2[j], rwkv_g1[j],
                                   rwkv_g2[j], rwkv_k_k[j], rwkv_k_a[j], rwkv_r_k[j], rwkv_ln_w[j],
                                   rwkv_ln_b[j], rwkv_w_out[j], not last)
        xl = xl + mod_l[2] * y_l
        h2_l = rms_norm(xl, norm_g[i, 1]) * (1.0 + mod_l[4]) + mod_l[3]
        if last:
            h2 = h2_l
        else:
            xc = xc + mod_c[2] * y_c
            h2_c = rms_norm(xc, norm_g[i, 1]) * (1.0 + mod_c[4]) + mod_c[3]
            h2 = jnp.concatenate([h2_c, h2_l], axis=1)
        if i % 2 == 0:
            f = swiglu(h2, ffn_w1[i // 2], ffn_w3[i // 2], ffn_w2[i // 2])
        else:
            f = moe_swiglu(h2, moe_router[i // 2], moe_w1[i // 2], moe_w3[i // 2], moe_w2[i // 2])
        if last:
            xl = xl + mod_l[5] * f
        else:
            xc = xc + mod_c[5] * f[:, :n_ctx]
            xl = xl + mod_l[5] * f[:, n_ctx:]
    return rms_norm(xl, final_g)
```

```python
import math
import numpy as np
import ml_dtypes
import concourse.bass as bass
import concourse.mybir as mybir
from concourse.bass_utils import run_bass_kernel_spmd

F32 = mybir.dt.float32
BF16 = mybir.dt.bfloat16
AF = mybir.ActivationFunctionType
ALU = mybir.AluOpType
AX = mybir.AxisListType
NPBF = ml_dtypes.bfloat16

NCORES = 8
D = 1024
DC = 8
SEQ = 16384
NCTX = 256
FFN = 2816
FC = 22
NE = 8
EPS = 1e-6


class Buf:
    __slots__ = ("name", "w", "r", "sem_in", "sem_out", "n_in", "n_out")

    def __init__(self, name):
        self.name = name
        self.w = None
        self.r = {}
        self.sem_in = None
        self.sem_out = None
        self.n_in = 0
        self.n_out = 0


class Sched:
    def __init__(self, nc):
        self.nc = nc
        self.engs = {}
        for name, e in (("pe", nc.tensor), ("act", nc.scalar), ("dve", nc.vector),
                        ("pool", nc.gpsimd), ("sp", nc.sync)):
            self.engs[name] = dict(e=e, sem=nc.alloc_semaphore(name="sem_" + name), cnt=0, seen={})
        self.nsem = 5
        self.ninst = 0
        self.dma_toks = {}

    def buf(self, name):
        return Buf(name)

    def bufs(self, name, n):
        return [Buf(f"{name}{i}") for i in range(n)]

    def _wait(self, eng, toks):
        E = self.engs[eng]
        best = {}
        for t in toks:
            if t is None:
                continue
            s, v = t
            k = id(s)
            if E["seen"].get(k, 0) >= v:
                continue
            if k not in best or best[k][1] < v:
                best[k] = (s, v)
        for k, (s, v) in best.items():
            if s is E["sem"] and eng == "pe":
                E["seen"][k] = v
                continue
            E["e"].wait_ge(s, v)
            E["seen"][k] = v
            self.ninst += 1

    def _deps(self, reads, writes):
        toks = []
        for b in reads:
            toks.append(b.w)
        for b in writes:
            toks.append(b.w)
            toks.extend(b.r.values())
        return toks

    def op(self, eng, fn, reads=(), writes=()):
        E = self.engs[eng]
        self._wait(eng, self._deps(reads, writes))
        ins = fn()
        E["cnt"] += 1
        ins.then_inc(E["sem"], 1)
        tok = (E["sem"], E["cnt"])
        for b in reads:
            b.r[id(tok[0])] = tok
        for b in writes:
            b.w = tok
            b.r = {}
        self.ninst += 1
        return tok

    def dma(self, q, out, in_, reads=(), writes=(), **kw):
        E = self.engs[q]
        toks = []
        for b in reads:
            toks.append(b.w)
        for b in writes:
            toks.extend(b.r.values())
            if b.w is not None and (b.sem_in is None or b.w[0] is not b.sem_in):
                toks.append(b.w)
        self._wait(q, toks)
        if writes:
            own = writes[0]
            if own.sem_in is None:
                own.sem_in = self.nc.alloc_semaphore(name=f"di_{own.name}_{self.nsem}")
                self.nsem += 1
            own.n_in += 16
            sem, val = own.sem_in, own.n_in
        else:
            own = reads[0]
            if own.sem_out is None:
                own.sem_out = self.nc.alloc_semaphore(name=f"do_{own.name}_{self.nsem}")
                self.nsem += 1
            own.n_out += 16
            sem, val = own.sem_out, own.n_out
        E["e"].dma_start(out=out, in_=in_, **kw).then_inc(sem, 16)
        tok = (sem, val)
        self.dma_toks[id(sem)] = tok
        for b in reads:
            b.r[id(sem)] = tok
        for b in writes:
            b.w = tok
            b.r = {}
        self.ninst += 1
        return tok

    def barrier(self):
        toks = [(E["sem"], E["cnt"]) for E in self.engs.values() if E["cnt"] > 0]
        toks += list(self.dma_toks.values())
        for q in self.engs:
            self._wait(q, toks)

    def finish(self, bufs):
        toks = []
        for b in bufs:
            toks.append(b.w)
            toks.extend(b.r.values())
        self._wait("sp", toks)


def bcast(ap, shape):
    return ap.to_broadcast(shape)


def build_stage_mod():
    nc = bass.Bass("TRN2", target_bir_lowering=False)
    W = nc.dram_tensor("w", [D, 3072], F32, kind="ExternalInput").ap()
    Bv = nc.dram_tensor("b", [128, 24], F32, kind="ExternalInput").ap()
    S = nc.dram_tensor("s", [128, DC, 2], F32, kind="ExternalInput").ap()
    O = nc.dram_tensor("o", [128, 24, 2], F32, kind="ExternalOutput").ap()
    sc = Sched(nc)
    wt = nc.alloc_sbuf_tensor("wt", [128, DC, 3072], F32)
    bt = nc.alloc_sbuf_tensor("bt", [128, 24], F32)
    st = nc.alloc_sbuf_tensor("st", [128, DC, 2], F32)
    ot = nc.alloc_sbuf_tensor("ot", [128, 24, 2], F32)
    ps = nc.alloc_psum_tensor("ps", [128, 24, 2], F32)
    bw = sc.bufs("w", DC)
    bb, bs, bo, bp = sc.buf("b"), sc.buf("s"), sc.buf("o"), sc.buf("ps")
    Wv = W.rearrange("(k p) n -> p k n", p=128)
    sc.dma("sp", st[:], S, writes=[bs])
    sc.dma("sp", bt[:], Bv, writes=[bb])
    for k in range(DC):
        sc.dma("sp", wt[:, k, :], Wv[:, k, :], writes=[bw[k]])
    sc.op("act", lambda: nc.scalar.activation(out=st[:], in_=st[:], func=AF.Silu), reads=[bs], writes=[bs])
    for n in range(24):
        for k in range(DC):
            sc.op("pe", lambda: nc.tensor.matmul(ps[:, n, :], lhsT=wt[:, k, n * 128:(n + 1) * 128],
                                                 rhs=st[:, k, :], start=(k == 0), stop=(k == DC - 1)),
                  reads=[bw[k], bs], writes=[bp])
    sc.op("dve", lambda: nc.vector.tensor_tensor(out=ot[:], in0=ps[:], in1=bt[:].unsqueeze(2).to_broadcast([128, 24, 2]),
                                                 op=ALU.add), reads=[bp, bb], writes=[bo])
    sc.dma("sp", O, ot[:], reads=[bo])
    sc.finish([bo])
    return nc


def run_stage_mod(inp):
    nc = build_stage_mod()
    s = np.stack([inp["c"][0], inp["c_ctx"]], axis=-1)
    s = np.ascontiguousarray(s.reshape(DC, 128, 2).transpose(1, 0, 2))
    maps = []
    for r in range(NCORES):
        l, hf = r // 2, r % 2
        w = np.ascontiguousarray(inp["mod_w"][l][:, hf * 3072:(hf + 1) * 3072])
        b = np.ascontiguousarray(inp["mod_b"][l][hf * 3072:(hf + 1) * 3072].reshape(24, 128).T)
        maps.append({"w": w, "b": b, "s": s})
    res = run_bass_kernel_spmd(nc, maps, core_ids=list(range(NCORES)))
    mod = np.zeros((4, 128, 48, 2), np.float32)
    for r in range(NCORES):
        l, hf = r // 2, r % 2
        mod[l][:, hf * 24:(hf + 1) * 24, :] = res.results[r]["o"]
    return mod


def token_tiles(T_lat, T_ctx):
    tiles = []
    t = 0
    while t < T_lat:
        w = min(512, T_lat - t)
        tiles.append((t, w, 0))
        t += w
    if T_ctx:
        tiles.append((T_lat, T_ctx, 1))
    return tiles


class TL:
    def __init__(self, nc, sc):
        self.nc, self.sc = nc, sc
        self.ps = nc.alloc_psum_tensor("ps", [128, 8, 512], F32)
        self.pb = sc.bufs("psb", 8)
        self.ones = nc.alloc_sbuf_tensor("ones_bf", [128, 128], BF16)
        self.b_ones = sc.buf("ones")
        sc.op("dve", lambda: nc.vector.memset(self.ones[:], 1.0), writes=[self.b_ones])
        self.cnt = {}

    def rot(self, key, n):
        v = self.cnt.get(key, 0)
        self.cnt[key] = v + 1
        return v % n

    def load_vec(self, name, dram_ap, shape, q="sp"):
        t = self.nc.alloc_sbuf_tensor(name, shape, F32)
        b = self.sc.buf(name)
        self.sc.dma(q, t[:], dram_ap, writes=[b])
        return t, b

    def load_w_bf16(self, name, dram_ap_pkn, shape):
        t = self.nc.alloc_sbuf_tensor(name, shape, BF16)
        b = self.sc.buf(name)
        self.sc.dma("pool", t[:], dram_ap_pkn, writes=[b])
        return t, b


def emit_norm_mod(tl, x_ap, xb, w, geff, shift, col, h_out_ap, hb, tmp, tmpb, sq, sqb, rstd, rstdb,
                  h32_ap=None, h32b=None):
    nc, sc = tl.nc, tl.sc
    ps = tl.ps[:, 6, :w]
    sc.op("act", lambda: nc.scalar.activation(out=sq[:, :, :w], in_=x_ap, func=AF.Square), reads=[xb], writes=[sqb])
    for c in range(DC):
        sc.op("pe", lambda: nc.tensor.matmul(ps, lhsT=tl.ones[:], rhs=sq[:, c, :w], start=(c == 0), stop=(c == DC - 1)),
              reads=[sqb, tl.b_ones], writes=[tl.pb[6]])
    sc.op("act", lambda: nc.scalar.activation(out=rstd[:, :w], in_=ps, func=AF.Sqrt, bias=float(D * EPS), scale=1.0),
          reads=[tl.pb[6]], writes=[rstdb])
    sc.op("dve", lambda: nc.vector.reciprocal(out=rstd[:, :w], in_=rstd[:, :w]), reads=[rstdb], writes=[rstdb])
    sc.op("dve", lambda: nc.vector.tensor_tensor(out=tmp[:, :, :w], in0=x_ap,
                                                 in1=rstd[:, :w].unsqueeze(1).to_broadcast([128, DC, w]), op=ALU.mult),
          reads=[xb, rstdb], writes=[tmpb])
    for c in range(DC):
        sc.op("act", lambda: nc.scalar.activation(out=h_out_ap[:, c, :], in_=tmp[:, c, :w], func=AF.Identity,
                                                  bias=shift[:, c, col:col + 1], scale=geff[:, c, col:col + 1]),
              reads=[tmpb], writes=[hb])
        if h32_ap is not None:
            sc.op("act", lambda: nc.scalar.activation(out=h32_ap[:, c, :], in_=tmp[:, c, :w], func=AF.Identity,
                                                      bias=shift[:, c, col:col + 1], scale=geff[:, c, col:col + 1]),
                  reads=[tmpb], writes=[h32b])


def emit_geff(tl, name, mod_t, mod_b, g_t, g_b, j_scale, j_shift):
    nc, sc = tl.nc, tl.sc
    geff = nc.alloc_sbuf_tensor(name, [128, DC, 2], F32)
    gb = sc.buf(name)
    sc.op("dve", lambda: nc.vector.tensor_scalar(out=geff[:], in0=mod_t[:, j_scale * 8:(j_scale + 1) * 8, :],
                                                 scalar1=1.0, scalar2=float(math.sqrt(D)), op0=ALU.add, op1=ALU.mult),
          reads=[mod_b], writes=[gb])
    sc.op("dve", lambda: nc.vector.tensor_tensor(out=geff[:], in0=geff[:],
                                                 in1=g_t[:].unsqueeze(2).to_broadcast([128, DC, 2]), op=ALU.mult),
          reads=[gb, g_b], writes=[gb])
    return geff, gb, mod_t[:, j_shift * 8:(j_shift + 1) * 8, :]


FGROUPS = [(0, 4), (4, 4), (8, 4), (12, 4), (16, 4), (20, 2)]


def emit_ffn(tl, es, tiles, hT, hTb, xT, xTb, mod_t, mod_b, w1d, w3d, w2d, n_exp, gates=None):
    nc, sc = tl.nc, tl.sc
    A = lambda name, shape, dt: es.enter_context(nc.sbuf_tensor(name, shape, dt))
    w1t = [A(f"w1t{i}", [128, DC, 512], BF16) for i in range(2)]
    w3t = [A(f"w3t{i}", [128, DC, 512], BF16) for i in range(2)]
    w2t = [A(f"w2t{i}", [128, 4, D], BF16) for i in range(2)]
    wb = [sc.buf("wg0"), sc.buf("wg1")]
    s_sb = [A(f"s_sb{i}", [128, 512], F32) for i in range(2)]
    s_b = sc.bufs("s_b", 2)
    t_sb = [A(f"t_sb{i}", [128, 512], F32) for i in range(2)]
    t_b = sc.bufs("t_b", 2)
    g_sb = [A(f"g_sb{i}", [128, 4, 512], BF16) for i in range(2)]
    g_b = sc.bufs("g_b", 2)
    if gates is not None:
        gbc = [A(f"gbc{i}", [128, 512], F32) for i in range(2)]
        gbc_b = sc.bufs("gbc_b", 2)
    gi = 0
    for e in range(n_exp):
        for (f0, nf) in FGROUPS:
            slot = gi % 2
            gi += 1
            fw = nf * 128
            sc.dma("pool", w1t[slot][:, :, :fw],
                   w1d[e].rearrange("(k p) n -> p k n", p=128)[:, :, f0 * 128:f0 * 128 + fw], writes=[wb[slot]])
            sc.dma("pool", w3t[slot][:, :, :fw],
                   w3d[e].rearrange("(k p) n -> p k n", p=128)[:, :, f0 * 128:f0 * 128 + fw], writes=[wb[slot]])
            sc.dma("pool", w2t[slot][:, :nf, :],
                   w2d[e][f0 * 128:f0 * 128 + fw, :].rearrange("(k p) n -> p k n", p=128), writes=[wb[slot]])
            for (t0, w, ty) in tiles:
                gs = tl.rot("g", 2)
                if gates is not None:
                    bs_ = tl.rot("gbc", 2)
                    nblk = (w + 127) // 128
                    for bi in range(nblk):
                        bw_ = min(128, w - bi * 128)
                        blk = t0 // 128 + bi
                        sc.op("pe", lambda: nc.tensor.matmul(
                            tl.ps[:, 7, bi * 128:bi * 128 + bw_],
                            lhsT=gates["tm"][:bw_, blk, e:e + 1].to_broadcast([bw_, 128]),
                            rhs=gates["ident"][:bw_, :bw_], start=True, stop=True),
                            reads=[gates["b"], gates["ident_b"]], writes=[tl.pb[7]])
                    sc.op("act", lambda: nc.scalar.copy(out=gbc[bs_][:, :w], in_=tl.ps[:, 7, :w]),
                          reads=[tl.pb[7]], writes=[gbc_b[bs_]])
                for fi in range(nf):
                    b1 = 2 + tl.rot("h1", 2)
                    b3 = 4 + tl.rot("h3", 2)
                    for k in range(DC):
                        sc.op("pe", lambda: nc.tensor.matmul(tl.ps[:, b1, :w], lhsT=w1t[slot][:, k, fi * 128:(fi + 1) * 128],
                                                             rhs=hT[:, k, t0:t0 + w], start=(k == 0), stop=(k == DC - 1)),
                              reads=[wb[slot], hTb], writes=[tl.pb[b1]])
                    for k in range(DC):
                        sc.op("pe", lambda: nc.tensor.matmul(tl.ps[:, b3, :w], lhsT=w3t[slot][:, k, fi * 128:(fi + 1) * 128],
                                                             rhs=hT[:, k, t0:t0 + w], start=(k == 0), stop=(k == DC - 1)),
                              reads=[wb[slot], hTb], writes=[tl.pb[b3]])
                    ss = tl.rot("s", 2)
                    sc.op("act", lambda: nc.scalar.activation(out=s_sb[ss][:, :w], in_=tl.ps[:, b1, :w], func=AF.Silu),
                          reads=[tl.pb[b1]], writes=[s_b[ss]])
                    if gates is None:
                        sc.op("dve", lambda: nc.vector.tensor_tensor(out=g_sb[gs][:, fi, :w], in0=tl.ps[:, b3, :w],
                                                                     in1=s_sb[ss][:, :w], op=ALU.mult),
                              reads=[tl.pb[b3], s_b[ss]], writes=[g_b[gs]])
                    else:
                        ts = tl.rot("t", 2)
                        sc.op("dve", lambda: nc.vector.tensor_tensor(out=t_sb[ts][:, :w], in0=tl.ps[:, b3, :w],
                                                                     in1=s_sb[ss][:, :w], op=ALU.mult),
                              reads=[tl.pb[b3], s_b[ss]], writes=[t_b[ts]])
                        sc.op("pool", lambda: nc.gpsimd.tensor_tensor(out=g_sb[gs][:, fi, :w], in0=t_sb[ts][:, :w],
                                                                      in1=gbc[bs_][:, :w], op=ALU.mult),
                              reads=[t_b[ts], gbc_b[bs_]], writes=[g_b[gs]])
                for dc in range(DC):
                    bo = tl.rot("o", 2)
                    for fi in range(nf):
                        sc.op("pe", lambda: nc.tensor.matmul(tl.ps[:, bo, :w], lhsT=w2t[slot][:, fi, dc * 128:(dc + 1) * 128],
                                                             rhs=g_sb[gs][:, fi, :w], start=(fi == 0), stop=(fi == nf - 1)),
                              reads=[wb[slot], g_b[gs]], writes=[tl.pb[bo]])
                    sc.op("dve", lambda: nc.vector.scalar_tensor_tensor(
                        out=xT[:, dc, t0:t0 + w], in0=tl.ps[:, bo, :w], scalar=mod_t[:, 40 + dc, ty:ty + 1],
                        in1=xT[:, dc, t0:t0 + w], op0=ALU.mult, op1=ALU.add),
                        reads=[tl.pb[bo], mod_b, xTb[(t0, dc)]], writes=[xTb[(t0, dc)]])


def emit_gates(tl, tiles, h32, h32b, t0, w, router_t, router_b, gates):
    nc, sc = tl.nc, tl.sc
    nblk = (w + 127) // 128
    lg = gates["lg"]
    for bi in range(nblk):
        bw_ = min(128, w - bi * 128)
        blk = t0 // 128 + bi
        for k in range(DC):
            sc.op("pe", lambda: nc.tensor.matmul(tl.ps[:bw_, 7, 0:8], lhsT=h32[:, k, bi * 128:bi * 128 + bw_],
                                                 rhs=router_t[:, k, :], start=(k == 0), stop=(k == DC - 1)),
                  reads=[h32b, router_b], writes=[tl.pb[7]])
        sc.op("dve", lambda: nc.vector.tensor_copy(out=lg[:bw_, 0:8], in_=tl.ps[:bw_, 7, 0:8]),
              reads=[tl.pb[7]], writes=[gates["lgb"]])
        sc.op("dve", lambda: nc.vector.tensor_reduce(out=lg[:bw_, 8:9], in_=lg[:bw_, 0:8], axis=AX.X, op=ALU.max),
              reads=[gates["lgb"]], writes=[gates["lgb"]])
        sc.op("dve", lambda: nc.vector.tensor_scalar(out=lg[:bw_, 16:24], in0=lg[:bw_, 0:8], scalar1=lg[:bw_, 8:9],
                                                     scalar2=None, op0=ALU.is_equal),
              reads=[gates["lgb"]], writes=[gates["lgb"]])
        sc.op("dve", lambda: nc.vector.scalar_tensor_tensor(out=lg[:bw_, 24:32], in0=lg[:bw_, 16:24], scalar=-1e30,
                                                            in1=lg[:bw_, 0:8], op0=ALU.mult, op1=ALU.add),
              reads=[gates["lgb"]], writes=[gates["lgb"]])
        sc.op("dve", lambda: nc.vector.tensor_reduce(out=lg[:bw_, 9:10], in_=lg[:bw_, 24:32], axis=AX.X, op=ALU.max),
              reads=[gates["lgb"]], writes=[gates["lgb"]])
        sc.op("dve", lambda: nc.vector.tensor_scalar(out=lg[:bw_, 32:40], in0=lg[:bw_, 24:32], scalar1=lg[:bw_, 9:10],
                                                     scalar2=None, op0=ALU.is_equal),
              reads=[gates["lgb"]], writes=[gates["lgb"]])
        sc.op("dve", lambda: nc.vector.tensor_tensor(out=lg[:bw_, 10:11], in0=lg[:bw_, 9:10], in1=lg[:bw_, 8:9],
                                                     op=ALU.subtract),
              reads=[gates["lgb"]], writes=[gates["lgb"]])
        sc.op("act", lambda: nc.scalar.activation(out=lg[:bw_, 11:12], in_=lg[:bw_, 10:11], func=AF.Sigmoid),
              reads=[gates["lgb"]], writes=[gates["lgb"]])
        sc.op("dve", lambda: nc.vector.tensor_scalar(out=lg[:bw_, 12:13], in0=lg[:bw_, 11:12], scalar1=-1.0,
                                                     scalar2=1.0, op0=ALU.mult, op1=ALU.add),
              reads=[gates["lgb"]], writes=[gates["lgb"]])
        sc.op("dve", lambda: nc.vector.tensor_scalar(out=lg[:bw_, 32:40], in0=lg[:bw_, 32:40], scalar1=lg[:bw_, 11:12],
                                                     scalar2=None, op0=ALU.mult),
              reads=[gates["lgb"]], writes=[gates["lgb"]])
        sc.op("dve", lambda: nc.vector.scalar_tensor_tensor(out=gates["tm"][:bw_, blk, :], in0=lg[:bw_, 16:24],
                                                            scalar=lg[:bw_, 12:13], in1=lg[:bw_, 32:40],
                                                            op0=ALU.mult, op1=ALU.add),
              reads=[gates["lgb"]], writes=[gates["b"]])


def emit_norm_mod_multi(tl, x_ap, xbs, w, geff, shift, geff_b, mod_b, col, h_out_ap, hb, tmp, tmpb, sq, sqb, rstd, rstdb):
    nc, sc = tl.nc, tl.sc
    ps = tl.ps[:, 6, :w]
    sc.op("act", lambda: nc.scalar.activation(out=sq[:, :, :w], in_=x_ap, func=AF.Square), reads=xbs, writes=[sqb])
    for c in range(DC):
        sc.op("pe", lambda: nc.tensor.matmul(ps, lhsT=tl.ones[:], rhs=sq[:, c, :w], start=(c == 0), stop=(c == DC - 1)),
              reads=[sqb, tl.b_ones], writes=[tl.pb[6]])
    sc.op("act", lambda: nc.scalar.activation(out=rstd[:, :w], in_=ps, func=AF.Sqrt, bias=float(D * EPS), scale=1.0),
          reads=[tl.pb[6]], writes=[rstdb])
    sc.op("dve", lambda: nc.vector.reciprocal(out=rstd[:, :w], in_=rstd[:, :w]), reads=[rstdb], writes=[rstdb])
    sc.op("dve", lambda: nc.vector.tensor_tensor(out=tmp[:, :, :w], in0=x_ap,
                                                 in1=rstd[:, :w].unsqueeze(1).to_broadcast([128, DC, w]), op=ALU.mult),
          reads=list(xbs) + [rstdb], writes=[tmpb])
    for c in range(DC):
        sc.op("act", lambda: nc.scalar.activation(out=tmp[:, c, :w], in_=tmp[:, c, :w], func=AF.Identity,
                                                  bias=shift[:, c, col:col + 1], scale=geff[:, c, col:col + 1]),
              reads=[tmpb, geff_b, mod_b], writes=[tmpb])
    sc.op("pool", lambda: nc.gpsimd.tensor_copy(out=h_out_ap, in_=tmp[:, :, :w]), reads=[tmpb], writes=[hb])


def emit_final_norm(tl, x_ap, xbs, w, gf_t, gf_b, tmp, tmpb, sq, sqb, rstd, rstdb):
    nc, sc = tl.nc, tl.sc
    ps = tl.ps[:, 6, :w]
    sc.op("act", lambda: nc.scalar.activation(out=sq[:, :, :w], in_=x_ap, func=AF.Square), reads=xbs, writes=[sqb])
    for c in range(DC):
        sc.op("pe", lambda: nc.tensor.matmul(ps, lhsT=tl.ones[:], rhs=sq[:, c, :w], start=(c == 0), stop=(c == DC - 1)),
              reads=[sqb, tl.b_ones], writes=[tl.pb[6]])
    sc.op("act", lambda: nc.scalar.activation(out=rstd[:, :w], in_=ps, func=AF.Sqrt, bias=float(D * EPS), scale=1.0),
          reads=[tl.pb[6]], writes=[rstdb])
    sc.op("dve", lambda: nc.vector.reciprocal(out=rstd[:, :w], in_=rstd[:, :w]), reads=[rstdb], writes=[rstdb])
    sc.op("dve", lambda: nc.vector.tensor_tensor(out=tmp[:, :, :w], in0=x_ap,
                                                 in1=rstd[:, :w].unsqueeze(1).to_broadcast([128, DC, w]), op=ALU.mult),
          reads=list(xbs) + [rstdb], writes=[tmpb])
    for c in range(DC):
        sc.op("dve", lambda: nc.vector.tensor_scalar(out=tmp[:, c, :w], in0=tmp[:, c, :w], scalar1=gf_t[:, c:c + 1],
                                                     scalar2=float(math.sqrt(D)), op0=ALU.mult, op1=ALU.mult),
              reads=[tmpb, gf_b], writes=[tmpb])


def build_post(T_lat, T_ctx, moe, final_norm):
    import contextlib
    nc = bass.Bass("TRN2", target_bir_lowering=False)
    T = T_lat + T_ctx
    n_exp = NE if moe else 1
    Xd = nc.dram_tensor("xT", [D, T], F32, kind="ExternalInput").ap()
    Od = nc.dram_tensor("oT", [D, T], BF16, kind="ExternalInput").ap()
    Wod = nc.dram_tensor("w_out", [D, D], F32, kind="ExternalInput").ap()
    Md = nc.dram_tensor("mod", [128, 48, 2], F32, kind="ExternalInput").ap()
    Gd = nc.dram_tensor("g2", [128, DC], F32, kind="ExternalInput").ap()
    FGd = nc.dram_tensor("gf", [128, DC], F32, kind="ExternalInput").ap()
    W1d = nc.dram_tensor("w1", [n_exp, D, FFN], F32, kind="ExternalInput").ap()
    W3d = nc.dram_tensor("w3", [n_exp, D, FFN], F32, kind="ExternalInput").ap()
    W2d = nc.dram_tensor("w2", [n_exp, FFN, D], F32, kind="ExternalInput").ap()
    Rd = nc.dram_tensor("router", [D, NE], F32, kind="ExternalInput").ap()
    Idd = nc.dram_tensor("ident", [128, 128], F32, kind="ExternalInput").ap()
    Yd = nc.dram_tensor("yT", [D, T], F32, kind="ExternalOutput").ap()
    sc = Sched(nc)
    tl = TL(nc, sc)
    tiles = token_tiles(T_lat, T_ctx)
    xT = nc.alloc_sbuf_tensor("xT_sb", [128, DC, T], F32)
    xTb = {(t0, dc): sc.buf(f"x{t0}_{dc}") for (t0, w, ty) in tiles for dc in range(DC)}
    hT = nc.alloc_sbuf_tensor("hT_sb", [128, DC, T], BF16)
    hTb = sc.buf("hT")
    mod_t, mod_b = tl.load_vec("mod_sb", Md, [128, 48, 2])
    g2_t, g2_b = tl.load_vec("g2_sb", Gd, [128, DC])
    gf_t, gf_b = tl.load_vec("gf_sb", FGd, [128, DC])
    Xv = Xd.rearrange("(c p) t -> p c t", p=128)
    Yv = Yd.rearrange("(c p) t -> p c t", p=128)
    for (t0, w, ty) in tiles:
        for dc in range(DC):
            sc.dma("sp", xT[:, dc, t0:t0 + w], Xv[:, dc, t0:t0 + w], writes=[xTb[(t0, dc)]])
    geff, geff_b, shift = emit_geff(tl, "geff2", mod_t, mod_b, g2_t, g2_b, 4, 3)
    gates = None
    if moe:
        router_t, router_b = tl.load_vec("router_sb", Rd.rearrange("(k p) e -> p k e", p=128), [128, DC, NE])
        ident_t, ident_b = tl.load_vec("ident_sb", Idd, [128, 128])
        nblk_tot = (T + 127) // 128
        gates = dict(tm=nc.alloc_sbuf_tensor("gates_tm", [128, nblk_tot, NE], F32), b=sc.buf("gates"),
                     lg=nc.alloc_sbuf_tensor("lg_sb", [128, 40], F32), lgb=sc.buf("lg"),
                     ident=ident_t, ident_b=ident_b)
    with contextlib.ExitStack() as es:
        A = lambda name, shape, dt: es.enter_context(nc.sbuf_tensor(name, shape, dt))
        tmp, tmpb = A("tmp_sb", [128, DC, 512], F32), sc.buf("tmp")
        sq, sqb = A("sq_sb", [128, DC, 512], BF16), sc.buf("sq")
        rstd, rstdb = A("rstd_sb", [128, 512], F32), sc.buf("rstd")
        wo_t, wo_b = A("wo_sb", [128, DC, D], BF16), sc.buf("wo")
        sc.dma("pool", wo_t[:], Wod.rearrange("(k p) n -> p k n", p=128), writes=[wo_b])
        oT, oTb = A("oT_sb", [128, DC, 512], BF16), sc.buf("oT")
        Ov = Od.rearrange("(c p) t -> p c t", p=128)
        for (t0, w, ty) in tiles:
            sc.dma("sp", oT[:, :, :w], Ov[:, :, t0:t0 + w], writes=[oTb])
            for dc in range(DC):
                bo = tl.rot("o", 2)
                for k in range(DC):
                    sc.op("pe", lambda: nc.tensor.matmul(tl.ps[:, bo, :w], lhsT=wo_t[:, k, dc * 128:(dc + 1) * 128],
                                                         rhs=oT[:, k, :w], start=(k == 0), stop=(k == DC - 1)),
                          reads=[wo_b, oTb], writes=[tl.pb[bo]])
                sc.op("dve", lambda: nc.vector.scalar_tensor_tensor(
                    out=xT[:, dc, t0:t0 + w], in0=tl.ps[:, bo, :w], scalar=mod_t[:, 16 + dc, ty:ty + 1],
                    in1=xT[:, dc, t0:t0 + w], op0=ALU.mult, op1=ALU.add),
                    reads=[tl.pb[bo], mod_b, xTb[(t0, dc)]], writes=[xTb[(t0, dc)]])
            xbs = [xTb[(t0, dc)] for dc in range(DC)]
            emit_norm_mod_multi(tl, xT[:, :, t0:t0 + w], xbs, w, geff, shift, geff_b, mod_b, ty,
                                hT[:, :, t0:t0 + w], hTb, tmp, tmpb, sq, sqb, rstd, rstdb)
            if moe:
                emit_gates(tl, tiles, tmp, tmpb, t0, w, router_t, router_b, gates)
        sc.barrier()
    with contextlib.ExitStack() as es:
        emit_ffn(tl, es, tiles, hT, hTb, xT, xTb, mod_t, mod_b, W1d, W3d, W2d, n_exp, gates)
        sc.barrier()
    with contextlib.ExitStack() as es:
        A = lambda name, shape, dt: es.enter_context(nc.sbuf_tensor(name, shape, dt))
        outb = []
        if final_norm:
            tmps = [A(f"tmpf{i}", [128, DC, 512], F32) for i in range(2)]
            tmpbs = sc.bufs("tmpf", 2)
            sq, sqb = A("sqf_sb", [128, DC, 512], BF16), sc.buf("sqf")
            rstd, rstdb = A("rstdf_sb", [128, 512], F32), sc.buf("rstdf")
        for ti, (t0, w, ty) in enumerate(tiles):
            if final_norm and ty == 0:
                xbs = [xTb[(t0, dc)] for dc in range(DC)]
                tmp, tmpb = tmps[ti % 2], tmpbs[ti % 2]
                emit_final_norm(tl, xT[:, :, t0:t0 + w], xbs, w, gf_t, gf_b, tmp, tmpb, sq, sqb, rstd, rstdb)
                for dc in range(DC):
                    sc.dma("sp", Yv[:, dc, t0:t0 + w], tmp[:, dc, :w], reads=[tmpb])
                outb.append(tmpb)
            else:
                for dc in range(DC):
                    sc.dma("sp", Yv[:, dc, t0:t0 + w], xT[:, dc, t0:t0 + w], reads=[xTb[(t0, dc)]])
                    outb.append(xTb[(t0, dc)])
        sc.finish(outb)
    return nc


def to_fm(a):
    return np.ascontiguousarray(a.T)


def vec_pc(v):
    return np.ascontiguousarray(v.reshape(DC, 128).T)


def shard_tokens(lat_fm, ctx_fm, r, use_ctx=True):
    parts = [lat_fm[:, r * 2048:(r + 1) * 2048]]
    if use_ctx:
        parts.append(ctx_fm[:, r * 32:(r + 1) * 32])
    return np.ascontiguousarray(np.concatenate(parts, axis=1))


_prog_cache = {}


def get_prog(key, builder):
    if key not in _prog_cache:
        _prog_cache[key] = builder()
    return _prog_cache[key]


def run_post(xl_fm, xc_fm, ol_fm, oc_fm, mod_l, inp, w_out, g2, li, moe, final_norm):
    use_ctx = xc_fm is not None
    T_ctx = 32 if use_ctx else 0
    nc = build_post(2048, T_ctx, moe, final_norm)
    fi = li // 2
    if moe:
        w1, w3, w2 = inp["moe_w1"][fi], inp["moe_w3"][fi], inp["moe_w2"][fi]
        router = inp["moe_router"][fi]
    else:
        w1, w3, w2 = inp["ffn_w1"][fi][None], inp["ffn_w3"][fi][None], inp["ffn_w2"][fi][None]
        router = np.zeros((D, NE), np.float32)
    common = {"w_out": np.ascontiguousarray(w_out), "mod": np.ascontiguousarray(mod_l), "g2": vec_pc(g2),
              "gf": vec_pc(inp["final_g"]), "w1": np.ascontiguousarray(w1), "w3": np.ascontiguousarray(w3),
              "w2": np.ascontiguousarray(w2), "router": np.ascontiguousarray(router),
              "ident": np.eye(128, dtype=np.float32)}
    maps = []
    for r in range(NCORES):
        m = dict(common)
        m["xT"] = shard_tokens(xl_fm, xc_fm, r, use_ctx)
        m["oT"] = shard_tokens(ol_fm, oc_fm, r, use_ctx)
        maps.append(m)
    res = run_bass_kernel_spmd(nc, maps, core_ids=list(range(NCORES)))
    ys = [res.results[r]["yT"] for r in range(NCORES)]
    xl_new = np.concatenate([y[:, :2048] for y in ys], axis=1)
    xc_new = np.concatenate([y[:, 2048:] for y in ys], axis=1) if use_ctx else None
    return xl_new, xc_new


def build_pre_attn(T_lat, T_ctx):
    nc = bass.Bass("TRN2", target_bir_lowering=False)
    T = T_lat + T_ctx
    Xd = nc.dram_tensor("xT", [D, T], F32, kind="ExternalInput").ap()
    Wd = nc.dram_tensor("w_in", [D, 3 * D], F32, kind="ExternalInput").ap()
    Md = nc.dram_tensor("mod", [128, 48, 2], F32, kind="ExternalInput").ap()
    Gd = nc.dram_tensor("g1", [128, DC], F32, kind="ExternalInput").ap()
    Cd = nc.dram_tensor("cos", [128, T], F32, kind="ExternalInput").ap()
    Sd = nc.dram_tensor("sin", [128, T], F32, kind="ExternalInput").ap()
    Qd = nc.dram_tensor("qkvT", [3 * D, T], BF16, kind="ExternalOutput").ap()
    sc = Sched(nc)
    tl = TL(nc, sc)
    tiles = token_tiles(T_lat, T_ctx)
    mod_t, mod_b = tl.load_vec("mod_sb", Md, [128, 48, 2])
    g1_t, g1_b = tl.load_vec("g1_sb", Gd, [128, DC])
    cos_t, cos_b = tl.load_vec("cos_sb", Cd, [128, T])
    sin_t, sin_b = tl.load_vec("sin_sb", Sd, [128, T])
    geff, geff_b, shift = emit_geff(tl, "geff1", mod_t, mod_b, g1_t, g1_b, 1, 0)
    w_t, w_b = tl.load_w_bf16("w_sb", Wd.rearrange("(k p) n -> p k n", p=128), [128, DC, 3 * D])
    wr_t = nc.alloc_sbuf_tensor("wr_sb", [128, DC, 2 * D], BF16)
    wr_b = sc.buf("wr")
    wv = w_t[:, :, 0:2 * D].rearrange("p k (m q e) -> p k m q e", q=4, e=16)
    wrv = wr_t[:].rearrange("p k (m q e) -> p k m q e", q=4, e=16)
    for k in range(DC):
        for (dst, src, sgn) in ((0, 1, -1.0), (1, 0, 1.0), (2, 3, -1.0), (3, 2, 1.0)):
            sc.op("pool", lambda: nc.gpsimd.tensor_scalar(out=wrv[:, k, :, dst, :], in0=wv[:, k, :, src, :], scalar1=sgn,
                                                          scalar2=None, op0=ALU.mult), reads=[w_b], writes=[wr_b])
    xt = nc.alloc_sbuf_tensor("x_sb", [128, DC, 512], F32)
    xb = sc.buf("x")
    hT = nc.alloc_sbuf_tensor("hT_sb", [128, DC, 512], BF16)
    hTb = sc.buf("hT")
    tmp, tmpb = nc.alloc_sbuf_tensor("tmp_sb", [128, DC, 512], F32), sc.buf("tmp")
    sq, sqb = nc.alloc_sbuf_tensor("sq_sb", [128, DC, 512], BF16), sc.buf("sq")
    rstd, rstdb = nc.alloc_sbuf_tensor("rstd_sb", [128, 512], F32), sc.buf("rstd")
    t1 = [nc.alloc_sbuf_tensor(f"t1_{i}", [128, 512], F32) for i in range(2)]
    t1b = sc.bufs("t1b", 2)
    t2 = [nc.alloc_sbuf_tensor(f"t2_{i}", [128, 512], F32) for i in range(2)]
    t2b = sc.bufs("t2b", 2)
    ob = [nc.alloc_sbuf_tensor(f"ob_{i}", [128, 512], BF16) for i in range(4)]
    obb = sc.bufs("obb", 4)
    Xv = Xd.rearrange("(c p) t -> p c t", p=128)
    Qv = Qd.rearrange("(c p) t -> p c t", p=128)
    outs = []
    for (t0, w, ty) in tiles:
        sc.dma("sp", xt[:, :, :w], Xv[:, :, t0:t0 + w], writes=[xb])
        emit_norm_mod_multi(tl, xt[:, :, :w], [xb], w, geff, shift, geff_b, mod_b, ty, hT[:, :, :w], hTb,
                            tmp, tmpb, sq, sqb, rstd, rstdb)
        for mc in range(24):
            oi = tl.rot("ob", 4)
            ba = tl.rot("pa", 2)
            for k in range(DC):
                sc.op("pe", lambda: nc.tensor.matmul(tl.ps[:, ba, :w], lhsT=w_t[:, k, mc * 128:(mc + 1) * 128],
                                                     rhs=hT[:, k, :w], start=(k == 0), stop=(k == DC - 1)),
                      reads=[w_b, hTb], writes=[tl.pb[ba]])
            if mc < 16:
                bb_ = 2 + tl.rot("pb", 2)
                for k in range(DC):
                    sc.op("pe", lambda: nc.tensor.matmul(tl.ps[:, bb_, :w], lhsT=wr_t[:, k, mc * 128:(mc + 1) * 128],
                                                         rhs=hT[:, k, :w], start=(k == 0), stop=(k == DC - 1)),
                          reads=[wr_b, hTb], writes=[tl.pb[bb_]])
                ti = tl.rot("t12", 2)
                sc.op("dve", lambda: nc.vector.tensor_tensor(out=t1[ti][:, :w], in0=tl.ps[:, ba, :w], in1=cos_t[:, t0:t0 + w],
                                                             op=ALU.mult), reads=[tl.pb[ba], cos_b], writes=[t1b[ti]])
                sc.op("dve", lambda: nc.vector.tensor_tensor(out=t2[ti][:, :w], in0=tl.ps[:, bb_, :w], in1=sin_t[:, t0:t0 + w],
                                                             op=ALU.mult), reads=[tl.pb[bb_], sin_b], writes=[t2b[ti]])
                sc.op("pool", lambda: nc.gpsimd.tensor_tensor(out=ob[oi][:, :w], in0=t1[ti][:, :w], in1=t2[ti][:, :w],
                                                              op=ALU.add), reads=[t1b[ti], t2b[ti]], writes=[obb[oi]])
            else:
                sc.op("act", lambda: nc.scalar.copy(out=ob[oi][:, :w], in_=tl.ps[:, ba, :w]),
                      reads=[tl.pb[ba]], writes=[obb[oi]])
            sc.dma("sp", Qv[:, mc, t0:t0 + w], ob[oi][:, :w], reads=[obb[oi]])
    sc.finish(obb)
    return nc


def rope_tables():
    rows = SEQ // 64
    row = np.repeat(np.arange(rows, dtype=np.float32), 64)
    col = np.tile(np.arange(64, dtype=np.float32), rows)
    inv_freq = (10000.0 ** (-np.arange(0, 32, 2, dtype=np.float32) / 32)).astype(np.float32)
    ang_r = row[:, None] * inv_freq
    ang_c = col[:, None] * inv_freq
    ang = np.concatenate([ang_r, ang_r, ang_c, ang_c], axis=-1)
    return np.cos(ang).T.astype(np.float32), np.sin(ang).T.astype(np.float32)


def run_pre_attn(xl_fm, xc_fm, mod_l, g1, w_in):
    nc = build_pre_attn(2048, 32)
    cos, sin = rope_tables()
    cos2 = np.concatenate([cos, cos], axis=0)
    sin2 = np.concatenate([sin, sin], axis=0)
    maps = []
    for r in range(NCORES):
        c = np.concatenate([cos2[:, r * 2048:(r + 1) * 2048], np.ones((128, 32), np.float32)], axis=1)
        s = np.concatenate([sin2[:, r * 2048:(r + 1) * 2048], np.zeros((128, 32), np.float32)], axis=1)
        maps.append({"xT": shard_tokens(xl_fm, xc_fm, r), "w_in": np.ascontiguousarray(w_in),
                     "mod": np.ascontiguousarray(mod_l), "g1": vec_pc(g1),
                     "cos": np.ascontiguousarray(c), "sin": np.ascontiguousarray(s)})
    res = run_bass_kernel_spmd(nc, maps, core_ids=list(range(NCORES)))
    qs = [res.results[r]["qkvT"] for r in range(NCORES)]
    lat = np.concatenate([q[:, :2048] for q in qs], axis=1)
    ctx = np.concatenate([q[:, 2048:] for q in qs], axis=1)
    return lat, ctx


NK_ALL = NCTX + SEQ
NKC_ALL = NK_ALL // 128


def build_attn_core(TQ_lat, TQ_ctx, lam_init):
    nc = bass.Bass("TRN2", target_bir_lowering=False)
    TQ = TQ_lat + TQ_ctx
    Qd = nc.dram_tensor("qT", [D, TQ], BF16, kind="ExternalInput").ap()
    Kd = nc.dram_tensor("kT", [D, NK_ALL], BF16, kind="ExternalInput").ap()
    Vd = nc.dram_tensor("v", [8, 128, NKC_ALL, 128], BF16, kind="ExternalInput").ap()
    Ld = nc.dram_tensor("lam_p", [128, 4, 64], F32, kind="ExternalInput").ap()
    Sd = nc.dram_tensor("subln", [128, 128], F32, kind="ExternalInput").ap()
    Od = nc.dram_tensor("o", [TQ, D], BF16, kind="ExternalOutput").ap()
    sc = Sched(nc)
    tl = TL(nc, sc)
    tiles = token_tiles(TQ_lat, TQ_ctx)
    lp_t, lp_b = tl.load_vec("lp_sb", Ld, [128, 4, 64])
    sub_t, sub_b = tl.load_vec("sub_sb", Sd, [128, 128])
    lt = nc.alloc_sbuf_tensor("lt_sb", [128, 2, 64], F32)
    ls = nc.alloc_sbuf_tensor("ls_sb", [128, 4], F32)
    lb = sc.buf("lam")
    lpv = lp_t[:].rearrange("p (a b) d -> p a b d", b=2)
    sc.op("dve", lambda: nc.vector.tensor_tensor(out=lt[:], in0=lpv[:, :, 0, :], in1=lpv[:, :, 1, :], op=ALU.mult),
          reads=[lp_b], writes=[lb])
    sc.op("dve", lambda: nc.vector.tensor_reduce(out=ls[:, 0:2], in_=lt[:], axis=AX.X, op=ALU.add), reads=[lb], writes=[lb])
    sc.op("act", lambda: nc.scalar.activation(out=ls[:, 0:2], in_=ls[:, 0:2], func=AF.Exp), reads=[lb], writes=[lb])
    sc.op("dve", lambda: nc.vector.tensor_tensor(out=ls[:, 2:3], in0=ls[:, 1:2], in1=ls[:, 0:1], op=ALU.subtract),
          reads=[lb], writes=[lb])
    sc.op("dve", lambda: nc.vector.tensor_scalar(out=ls[:, 3:4], in0=ls[:, 2:3], scalar1=float(-lam_init), scalar2=None,
                                                 op0=ALU.add), reads=[lb], writes=[lb])
    neg_lam = ls[:, 3:4]
    sc.op("dve", lambda: nc.vector.tensor_scalar(out=sub_t[:], in0=sub_t[:], scalar1=float(1.0 - lam_init), scalar2=None,
                                                 op0=ALU.mult), reads=[sub_b], writes=[sub_b])
    kT = [nc.alloc_sbuf_tensor(f"kT{i}", [128, NK_ALL], BF16) for i in range(2)]
    kb = sc.bufs("kb", 2)
    vt = [nc.alloc_sbuf_tensor(f"vt{i}", [128, NKC_ALL, 129], BF16) for i in range(2)]
    vb = sc.bufs("vb", 2)
    qt = [nc.alloc_sbuf_tensor(f"qt{i}", [128, TQ], BF16) for i in range(2)]
    qb_ = sc.bufs("qb", 2)
    for i in range(2):
        sc.op("pool", lambda: nc.gpsimd.memset(vt[i][:, :, 128:129], 1.0), writes=[vb[i]])
    P = [nc.alloc_sbuf_tensor(f"P{i}", [128, 2, 512], BF16) for i in range(3)]
    Pb = sc.bufs("Pb", 3)
    rr = nc.alloc_sbuf_tensor("rr", [128, 8], F32)
    rrb = sc.buf("rr")
    d0 = nc.alloc_sbuf_tensor("d0", [128, 128], F32)
    d1 = nc.alloc_sbuf_tensor("d1", [128, 128], F32)
    junk = nc.alloc_sbuf_tensor("junk", [128, 128], F32)
    db = sc.buf("d")
    osb = [nc.alloc_sbuf_tensor(f"osb{i}", [128, 4, 128], BF16) for i in range(2)]
    osbb = sc.bufs("osbb", 2)
    ps = tl.ps

    def emit_qk(slot, t0, w, kc, sbk):
        for m in range(2):
            sc.op("pe", lambda: nc.tensor.matmul(ps[:, 2 * sbk + m, :w], lhsT=kT[slot][m * 64:(m + 1) * 64, kc * 128:(kc + 1) * 128],
                                                 rhs=qt[slot][m * 64:(m + 1) * 64, t0:t0 + w], start=True, stop=True),
                  reads=[kb[slot], qb_[slot]], writes=[tl.pb[2 * sbk + m]])

    for h in range(8):
        slot = h % 2
        sc.dma("sp", kT[slot][:], Kd[h * 128:(h + 1) * 128, :], writes=[kb[slot]])
        sc.dma("sp", vt[slot][:, :, 0:128], Vd[h], writes=[vb[slot]])
        sc.dma("sp", qt[slot][:], Qd[h * 128:(h + 1) * 128, :], writes=[qb_[slot]])
        for (t0, w, ty) in tiles:
            nkc = NKC_ALL if ty == 0 else NCTX // 128
            nqb = (w + 127) // 128
            sbk = tl.rot("S", 2)
            emit_qk(slot, t0, w, 0, sbk)
            for kc in range(nkc):
                cur = sbk
                if kc + 1 < nkc:
                    sbk = tl.rot("S", 2)
                    emit_qk(slot, t0, w, kc + 1, sbk)
                pi = tl.rot("P", 3)
                sc.op("act", lambda: nc.scalar.activation(out=P[pi][:, :, :w], in_=ps[:, 2 * cur:2 * cur + 2, :w], func=AF.Exp,
                                                          scale=0.125),
                      reads=[tl.pb[2 * cur], tl.pb[2 * cur + 1]], writes=[Pb[pi]])
                seen_bank = set()
                for m in range(2):
                    for qi in range(nqb):
                        bw_ = min(128, w - qi * 128)
                        so = m * 4 + qi
                        bank, off = 4 + so // 3, (so % 3) * 129
                        first = (kc == 0) and (bank not in seen_bank)
                        seen_bank.add(bank)
                        sc.op("pe", lambda: nc.tensor.matmul(ps[:bw_, bank, off:off + 129], lhsT=P[pi][:, m, qi * 128:qi * 128 + bw_],
                                                             rhs=vt[slot][:, kc, :], start=first, stop=(kc == nkc - 1),
                                                             skip_group_check=True),
                              reads=[Pb[pi], vb[slot]], writes=[tl.pb[bank]])
            oi = tl.rot("osb", 2)
            for qi in range(nqb):
                bw_ = min(128, w - qi * 128)
                b0, o0 = 4 + qi // 3, (qi % 3) * 129
                b1, o1 = 4 + (4 + qi) // 3, ((4 + qi) % 3) * 129
                O0 = ps[:bw_, b0, o0:o0 + 129]
                O1 = ps[:bw_, b1, o1:o1 + 129]
                sc.op("dve", lambda: nc.vector.reciprocal(out=rr[:bw_, 0:1], in_=O0[:, 128:129]), reads=[tl.pb[b0]], writes=[rrb])
                sc.op("dve", lambda: nc.vector.reciprocal(out=rr[:bw_, 1:2], in_=O1[:, 128:129]), reads=[tl.pb[b1]], writes=[rrb])
                sc.op("dve", lambda: nc.vector.tensor_tensor(out=rr[:bw_, 2:3], in0=rr[:bw_, 1:2], in1=neg_lam[:bw_, :], op=ALU.mult),
                      reads=[rrb, lb], writes=[rrb])
                sc.op("dve", lambda: nc.vector.tensor_scalar(out=d0[:bw_, :], in0=O0[:, 0:128], scalar1=rr[:bw_, 0:1], scalar2=None,
                                                             op0=ALU.mult), reads=[tl.pb[b0], rrb], writes=[db])
                sc.op("dve", lambda: nc.vector.scalar_tensor_tensor(out=d1[:bw_, :], in0=O1[:, 0:128], scalar=rr[:bw_, 2:3],
                                                                    in1=d0[:bw_, :], op0=ALU.mult, op1=ALU.add),
                      reads=[tl.pb[b1], rrb, db], writes=[db])
                sc.op("act", lambda: nc.scalar.activation(out=junk[:bw_, :], in_=d1[:bw_, :], func=AF.Square), reads=[db], writes=[db])
                sc.op("dve", lambda: nc.vector.tensor_reduce(out=rr[:bw_, 3:4], in_=junk[:bw_, :], axis=AX.X, op=ALU.add),
                      reads=[db], writes=[rrb])
                sc.op("act", lambda: nc.scalar.activation(out=rr[:bw_, 4:5], in_=rr[:bw_, 3:4], func=AF.Sqrt, bias=1e-5,
                                                          scale=1.0 / 128), reads=[rrb], writes=[rrb])
                sc.op("dve", lambda: nc.vector.reciprocal(out=rr[:bw_, 5:6], in_=rr[:bw_, 4:5]), reads=[rrb], writes=[rrb])
                sc.op("dve", lambda: nc.vector.scalar_tensor_tensor(out=osb[oi][:bw_, qi, :], in0=d1[:bw_, :], scalar=rr[:bw_, 5:6],
                                                                    in1=sub_t[:bw_, :], op0=ALU.mult, op1=ALU.mult),
                      reads=[db, rrb, sub_b], writes=[osbb[oi]])
            if w % 128 == 0:
                sc.dma("sp", Od[t0:t0 + w, h * 128:(h + 1) * 128].rearrange("(q p) e -> p q e", p=128), osb[oi][:, :nqb, :],
                       reads=[osbb[oi]])
            else:
                sc.dma("sp", Od[t0:t0 + w, h * 128:(h + 1) * 128], osb[oi][:w, 0, :], reads=[osbb[oi]])
    sc.finish(osbb)
    return nc


def run_attn_core(qkv_lat, qkv_ctx, lam_p, subln, lam_init, with_ctx_q):
    nc = build_attn_core(2048, 32 if with_ctx_q else 0, lam_init)
    kT = np.ascontiguousarray(np.concatenate([qkv_ctx[D:2 * D], qkv_lat[D:2 * D]], axis=1))
    vT = np.concatenate([qkv_ctx[2 * D:], qkv_lat[2 * D:]], axis=1)
    v = np.ascontiguousarray(vT.reshape(8, 128, NKC_ALL, 128).transpose(0, 3, 2, 1))
    lam_b = np.ascontiguousarray(np.broadcast_to(lam_p[None], (128, 4, 64)))
    sub_b = np.ascontiguousarray(np.broadcast_to(subln[None], (128, 128)))
    maps = []
    for r in range(NCORES):
        parts = [qkv_lat[:D, r * 2048:(r + 1) * 2048]]
        if with_ctx_q:
            parts.append(qkv_ctx[:D, r * 32:(r + 1) * 32])
        maps.append({"qT": np.ascontiguousarray(np.concatenate(parts, axis=1)), "kT": kT, "v": v,
                     "lam_p": lam_b, "subln": sub_b})
    res = run_bass_kernel_spmd(nc, maps, core_ids=list(range(NCORES)))
    os_ = [res.results[r]["o"] for r in range(NCORES)]
    o_lat = np.concatenate([o[:2048] for o in os_], axis=0)
    o_ctx = np.concatenate([o[2048:] for o in os_], axis=0) if with_ctx_q else None
    return to_fm(o_lat), (to_fm(o_ctx) if with_ctx_q else None)


def build_gmlp(T_lat, T_ctx):
    nc = bass.Bass("TRN2", target_bir_lowering=False)
    T = T_lat + T_ctx
    Xd = nc.dram_tensor("xT", [D, T], F32, kind="ExternalInput").ap()
    Wd = nc.dram_tensor("w_in", [D, 2 * D], F32, kind="ExternalInput").ap()
    Md = nc.dram_tensor("mod", [128, 48, 2], F32, kind="ExternalInput").ap()
    Gd = nc.dram_tensor("g1", [128, DC], F32, kind="ExternalInput").ap()
    LGd = nc.dram_tensor("ln_g", [128, D], F32, kind="ExternalInput").ap()
    LBd = nc.dram_tensor("ln_b", [128, D], F32, kind="ExternalInput").ap()
    WSd = nc.dram_tensor("w_sT", [128, 8, 128], F32, kind="ExternalInput").ap()
    BSd = nc.dram_tensor("b_s", [128, 8, 128], F32, kind="ExternalInput").ap()
    Od = nc.dram_tensor("oT", [D, T], BF16, kind="ExternalOutput").ap()
    sc = Sched(nc)
    tl = TL(nc, sc)
    tiles = token_tiles(T_lat, T_ctx)
    mod_t, mod_b = tl.load_vec("mod_sb", Md, [128, 48, 2])
    g1_t, g1_b = tl.load_vec("g1_sb", Gd, [128, DC])
    lng_t, lng_b = tl.load_vec("lng_sb", LGd, [128, D])
    lnb_t, lnb_b = tl.load_vec("lnb_sb", LBd, [128, D])
    bs_t, bs_b = tl.load_vec("bs_sb", BSd, [128, 8, 128])
    geff, geff_b, shift = emit_geff(tl, "geff1", mod_t, mod_b, g1_t, g1_b, 1, 0)
    w_t, w_b = tl.load_w_bf16("w_sb", Wd.rearrange("(k p) n -> p k n", p=128), [128, DC, 2 * D])
    ws_t, ws_b = tl.load_w_bf16("ws_sb", WSd, [128, 8, 128])
    xt, xb = nc.alloc_sbuf_tensor("x_sb", [128, DC, 512], F32), sc.buf("x")
    hT, hTb = nc.alloc_sbuf_tensor("hT_sb", [128, DC, 512], BF16), sc.buf("hT")
    tmp, tmpb = nc.alloc_sbuf_tensor("tmp_sb", [128, DC, 512], F32), sc.buf("tmp")
    sq, sqb = nc.alloc_sbuf_tensor("sq_sb", [128, DC, 512], BF16), sc.buf("sq")
    rstd, rstdb = nc.alloc_sbuf_tensor("rstd_sb", [128, 512], F32), sc.buf("rstd")
    uT, uTb = nc.alloc_sbuf_tensor("uT_sb", [128, DC, 512], F32), sc.buf("uT")
    vs = [nc.alloc_sbuf_tensor(f"v_sb{i}", [128, D], F32) for i in range(2)]
    vsb = sc.bufs("vsb", 2)
    st = nc.alloc_sbuf_tensor("st_sb", [128, 8], F32)
    stb = sc.buf("st")
    vn, vnb = nc.alloc_sbuf_tensor("vn_sb", [128, 4, D], BF16), sc.buf("vn")
    ts_ = [nc.alloc_sbuf_tensor(f"ts_sb{i}", [128, 512], F32) for i in range(2)]
    tsb = sc.bufs("tsb", 2)
    ob = [nc.alloc_sbuf_tensor(f"ob_{i}", [128, 512], BF16) for i in range(3)]
    obb = sc.bufs("obb", 3)
    Xv = Xd.rearrange("(c p) t -> p c t", p=128)
    Ov = Od.rearrange("(c p) t -> p c t", p=128)
    ps = tl.ps
    for (t0, w, ty) in tiles:
        nch = w // 128
        sc.dma("sp", xt[:, :, :w], Xv[:, :, t0:t0 + w], writes=[xb])
        emit_norm_mod_multi(tl, xt[:, :, :w], [xb], w, geff, shift, geff_b, mod_b, ty, hT[:, :, :w], hTb,
                            tmp, tmpb, sq, sqb, rstd, rstdb)
        for mc in range(DC):
            ba = tl.rot("pa", 2)
            for k in range(DC):
                sc.op("pe", lambda: nc.tensor.matmul(ps[:, ba, :w], lhsT=w_t[:, k, mc * 128:(mc + 1) * 128], rhs=hT[:, k, :w],
                                                     start=(k == 0), stop=(k == DC - 1)), reads=[w_b, hTb], writes=[tl.pb[ba]])
            sc.op("act", lambda: nc.scalar.activation(out=uT[:, mc, :w], in_=ps[:, ba, :w], func=AF.Gelu),
                  reads=[tl.pb[ba]], writes=[uTb])
        for ci in range(nch):
            vi = tl.rot("v", 2)
            v_ = vs[vi]
            for half in range(2):
                bb_ = 2 + tl.rot("pb", 2)
                for k in range(DC):
                    sc.op("pe", lambda: nc.tensor.matmul(ps[:, bb_, :], lhsT=hT[:, k, ci * 128:(ci + 1) * 128],
                                                         rhs=w_t[:, k, D + half * 512:D + (half + 1) * 512],
                                                         start=(k == 0), stop=(k == DC - 1)), reads=[w_b, hTb], writes=[tl.pb[bb_]])
                sc.op("act", lambda: nc.scalar.activation(out=v_[:, half * 512:(half + 1) * 512], in_=ps[:, bb_, :], func=AF.Gelu),
                      reads=[tl.pb[bb_]], writes=[vsb[vi]])
            sc.op("dve", lambda: nc.vector.tensor_reduce(out=st[:, 0:1], in_=v_[:], axis=AX.X, op=ALU.add), reads=[vsb[vi]], writes=[stb])
            sc.op("dve", lambda: nc.vector.tensor_scalar(out=st[:, 1:2], in0=st[:, 0:1], scalar1=float(-1.0 / D), scalar2=None,
                                                         op0=ALU.mult), reads=[stb], writes=[stb])
            sc.op("dve", lambda: nc.vector.tensor_scalar(out=v_[:], in0=v_[:], scalar1=st[:, 1:2], scalar2=None, op0=ALU.add),
                  reads=[vsb[vi], stb], writes=[vsb[vi]])
            sc.op("act", lambda: nc.scalar.activation(out=tmp[:, 0:2, :].rearrange("p a b -> p (a b)"), in_=v_[:], func=AF.Square),
                  reads=[vsb[vi]], writes=[tmpb])
            sc.op("dve", lambda: nc.vector.tensor_reduce(out=st[:, 2:3], in_=tmp[:, 0:2, :].rearrange("p a b -> p (a b)"), axis=AX.X,
                                                         op=ALU.add), reads=[tmpb], writes=[stb])
            sc.op("act", lambda: nc.scalar.activation(out=st[:, 3:4], in_=st[:, 2:3], func=AF.Sqrt, bias=1e-5, scale=1.0 / D),
                  reads=[stb], writes=[stb])
            sc.op("dve", lambda: nc.vector.reciprocal(out=st[:, 4:5], in_=st[:, 3:4]), reads=[stb], writes=[stb])
            sc.op("dve", lambda: nc.vector.scalar_tensor_tensor(out=v_[:], in0=v_[:], scalar=st[:, 4:5], in1=lng_t[:],
                                                                op0=ALU.mult, op1=ALU.mult), reads=[vsb[vi], stb, lng_b], writes=[vsb[vi]])
            sc.op("pool", lambda: nc.gpsimd.tensor_tensor(out=vn[:, ci, :], in0=v_[:], in1=lnb_t[:], op=ALU.add),
                  reads=[vsb[vi], lnb_b], writes=[vnb])
        for g in range(8):
            bc_ = 4 + tl.rot("pc", 2)
            for ci in range(nch):
                sc.op("pe", lambda: nc.tensor.matmul(ps[:, bc_, ci * 128:(ci + 1) * 128], lhsT=vn[:, ci, g * 128:(g + 1) * 128],
                                                     rhs=ws_t[:, g, :], start=True, stop=True), reads=[vnb, ws_b], writes=[tl.pb[bc_]])
            ti = tl.rot("ts", 2)
            oi = tl.rot("ob", 3)
            sc.op("dve", lambda: nc.vector.tensor_tensor(out=ts_[ti][:, :w].rearrange("p (c q) -> p c q", q=128),
                                                         in0=ps[:, bc_, :w].rearrange("p (c q) -> p c q", q=128),
                                                         in1=bs_t[:, g, :].unsqueeze(1).to_broadcast([128, nch, 128]), op=ALU.add),
                  reads=[tl.pb[bc_], bs_b], writes=[tsb[ti]])
            sc.op("pool", lambda: nc.gpsimd.tensor_tensor(out=ob[oi][:, :w], in0=ts_[ti][:, :w], in1=uT[:, g, :w], op=ALU.mult),
                  reads=[tsb[ti], uTb], writes=[obb[oi]])
            sc.dma("sp", Ov[:, g, t0:t0 + w], ob[oi][:, :w], reads=[obb[oi]])
    sc.finish(obb)
    return nc


def run_gmlp(xl_fm, xc_fm, mod_l, g1, inp):
    nc = build_gmlp(2048, 128)
    w_sT = np.ascontiguousarray(inp["gmlp_w_s"][0].transpose(2, 0, 1))
    b_s = np.ascontiguousarray(np.broadcast_to(inp["gmlp_b_s"][0][None], (128, 8, 128)))
    maps = []
    for r in range(NCORES):
        xs = np.concatenate([xl_fm[:, r * 2048:(r + 1) * 2048], xc_fm[:, (r % 2) * 128:(r % 2 + 1) * 128]], axis=1)
        maps.append({"xT": np.ascontiguousarray(xs), "w_in": np.ascontiguousarray(inp["gmlp_w_in"][0]),
                     "mod": np.ascontiguousarray(mod_l), "g1": vec_pc(g1),
                     "ln_g": np.ascontiguousarray(np.broadcast_to(inp["gmlp_ln_g"][0][None], (128, D))),
                     "ln_b": np.ascontiguousarray(np.broadcast_to(inp["gmlp_ln_b"][0][None], (128, D))),
                     "w_sT": w_sT, "b_s": b_s})
    res = run_bass_kernel_spmd(nc, maps, core_ids=list(range(NCORES)))
    os_ = [res.results[r]["oT"] for r in range(NCORES)]
    o_lat = np.concatenate([o[:, :2048] for o in os_], axis=1)
    o_ctx = np.concatenate([os_[0][:, 2048:], os_[1][:, 2048:]], axis=1)
    return o_lat, o_ctx


RW_TW = 256


def build_rwkv_pre():
    nc = bass.Bass("TRN2", target_bir_lowering=False)
    TX = 2050 + 34
    TO = 2048 + 32
    Xd = nc.dram_tensor("xT", [D, TX], F32, kind="ExternalInput").ap()
    Md = nc.dram_tensor("mod", [128, 48, 2], F32, kind="ExternalInput").ap()
    Gd = nc.dram_tensor("g1n", [128, DC], F32, kind="ExternalInput").ap()
    Ed = nc.dram_tensor("edge", [128, 4], F32, kind="ExternalInput").ap()
    MXd = nc.dram_tensor("mix", [128, 6, DC], F32, kind="ExternalInput").ap()
    Wd = nc.dram_tensor("w_rkv", [3, D, D], F32, kind="ExternalInput").ap()
    W0d = nc.dram_tensor("w0", [128, 2, DC], F32, kind="ExternalInput").ap()
    W1d = nc.dram_tensor("w1", [2, D, 64], F32, kind="ExternalInput").ap()
    W2d = nc.dram_tensor("w2", [2, 64, D], F32, kind="ExternalInput").ap()
    A0d = nc.dram_tensor("a0", [128, 2, DC], F32, kind="ExternalInput").ap()
    A1d = nc.dram_tensor("a1", [2, D, 64], F32, kind="ExternalInput").ap()
    A2d = nc.dram_tensor("a2", [2, 64, D], F32, kind="ExternalInput").ap()
    G1d = nc.dram_tensor("gw1", [D, 160], F32, kind="ExternalInput").ap()
    G2d = nc.dram_tensor("gw2", [160, D], F32, kind="ExternalInput").ap()
    KKd = nc.dram_tensor("k_k", [128, DC], F32, kind="ExternalInput").ap()
    KAd = nc.dram_tensor("k_a", [128, DC], F32, kind="ExternalInput").ap()
    BOd = nc.dram_tensor("blk", [128, 128], F32, kind="ExternalInput").ap()
    Od = nc.dram_tensor("out", [10, D, TO], F32, kind="ExternalOutput").ap()
    sc = Sched(nc)
    tl = TL(nc, sc)
    ps = tl.ps
    mod_t, mod_b = tl.load_vec("mod_sb", Md, [128, 48, 2])
    g1_t, g1_b = tl.load_vec("g1_sb", Gd, [128, DC])
    ed_t, ed_b = tl.load_vec("ed_sb", Ed, [128, 4])
    mx_t, mx_b = tl.load_vec("mx_sb", MXd, [128, 6, DC])
    w0_t, w0_b = tl.load_vec("w0_sb", W0d, [128, 2, DC])
    a0_t, a0_b = tl.load_vec("a0_sb", A0d, [128, 2, DC])
    kk_t, kk_b = tl.load_vec("kk_sb", KKd, [128, DC])
    ka_t, ka_b = tl.load_vec("ka_sb", KAd, [128, DC])
    blk_t, blk_b = tl.load_vec("blk_sb", BOd, [128, 128])
    omk_t = nc.alloc_sbuf_tensor("omk_sb", [128, DC], F32)
    omk_b = sc.buf("omk")
    sc.op("dve", lambda: nc.vector.tensor_scalar(out=omk_t[:], in0=ka_t[:], scalar1=-1.0, scalar2=1.0, op0=ALU.mult, op1=ALU.add),
          reads=[ka_b], writes=[omk_b])
    geff, geff_b, shift = emit_geff(tl, "geff1", mod_t, mod_b, g1_t, g1_b, 1, 0)
    wr = []
    for i in range(3):
        wr.append(tl.load_w_bf16(f"wrkv{i}", Wd[i].rearrange("(k p) n -> p k n", p=128), [128, DC, D]))
    w1_t, w1_b = tl.load_w_bf16("w1_sb", W1d.rearrange("z (k p) l -> p z k l", p=128), [128, 2, DC, 64])
    a1_t, a1_b = tl.load_w_bf16("a1_sb", A1d.rearrange("z (k p) l -> p z k l", p=128), [128, 2, DC, 64])
    w2_t, w2_b = tl.load_w_bf16("w2_sb", W2d.rearrange("z l n -> l z n"), [64, 2, D])
    a2_t, a2_b = tl.load_w_bf16("a2_sb", A2d.rearrange("z l n -> l z n"), [64, 2, D])
    gw1_t, gw1_b = tl.load_w_bf16("gw1_sb", G1d.rearrange("(k p) l -> p k l", p=128), [128, DC, 160])
    gw2_t = nc.alloc_sbuf_tensor("gw2_sb", [128, 2, D], BF16)
    gw2_b = sc.buf("gw2")
    sc.dma("pool", gw2_t[:, 0, :], G2d[0:128, :], writes=[gw2_b])
    sc.dma("pool", gw2_t[0:32, 1, :], G2d[128:160, :], writes=[gw2_b])
    TWX = RW_TW + 2
    xt, xb = nc.alloc_sbuf_tensor("x_sb", [128, DC, TWX], F32), sc.buf("x")
    hx, hxb = nc.alloc_sbuf_tensor("hx_sb", [128, DC, TWX], F32), sc.buf("hx")
    tmp, tmpb = nc.alloc_sbuf_tensor("tmp_sb", [128, DC, TWX], F32), sc.buf("tmp")
    sq, sqb = nc.alloc_sbuf_tensor("sq_sb", [128, DC, TWX], BF16), sc.buf("sq")
    rstd, rstdb = nc.alloc_sbuf_tensor("rstd_sb", [128, TWX], F32), sc.buf("rstd")
    ts_, tsb = nc.alloc_sbuf_tensor("ts_sb", [128, DC, RW_TW], F32), sc.buf("ts")
    xm = [nc.alloc_sbuf_tensor(f"xm{j}", [128, DC, RW_TW], BF16) for j in range(6)]
    xmb = sc.bufs("xmb", 6)
    NS = 16
    stg = [nc.alloc_sbuf_tensor(f"stg{i}", [128, RW_TW], F32) for i in range(NS)]
    stgb = sc.bufs("stgb", NS)
    lt = [nc.alloc_sbuf_tensor(f"lt{i}", [128, 2, RW_TW], BF16) for i in range(2)]
    ltb = sc.bufs("ltb", 2)
    gsb, gsbb = nc.alloc_sbuf_tensor("gsb", [128, 2, RW_TW], BF16), sc.buf("gsbb")
    Xv = Xd.rearrange("(c p) t -> p c t", p=128)

    def stage():
        i = tl.rot("stg", NS)
        return stg[i], stgb[i]

    def out_dma(qi, c, o0, w, t_, b_):
        sc.dma("sp", Od[qi, c * 128:(c + 1) * 128, o0:o0 + w], t_[:, :w], reads=[b_])

    def proj(wt, wb_, xj, mc, w, bank):
        for k in range(DC):
            sc.op("pe", lambda: nc.tensor.matmul(ps[:, bank, :w], lhsT=wt[:, k, mc * 128:(mc + 1) * 128], rhs=xm[xj][:, k, :w],
                                                 start=(k == 0), stop=(k == DC - 1)), reads=[wb_, xmb[xj]], writes=[tl.pb[bank]])

    tiles = [(k * RW_TW, k * RW_TW, RW_TW, 0) for k in range(2048 // RW_TW)] + [(2050, 2048, 32, 1)]
    for ti, (x0, o0, w, ty) in enumerate(tiles):
        wx = w + 2
        sc.dma("sp", xt[:, :, :wx], Xv[:, :, x0:x0 + wx], writes=[xb])
        emit_norm_mod_multi(tl, xt[:, :, :wx], [xb], wx, geff, shift, geff_b, mod_b, ty, hx[:, :, :wx], hxb,
                            tmp, tmpb, sq, sqb, rstd, rstdb)
        edges = []
        if ty == 1:
            edges = [(0, 2), (wx - 1, 3)]
        elif ti == 0:
            edges = [(0, 0)]
        elif ti == 2048 // RW_TW - 1:
            edges = [(wx - 1, 1)]
        for (colx, ei) in edges:
            sc.op("dve", lambda: nc.vector.tensor_scalar(out=hx[:, :, colx:colx + 1], in0=hx[:, :, colx:colx + 1],
                                                         scalar1=ed_t[:, ei:ei + 1], scalar2=None, op0=ALU.mult),
                  reads=[hxb, ed_b], writes=[hxb])
        hc = hx[:, :, 1:1 + w]
        sc.op("dve", lambda: nc.vector.tensor_tensor(out=ts_[:, :, :w], in0=hx[:, :, 0:w], in1=hx[:, :, 2:2 + w], op=ALU.add),
              reads=[hxb], writes=[tsb])
        sc.op("dve", lambda: nc.vector.scalar_tensor_tensor(out=ts_[:, :, :w], in0=ts_[:, :, :w], scalar=0.5, in1=hc,
                                                            op0=ALU.mult, op1=ALU.subtract), reads=[tsb, hxb], writes=[tsb])
        for j in range(6):
            for c in range(DC):
                eng = "dve"
                e_ = nc.vector
                sc.op(eng, lambda: e_.scalar_tensor_tensor(out=xm[j][:, c, :w], in0=ts_[:, c, :w], scalar=mx_t[:, j, c:c + 1],
                                                           in1=hx[:, c, 1:1 + w], op0=ALU.mult, op1=ALU.add),
                      reads=[tsb, hxb, mx_b], writes=[xmb[j]])
        for z in range(2):
            bk = tl.rot("pl", 2)
            for k in range(DC):
                sc.op("pe", lambda: nc.tensor.matmul(ps[:64, bk, :w], lhsT=w1_t[:, z, k, :], rhs=xm[1][:, k, :w],
                                                     start=(k == 0), stop=(k == DC - 1)), reads=[w1_b, xmb[1]], writes=[tl.pb[bk]])
            sc.op("act", lambda: nc.scalar.activation(out=lt[0][:64, z, :w], in_=ps[:64, bk, :w], func=AF.Tanh),
                  reads=[tl.pb[bk]], writes=[ltb[0]])
            bk = tl.rot("pl", 2)
            for k in range(DC):
                sc.op("pe", lambda: nc.tensor.matmul(ps[:64, bk, :w], lhsT=a1_t[:, z, k, :], rhs=xm[4][:, k, :w],
                                                     start=(k == 0), stop=(k == DC - 1)), reads=[a1_b, xmb[4]], writes=[tl.pb[bk]])
            sc.op("act", lambda: nc.scalar.copy(out=lt[1][:64, z, :w], in_=ps[:64, bk, :w]), reads=[tl.pb[bk]], writes=[ltb[1]])
        for (m0, mw, gi) in ((0, 128, 0), (128, 32, 1)):
            bk = tl.rot("pl", 2)
            for k in range(DC):
                sc.op("pe", lambda: nc.tensor.matmul(ps[:mw, bk, :w], lhsT=gw1_t[:, k, m0:m0 + mw], rhs=xm[5][:, k, :w],
                                                     start=(k == 0), stop=(k == DC - 1)), reads=[gw1_b, xmb[5]], writes=[tl.pb[bk]])
            sc.op("act", lambda: nc.scalar.activation(out=gsb[:mw, gi, :w], in_=ps[:mw, bk, :w], func=AF.Sigmoid),
                  reads=[tl.pb[bk]], writes=[gsbb])
        for c in range(DC):
            bk = 2 + tl.rot("pm", 4)
            proj(wr[0][0], wr[0][1], 0, c, w, bk)
            s_, sb_ = stage()
            sc.op("act", lambda: nc.scalar.copy(out=s_[:, :w], in_=ps[:, bk, :w]), reads=[tl.pb[bk]], writes=[sb_])
            out_dma(0, c, o0, w, s_, sb_)
            bk = 2 + tl.rot("pm", 4)
            proj(wr[2][0], wr[2][1], 3, c, w, bk)
            s_, sb_ = stage()
            sc.op("act", lambda: nc.scalar.copy(out=s_[:, :w], in_=ps[:, bk, :w]), reads=[tl.pb[bk]], writes=[sb_])
            out_dma(1, c, o0, w, s_, sb_)
            bk = 2 + tl.rot("pm", 4)
            sc.op("pe", lambda: nc.tensor.matmul(ps[:, bk, :w], lhsT=gw2_t[:, 0, c * 128:(c + 1) * 128], rhs=gsb[:, 0, :w],
                                                 start=True, stop=False), reads=[gw2_b, gsbb], writes=[tl.pb[bk]])
            sc.op("pe", lambda: nc.tensor.matmul(ps[:, bk, :w], lhsT=gw2_t[0:32, 1, c * 128:(c + 1) * 128], rhs=gsb[0:32, 1, :w],
                                                 start=False, stop=True), reads=[gw2_b, gsbb], writes=[tl.pb[bk]])
            s_, sb_ = stage()
            sc.op("act", lambda: nc.scalar.copy(out=s_[:, :w], in_=ps[:, bk, :w]), reads=[tl.pb[bk]], writes=[sb_])
            out_dma(9, c, o0, w, s_, sb_)
            bk = 2 + tl.rot("pm", 4)
            proj(wr[1][0], wr[1][1], 2, c, w, bk)
            k_s, k_b = stage()
            sc.op("act", lambda: nc.scalar.copy(out=k_s[:, :w], in_=ps[:, bk, :w]), reads=[tl.pb[bk]], writes=[k_b])
            kk_s, kk_sb = stage()
            sc.op("dve", lambda: nc.vector.tensor_scalar(out=kk_s[:, :w], in0=k_s[:, :w], scalar1=kk_t[:, c:c + 1], scalar2=None,
                                                         op0=ALU.mult), reads=[k_b, kk_b], writes=[kk_sb])
            q_s, q_b = stage()
            sc.op("pool", lambda: nc.gpsimd.tensor_tensor(out=q_s[:, :w], in0=kk_s[:, :w], in1=kk_s[:, :w], op=ALU.mult),
                  reads=[kk_sb], writes=[q_b])
            bk2 = tl.rot("pl", 2)
            sc.op("pe", lambda: nc.tensor.matmul(ps[:, bk2, :w], lhsT=blk_t[:], rhs=q_s[:, :w], start=True, stop=True),
                  reads=[blk_b, q_b], writes=[tl.pb[bk2]])
            n_s, n_b = stage()
            sc.op("act", lambda: nc.scalar.activation(out=n_s[:, :w], in_=ps[:, bk2, :w], func=AF.Sqrt), reads=[tl.pb[bk2]], writes=[n_b])
            sc.op("dve", lambda: nc.vector.tensor_scalar(out=n_s[:, :w], in0=n_s[:, :w], scalar1=1e-12, scalar2=None, op0=ALU.max),
                  reads=[n_b], writes=[n_b])
            sc.op("dve", lambda: nc.vector.reciprocal(out=n_s[:, :w], in_=n_s[:, :w]), reads=[n_b], writes=[n_b])
            kap_s, kap_b = stage()
            sc.op("dve", lambda: nc.vector.tensor_tensor(out=kap_s[:, :w], in0=kk_s[:, :w], in1=n_s[:, :w], op=ALU.mult),
                  reads=[kk_sb, n_b], writes=[kap_b])
            out_dma(2, c, o0, w, kap_s, kap_b)
            for z in range(2):
                bk = 2 + tl.rot("pm", 4)
                sc.op("pe", lambda: nc.tensor.matmul(ps[:, bk, :w], lhsT=a2_t[:, z, c * 128:(c + 1) * 128], rhs=lt[1][:64, z, :w],
                                                     start=True, stop=True), reads=[a2_b, ltb[1]], writes=[tl.pb[bk]])
                a_s, a_b = stage()
                sc.op("act", lambda: nc.scalar.activation(out=a_s[:, :w], in_=ps[:, bk, :w], func=AF.Sigmoid,
                                                          bias=a0_t[:, z, c:c + 1], scale=1.0), reads=[tl.pb[bk], a0_b], writes=[a_b])
                t_s, t_b = stage()
                sc.op("dve", lambda: nc.vector.tensor_scalar(out=t_s[:, :w], in0=a_s[:, :w], scalar1=ka_t[:, c:c + 1],
                                                             scalar2=omk_t[:, c:c + 1], op0=ALU.mult, op1=ALU.add),
                      reads=[a_b, ka_b, omk_b], writes=[t_b])
                sc.op("pool", lambda: nc.gpsimd.tensor_tensor(out=t_s[:, :w], in0=t_s[:, :w], in1=k_s[:, :w], op=ALU.mult),
                      reads=[t_b, k_b], writes=[t_b])
                out_dma(3 + z, c, o0, w, t_s, t_b)
                sc.op("pool", lambda: nc.gpsimd.tensor_tensor(out=a_s[:, :w], in0=a_s[:, :w], in1=kap_s[:, :w], op=ALU.mult),
                      reads=[a_b, kap_b], writes=[a_b])
                out_dma(7 + z, c, o0, w, a_s, a_b)
                bk = 2 + tl.rot("pm", 4)
                sc.op("pe", lambda: nc.tensor.matmul(ps[:, bk, :w], lhsT=w2_t[:, z, c * 128:(c + 1) * 128], rhs=lt[0][:64, z, :w],
                                                     start=True, stop=True), reads=[w2_b, ltb[0]], writes=[tl.pb[bk]])
                l_s, l_b = stage()
                sc.op("act", lambda: nc.scalar.activation(out=l_s[:, :w], in_=ps[:, bk, :w], func=AF.Sigmoid,
                                                          bias=w0_t[:, z, c:c + 1], scale=1.0), reads=[tl.pb[bk], w0_b], writes=[l_b])
                sc.op("dve", lambda: nc.vector.tensor_scalar(out=l_s[:, :w], in0=l_s[:, :w], scalar1=float(-math.exp(-0.5)),
                                                             scalar2=None, op0=ALU.mult), reads=[l_b], writes=[l_b])
                out_dma(5 + z, c, o0, w, l_s, l_b)
    sc.finish(stgb)
    return nc


def blockones():
    b = np.zeros((128, 128), np.float32)
    b[:64, :64] = 1.0
    b[64:, 64:] = 1.0
    return b


def vec_pzc(v):
    return np.ascontiguousarray(v.reshape(v.shape[0], DC, 128).transpose(2, 0, 1))


def run_rwkv_pre(xl_fm, xc_fm, mod_l, g1, inp):
    nc = build_rwkv_pre()
    zl = np.zeros((D, 1), np.float32)
    maps = []
    common = {"mod": np.ascontiguousarray(mod_l), "g1n": vec_pc(g1), "mix": vec_pzc(inp["rwkv_mix"][0]),
              "w_rkv": np.ascontiguousarray(inp["rwkv_w_rkv"][0]), "w0": vec_pzc(inp["rwkv_w0"][0]),
              "w1": np.ascontiguousarray(inp["rwkv_w1"][0]), "w2": np.ascontiguousarray(inp["rwkv_w2"][0]),
              "a0": vec_pzc(inp["rwkv_a0"][0]), "a1": np.ascontiguousarray(inp["rwkv_a1"][0]),
              "a2": np.ascontiguousarray(inp["rwkv_a2"][0]), "gw1": np.ascontiguousarray(inp["rwkv_g1"][0]),
              "gw2": np.ascontiguousarray(inp["rwkv_g2"][0]), "k_k": vec_pc(inp["rwkv_k_k"][0]),
              "k_a": vec_pc(inp["rwkv_k_a"][0]), "blk": blockones()}
    xlp = np.concatenate([zl, xl_fm, zl], axis=1)
    xcp = np.concatenate([zl, xc_fm, zl], axis=1)
    for r in range(NCORES):
        xs = np.concatenate([xlp[:, r * 2048:r * 2048 + 2050], xcp[:, r * 32:r * 32 + 34]], axis=1)
        edge = np.ones((128, 4), np.float32)
        if r == 0:
            edge[:, 0] = 0.0
            edge[:, 2] = 0.0
        if r == NCORES - 1:
            edge[:, 1] = 0.0
            edge[:, 3] = 0.0
        m = dict(common)
        m["xT"] = np.ascontiguousarray(xs)
        m["edge"] = edge
        maps.append(m)
    res = run_bass_kernel_spmd(nc, maps, core_ids=list(range(NCORES)))
    outs = [res.results[r]["out"] for r in range(NCORES)]
    lat = np.concatenate([o[:, :, :2048] for o in outs], axis=2)
    ctx = np.concatenate([o[:, :, 2048:] for o in outs], axis=2)
    return lat, ctx


RW_N = NCTX + SEQ
RW_NCH = RW_N // 64
SEG = 8


def build_rwkv_scan(n_pairs=4, n_chunks=RW_NCH):
    import itertools
    nc = bass.Bass("TRN2", target_bir_lowering=False)
    N = n_chunks * 64
    Fd = nc.dram_tensor("F", [n_pairs, 4, 128, N], F32, kind="ExternalInput").ap()
    Td = nc.dram_tensor("Tm", [n_pairs, 4, 128, n_chunks, 64], F32, kind="ExternalInput").ap()
    Cd = nc.dram_tensor("C", [128, 4, 64], F32, kind="ExternalInput").ap()
    Yd = nc.dram_tensor("Y", [n_pairs, 2, N, 64], F32, kind="ExternalOutput").ap()
    sc = Sched(nc)
    ps = nc.alloc_psum_tensor("ps", [128, 8, 512], F32)
    pb = sc.bufs("psb", 8)
    zb, ub, hb_ = sc.buf("zps"), sc.buf("ups"), sc.buf("hps")
    ct = nc.alloc_sbuf_tensor("ct", [128, 4, 64], F32)
    ctb = nc.alloc_sbuf_tensor("ctb", [128, 4, 64], BF16)
    cb = sc.buf("c")
    sc.dma("sp", ct[:], Cd, writes=[cb])
    sc.op("dve", lambda: nc.vector.tensor_copy(out=ctb[:], in_=ct[:]), reads=[cb], writes=[cb])
    TRI, TRS, TRST, IDN = ct[:, 0, :], ct[:, 1, :], ct[:, 2, :], ct[:, 3, :]
    TRIb, TRSb, IDNb = ctb[:, 0, :], ctb[:, 1, :], ctb[:, 3, :]

    def T_(name, shape, dt=F32):
        return nc.alloc_sbuf_tensor(name, shape, dt)

    S = []
    for i in range(2):
        d = dict(
            fm=T_(f"fm{i}", [128, 4, SEG * 64]), fmb=sc.buf(f"fm{i}"),
            tm=T_(f"tm{i}", [128, 4, SEG, 64]), tmb=sc.buf(f"tm{i}"),
            ein=T_(f"ein{i}", [128, SEG * 64]), einb=sc.buf(f"ein{i}"),
            eex=T_(f"eex{i}", [128, SEG * 64]), eexb=sc.buf(f"eex{i}"),
            eng=T_(f"eng{i}", [128, SEG * 64]), engb=sc.buf(f"eng{i}"),
            ent=T_(f"ent{i}", [128, SEG * 64]), entb=sc.buf(f"ent{i}"),
            ar=T_(f"ar{i}", [128, SEG, 128], BF16), arb=sc.buf(f"ar{i}"),
            bt=T_(f"bt{i}", [128, SEG * 64], BF16), btb=sc.buf(f"bt{i}"),
            kt=T_(f"kt{i}", [128, SEG * 64], BF16), ktb=sc.buf(f"kt{i}"),
            btm=T_(f"btm{i}", [128, SEG, 64], BF16), btmb=sc.buf(f"btm{i}"),
            ktm=T_(f"ktm{i}", [128, SEG, 64], BF16), ktmb=sc.buf(f"ktm{i}"),
            x=[T_(f"x{i}_{q}", [128, SEG, 64], BF16) for q in range(2)], xb=sc.bufs(f"x{i}_", 2),
            xt=[T_(f"xt{i}_{q}", [128, SEG, 64], BF16) for q in range(2)], xtb=sc.bufs(f"xt{i}_", 2),
            tt=T_(f"tt{i}", [128, SEG, 64], BF16), ttb=sc.buf(f"tt{i}"),
            qbt=T_(f"qbt{i}", [128, SEG, 64], BF16), qbtb=sc.buf(f"qbt{i}"),
            aak=T_(f"aak{i}", [128, SEG, 64], BF16), aakb=sc.buf(f"aak{i}"),
            qkt=T_(f"qkt{i}", [128, SEG, 64], BF16), qktb=sc.buf(f"qkt{i}"),
            ysb=T_(f"ysb{i}", [128, SEG * 64]), ysbb=sc.buf(f"ysb{i}"),
            lwb=T_(f"lwb{i}", [128, SEG, 64], BF16), lwbb=sc.buf(f"lwb{i}"),
            vb=T_(f"vb{i}", [128, SEG, 64], BF16), vbb=sc.buf(f"vb{i}"),
        )
        S.append(d)
    Hs = [T_(f"H{i}", [128, 64], BF16) for i in range(3)]
    Hb = sc.bufs("H", 3)
    Zs = [T_(f"Z{i}", [128, 64], BF16) for i in range(2)]
    Zb = sc.bufs("Z", 2)
    Us = [T_(f"U{i}", [128, 64], BF16) for i in range(2)]
    Ub = sc.bufs("U", 2)
    cnt = {}

    def rot(key, n):
        v = cnt.get(key, 0)
        cnt[key] = v + 1
        return v % n

    import os
    PRELOAD = False
    REP = 2
    ISL = int(os.environ.get("SCAN_ISL", "48"))

    def mm(out, lhsT, rhs, start, stop, reads, writes, pre_=False):
        for l in range(2):
            sl = slice(l * 64, (l + 1) * 64)
            sc.op("pe", lambda: nc.tensor.matmul(out[sl], lhsT=lhsT[sl], rhs=rhs[sl], start=start, stop=stop),
                  reads=reads, writes=writes)

    def bc(ap, n):
        return ap.unsqueeze(1).to_broadcast([128, n, 64])

    def pre(pair, g, d):
        c0 = g * SEG
        n = min(SEG, n_chunks - c0)
        w = n * 64
        sc.dma("sp", d["fm"][:, :, :w], Fd[pair, :, :, c0 * 64:c0 * 64 + w].rearrange("q j t -> j q t"), writes=[d["fmb"]])
        sc.dma("sp", d["tm"][:, :, :n, :], Td[pair, :, :, c0:c0 + n, :].rearrange("q s c j -> s q c j"), writes=[d["tmb"]])
        yield
        fm, tm = d["fm"], d["tm"]
        sc.op("act", lambda: nc.scalar.copy(out=d["lwb"][:, :n, :], in_=tm[:, 0, :n, :]), reads=[d["tmb"]], writes=[d["lwbb"]])
        sc.op("act", lambda: nc.scalar.copy(out=d["vb"][:, :n, :], in_=tm[:, 3, :n, :]), reads=[d["tmb"]], writes=[d["vbb"]])
        yield
        for c in range(n):
            mm(ps[:, 0, c * 64:(c + 1) * 64], d["lwb"][:, c, :], TRIb, True, True, [d["lwbb"], cb], [pb[0]])
            mm(ps[:, 1, c * 64:(c + 1) * 64], d["lwb"][:, c, :], TRSb, True, True, [d["lwbb"], cb], [pb[1]])
            yield
        mm(ps[:, 2, :w], TRIb, d["lwb"][:, :n, :].rearrange("p c j -> p (c j)"), True, True, [d["lwbb"], cb], [pb[2]])
        sc.op("act", lambda: nc.scalar.activation(out=d["ein"][:, :w], in_=ps[:, 0, :w], func=AF.Exp), reads=[pb[0]], writes=[d["einb"]])
        sc.op("act", lambda: nc.scalar.activation(out=d["eng"][:, :w], in_=ps[:, 0, :w], func=AF.Exp, scale=-1.0),
              reads=[pb[0]], writes=[d["engb"]])
        yield
        sc.op("act", lambda: nc.scalar.activation(out=d["eex"][:, :w], in_=ps[:, 1, :w], func=AF.Exp), reads=[pb[1]], writes=[d["eexb"]])
        sc.op("act", lambda: nc.scalar.activation(out=d["ent"][:, :w], in_=ps[:, 2, :w], func=AF.Exp, scale=-1.0),
              reads=[pb[2]], writes=[d["entb"]])
        yield
        v3 = lambda t_: t_[:, :w].rearrange("p (c t) -> p c t", t=64)
        sc.op("dve", lambda: nc.vector.scalar_tensor_tensor(out=d["ar"][:, :n, 0:64], in0=v3(fm[:, 2, :]), scalar=-1.0, in1=v3(d["eex"]),
                                                            op0=ALU.mult, op1=ALU.mult), reads=[d["fmb"], d["eexb"]], writes=[d["arb"]])
        sc.op("dve", lambda: nc.vector.tensor_tensor(out=d["ar"][:, :n, 64:128], in0=v3(fm[:, 0, :]), in1=v3(d["ein"]), op=ALU.mult),
              reads=[d["fmb"], d["einb"]], writes=[d["arb"]])
        yield
        sc.op("pool", lambda: nc.gpsimd.tensor_tensor(out=d["bt"][:, :w], in0=fm[:, 3, :w], in1=d["eng"][:, :w], op=ALU.mult),
              reads=[d["fmb"], d["engb"]], writes=[d["btb"]])
        sc.op("pool", lambda: nc.gpsimd.tensor_tensor(out=d["kt"][:, :w], in0=fm[:, 1, :w], in1=d["eng"][:, :w], op=ALU.mult),
              reads=[d["fmb"], d["engb"]], writes=[d["ktb"]])
        yield
        sc.op("pool", lambda: nc.gpsimd.tensor_tensor(out=d["btm"][:, :n, :], in0=tm[:, 2, :n, :], in1=v3(d["ent"]).rearrange("p c t -> p c t"),
                                                      op=ALU.mult), reads=[d["tmb"], d["entb"]], writes=[d["btmb"]])
        sc.op("pool", lambda: nc.gpsimd.tensor_tensor(out=d["ktm"][:, :n, :], in0=tm[:, 1, :n, :], in1=v3(d["ent"]), op=ALU.mult),
              reads=[d["tmb"], d["entb"]], writes=[d["ktmb"]])
        yield
        for c in range(n):
            bk, off = c // 4, (c % 4) * 128
            mm(ps[:, 0 + bk, off:off + 128], d["bt"][:, c * 64:(c + 1) * 64], d["ar"][:, c, :], True, True, [d["btb"], d["arb"]], [pb[0 + bk]])
            mm(ps[:, 2 + bk, off:off + 128], d["kt"][:, c * 64:(c + 1) * 64], d["ar"][:, c, :], True, True, [d["ktb"], d["arb"]], [pb[2 + bk]])
            mm(ps[:, 4, c * 64:(c + 1) * 64], d["ar"][:, c, 0:64], d["bt"][:, c * 64:(c + 1) * 64], True, True, [d["btb"], d["arb"]], [pb[4]])
            yield
        nb = (n + 3) // 4
        for bk in range(nb):
            cc = min(4, n - bk * 4)
            o1 = ps[:, 0 + bk, :cc * 128].rearrange("p (c x) -> p c x", x=128)
            o2 = ps[:, 2 + bk, :cc * 128].rearrange("p (c x) -> p c x", x=128)
            sl = slice(bk * 4, bk * 4 + cc)
            sc.op("dve", lambda: nc.vector.tensor_tensor(out=d["x"][0][:, sl, :], in0=o1[:, :, 0:64], in1=bc(TRS, cc), op=ALU.mult),
                  reads=[pb[0 + bk], cb], writes=[d["xb"][0]])
            sc.op("dve", lambda: nc.vector.tensor_tensor(out=d["qbt"][:, sl, :], in0=o1[:, :, 64:128], in1=bc(TRI, cc), op=ALU.mult),
                  reads=[pb[0 + bk], cb], writes=[d["qbtb"]])
            yield
            sc.op("dve", lambda: nc.vector.tensor_tensor(out=d["aak"][:, sl, :], in0=o2[:, :, 0:64], in1=bc(TRS, cc), op=ALU.mult),
                  reads=[pb[2 + bk], cb], writes=[d["aakb"]])
            sc.op("dve", lambda: nc.vector.tensor_tensor(out=d["qkt"][:, sl, :], in0=o2[:, :, 64:128], in1=bc(TRI, cc), op=ALU.mult),
                  reads=[pb[2 + bk], cb], writes=[d["qktb"]])
            yield
        p4 = ps[:, 4, :w].rearrange("p (c t) -> p c t", t=64)
        sc.op("dve", lambda: nc.vector.tensor_tensor(out=d["xt"][0][:, :n, :], in0=p4, in1=bc(TRST, n), op=ALU.mult),
              reads=[pb[4], cb], writes=[d["xtb"][0]])
        sc.op("pool", lambda: nc.gpsimd.tensor_tensor(out=d["tt"][:, :n, :], in0=d["x"][0][:, :n, :], in1=bc(IDN, n), op=ALU.add),
              reads=[d["xb"][0], cb], writes=[d["ttb"]])
        yield
        cur = 0
        for lvl in range(1, 6):
            nxt = 1 - cur
            X, XT = d["x"][cur], d["xt"][cur]
            if lvl < 5:
                for c in range(n):
                    mm(ps[:, 0, c * 64:(c + 1) * 64], XT[:, c, :], X[:, c, :], True, True, [d["xb"][cur], d["xtb"][cur]], [pb[0]])
                    yield
            for c in range(n):
                mm(ps[:, 1, c * 64:(c + 1) * 64], X[:, c, :], XT[:, c, :], True, True, [d["xb"][cur], d["xtb"][cur]], [pb[1]])
                yield
            if lvl < 5:
                sc.op("act", lambda: nc.scalar.copy(out=d["x"][nxt][:, :n, :], in_=ps[:, 0, :w].rearrange("p (c t) -> p c t", t=64)),
                      reads=[pb[0]], writes=[d["xb"][nxt]])
            sc.op("dve", lambda: nc.vector.tensor_copy(out=d["xt"][nxt][:, :n, :], in_=ps[:, 1, :w].rearrange("p (c t) -> p c t", t=64)),
                  reads=[pb[1]], writes=[d["xtb"][nxt]])
            yield
            for c in range(n):
                mm(ps[:, 2, c * 64:(c + 1) * 64], d["xt"][nxt][:, c, :], d["tt"][:, c, :], True, True, [d["xtb"][nxt], d["ttb"]], [pb[2]])
                yield
            sc.op("dve", lambda: nc.vector.tensor_tensor(out=d["tt"][:, :n, :], in0=ps[:, 2, :w].rearrange("p (c t) -> p c t", t=64),
                                                         in1=d["tt"][:, :n, :], op=ALU.add), reads=[pb[2], d["ttb"]], writes=[d["ttb"]])
            yield
            cur = nxt

    n_seg = (n_chunks + SEG - 1) // SEG
    work = [(p_, g) for p_ in range(n_pairs) for g in range(n_seg)]
    hc = 0
    import os
    DBG = os.environ.get("SCAN_DBG", "")
    gen = pre(work[0][0], work[0][1], S[0])
    for _ in gen:
        pass
    if "b" in DBG:
        sc.barrier()
    for wi, (pair, g) in enumerate(work):
        d = S[wi % 2]
        gen = pre(work[wi + 1][0], work[wi + 1][1], S[(wi + 1) % 2]) if wi + 1 < len(work) else iter(())
        c0 = g * SEG
        n = min(SEG, n_chunks - c0)
        w = n * 64
        if g == 0:
            hc = rot("H", 3)
            sc.op("pool", lambda: nc.gpsimd.memset(Hs[hc][:], 0.0), writes=[Hb[hc]])
        yb = 6
        for c in range(n):
            vtm = d["vb"][:, c, :]
            zi, ui = rot("Z", 2), rot("U", 2)
            hn = rot("H", 3)
            mm(ps[:, 7, 0:64], d["ar"][:, c, 0:64], Hs[hc][:], True, False, [d["arb"], Hb[hc]], [zb])
            mm(ps[:, 7, 0:64], d["aak"][:, c, :], vtm, False, True, [d["aakb"], d["vbb"]], [zb])
            for _rep in range(REP):
                sc.op("act", lambda: nc.scalar.copy(out=Zs[zi][:], in_=ps[:, 7, 0:64]), reads=[zb], writes=[Zb[zi]])
            mm(ps[:, 7, 64:128], d["tt"][:, c, :], Zs[zi][:], True, True, [d["ttb"], Zb[zi]], [ub])
            for _rep in range(REP):
                sc.op("dve", lambda: nc.vector.tensor_copy(out=Us[ui][:], in_=ps[:, 7, 64:128]), reads=[ub], writes=[Ub[ui]])
            mm(ps[:, 7, 128:192], IDNb, Hs[hc][:], True, False, [cb, Hb[hc]], [hb_])
            mm(ps[:, 7, 128:192], d["btm"][:, c, :], Us[ui][:], False, False, [d["btmb"], Ub[ui]], [hb_])
            mm(ps[:, 7, 128:192], d["ktm"][:, c, :], vtm, False, True, [d["ktmb"], d["vbb"]], [hb_])
            for _rep in range(REP):
                sc.op("dve", lambda: nc.vector.tensor_scalar(out=Hs[hn][:], in0=ps[:, 7, 128:192],
                                                             scalar1=d["ein"][:, c * 64 + 63:c * 64 + 64], scalar2=None, op0=ALU.mult),
                      reads=[hb_, d["einb"]], writes=[Hb[hn]])
            mm(ps[:, yb, c * 64:(c + 1) * 64], d["ar"][:, c, 64:128], Hs[hc][:], True, False, [Hb[hc], d["arb"]], [pb[yb]])
            mm(ps[:, yb, c * 64:(c + 1) * 64], d["qbt"][:, c, :], Us[ui][:], False, True, [Ub[ui], d["qbtb"]], [pb[yb]])
            mm(ps[:, 5, c * 64:(c + 1) * 64], d["qkt"][:, c, :], vtm, True, True, [d["vbb"], d["qktb"]], [pb[5]])
            hc = hn
            if "n" not in DBG:
                for _ in itertools.islice(gen, ISL):
                    pass
            if "c" in DBG:
                sc.barrier()
        sc.op("act", lambda: nc.scalar.copy(out=d["ysb"][:, :w], in_=ps[:, 5, :w]), reads=[pb[5]], writes=[d["ysbb"]])
        sc.op("dve", lambda: nc.vector.tensor_tensor(out=d["ysb"][:, :w], in0=ps[:, yb, :w], in1=d["ysb"][:, :w], op=ALU.add),
              reads=[pb[yb], d["ysbb"]], writes=[d["ysbb"]])
        for l in range(2):
            sc.dma("sp", Yd[pair, l, c0 * 64:c0 * 64 + w, :].rearrange("(c t) i -> t c i", t=64),
                   d["ysb"][l * 64:(l + 1) * 64, :w].rearrange("p (c i) -> p c i", i=64), reads=[d["ysbb"]])
        if "b" in DBG:
            sc.barrier()
        for _ in gen:
            pass
        if "b" in DBG:
            sc.barrier()
    sc.finish([S[0]["ysbb"], S[1]["ysbb"]])
    return nc


def scan_consts():
    s = np.arange(64)[:, None]
    t = np.arange(64)[None, :]
    tri = (s <= t).astype(np.float32)
    trs = (s < t).astype(np.float32)
    c = np.stack([tri, trs, trs.T, np.eye(64, dtype=np.float32)], axis=1)
    return np.ascontiguousarray(np.concatenate([c, c], axis=0))


RW_NCH_PAD = ((RW_NCH + SEG - 1) // SEG) * SEG


def run_rwkv_scan(lat, ctx):
    nc = build_rwkv_scan(2, RW_NCH_PAD)
    NP = RW_NCH_PAD * 64
    pad = np.zeros((10, D, NP - RW_N), np.float32)
    full = np.concatenate([ctx, lat, pad], axis=2)
    rev = np.concatenate([ctx[:, :, ::-1], lat[:, :, ::-1], pad], axis=2)
    C = scan_consts()
    maps = []
    for r in range(NCORES):
        Fs, Ts = [], []
        sl = slice(r * 128, (r + 1) * 128)
        for z in range(2):
            src = full if z == 0 else rev
            rr_, vv, kap, kz, lw, bz = src[0, sl], src[1, sl], src[2, sl], src[3 + z, sl], src[5 + z, sl], src[7 + z, sl]
            Fs.append(np.stack([rr_, kz, kap, bz]))
            tmaj = lambda a: a.reshape(2, 64, RW_NCH_PAD, 64).transpose(0, 3, 2, 1).reshape(128, RW_NCH_PAD, 64)
            Ts.append(np.stack([tmaj(lw), tmaj(kz), tmaj(bz), tmaj(vv)]))
        maps.append({"F": np.ascontiguousarray(np.stack(Fs)), "Tm": np.ascontiguousarray(np.stack(Ts)), "C": C})
    res = run_bass_kernel_spmd(nc, maps, core_ids=list(range(NCORES)))
    y0 = np.zeros((D, RW_N), np.float32)
    y1 = np.zeros((D, RW_N), np.float32)
    for r in range(NCORES):
        Y = res.results[r]["Y"]
        for hh in range(2):
            h = 2 * r + hh
            y0[h * 64:(h + 1) * 64] = Y[0, hh, :RW_N].T
            yr = Y[1, hh, :RW_N].T
            y1[h * 64:(h + 1) * 64] = np.concatenate([yr[:, :NCTX][:, ::-1], yr[:, NCTX:][:, ::-1]], axis=1)
    return y0, y1


def build_rwkv_mid(T_lat, T_ctx):
    nc = bass.Bass("TRN2", target_bir_lowering=False)
    T = T_lat + T_ctx
    Id = nc.dram_tensor("inp", [7, D, T], F32, kind="ExternalInput").ap()
    RKd = nc.dram_tensor("r_k", [128, DC], F32, kind="ExternalInput").ap()
    LWd = nc.dram_tensor("ln_w", [128, DC], F32, kind="ExternalInput").ap()
    LBd = nc.dram_tensor("ln_b", [128, DC], F32, kind="ExternalInput").ap()
    BOd = nc.dram_tensor("blk", [128, 128], F32, kind="ExternalInput").ap()
    Od = nc.dram_tensor("oT", [D, T], BF16, kind="ExternalOutput").ap()
    sc = Sched(nc)
    tl = TL(nc, sc)
    ps = tl.ps
    rk_t, rk_b = tl.load_vec("rk_sb", RKd, [128, DC])
    lw_t, lw_b = tl.load_vec("lw_sb", LWd, [128, DC])
    lb_t, lb_b = tl.load_vec("lb_sb", LBd, [128, DC])
    blk_t, blk_b = tl.load_vec("blk_sb", BOd, [128, 128])
    tiles = token_tiles(T_lat, T_ctx)
    NI = 2
    it = [[nc.alloc_sbuf_tensor(f"in{q}_{i}", [128, 512], F32) for i in range(NI)] for q in range(7)]
    itb = [[sc.buf(f"in{q}_{i}") for i in range(NI)] for q in range(7)]
    NS = 8
    stg = [nc.alloc_sbuf_tensor(f"stg{i}", [128, 512], F32) for i in range(NS)]
    stgb = sc.bufs("stgb", NS)
    ob = [nc.alloc_sbuf_tensor(f"ob{i}", [128, 512], BF16) for i in range(2)]
    obb = sc.bufs("obb", 2)

    def stage():
        i = tl.rot("stg", NS)
        return stg[i], stgb[i]

    for (t0, w, ty) in tiles:
        for c in range(DC):
            sl = tl.rot("in", NI)
            X = []
            for q in range(7):
                sc.dma("sp", it[q][sl][:, :w], Id[q, c * 128:(c + 1) * 128, t0:t0 + w], writes=[itb[q][sl]])
                X.append((it[q][sl], itb[q][sl]))
            (y0, y0b), (y1, y1b), (r_, rb), (k0, k0b), (k1, k1b), (v_, vb_), (g_, gb) = X
            y, yb = stage()
            sc.op("pool", lambda: nc.gpsimd.tensor_tensor(out=y[:, :w], in0=y0[:, :w], in1=y1[:, :w], op=ALU.add),
                  reads=[y0b, y1b], writes=[yb])
            b1 = tl.rot("pa", 2)
            sc.op("pe", lambda: nc.tensor.matmul(ps[:, b1, :w], lhsT=blk_t[:], rhs=y[:, :w], start=True, stop=True),
                  reads=[blk_b, yb], writes=[tl.pb[b1]])
            yc, ycb = stage()
            sc.op("dve", lambda: nc.vector.scalar_tensor_tensor(out=yc[:, :w], in0=ps[:, b1, :w], scalar=float(-1.0 / 64), in1=y[:, :w],
                                                                op0=ALU.mult, op1=ALU.add), reads=[tl.pb[b1], yb], writes=[ycb])
            q2, q2b = stage()
            sc.op("pool", lambda: nc.gpsimd.tensor_tensor(out=q2[:, :w], in0=yc[:, :w], in1=yc[:, :w], op=ALU.mult),
                  reads=[ycb], writes=[q2b])
            b2 = 2 + tl.rot("pb", 2)
            sc.op("pe", lambda: nc.tensor.matmul(ps[:, b2, :w], lhsT=blk_t[:], rhs=q2[:, :w], start=True, stop=True),
                  reads=[blk_b, q2b], writes=[tl.pb[b2]])
            rs, rsb = stage()
            sc.op("act", lambda: nc.scalar.activation(out=rs[:, :w], in_=ps[:, b2, :w], func=AF.Sqrt, bias=64e-5, scale=1.0 / 64),
                  reads=[tl.pb[b2]], writes=[rsb])
            sc.op("dve", lambda: nc.vector.reciprocal(out=rs[:, :w], in_=rs[:, :w]), reads=[rsb], writes=[rsb])
            sc.op("dve", lambda: nc.vector.tensor_tensor(out=yc[:, :w], in0=yc[:, :w], in1=rs[:, :w], op=ALU.mult),
                  reads=[ycb, rsb], writes=[ycb])
            sc.op("act", lambda: nc.scalar.activation(out=yc[:, :w], in_=yc[:, :w], func=AF.Identity, bias=lb_t[:, c:c + 1],
                                                      scale=lw_t[:, c:c + 1]), reads=[ycb, lw_b, lb_b], writes=[ycb])
            kk_, kkb = stage()
            sc.op("pool", lambda: nc.gpsimd.tensor_tensor(out=kk_[:, :w], in0=k0[:, :w], in1=k1[:, :w], op=ALU.add),
                  reads=[k0b, k1b], writes=[kkb])
            sc.op("dve", lambda: nc.vector.scalar_tensor_tensor(out=kk_[:, :w], in0=kk_[:, :w], scalar=rk_t[:, c:c + 1], in1=r_[:, :w],
                                                                op0=ALU.mult, op1=ALU.mult), reads=[kkb, rk_b, rb], writes=[kkb])
            b3 = 4 + tl.rot("pc", 2)
            sc.op("pe", lambda: nc.tensor.matmul(ps[:, b3, :w], lhsT=blk_t[:], rhs=kk_[:, :w], start=True, stop=True),
                  reads=[blk_b, kkb], writes=[tl.pb[b3]])
            bn, bnb = stage()
            sc.op("dve", lambda: nc.vector.tensor_tensor(out=bn[:, :w], in0=ps[:, b3, :w], in1=v_[:, :w], op=ALU.mult),
                  reads=[tl.pb[b3], vb_], writes=[bnb])
            sc.op("pool", lambda: nc.gpsimd.tensor_tensor(out=bn[:, :w], in0=bn[:, :w], in1=yc[:, :w], op=ALU.add),
                  reads=[bnb, ycb], writes=[bnb])
            oi = tl.rot("ob", 2)
            sc.op("pool", lambda: nc.gpsimd.tensor_tensor(out=ob[oi][:, :w], in0=bn[:, :w], in1=g_[:, :w], op=ALU.mult),
                  reads=[bnb, gb], writes=[obb[oi]])
            sc.dma("sp", Od[c * 128:(c + 1) * 128, t0:t0 + w], ob[oi][:, :w], reads=[obb[oi]])
    sc.finish(obb)
    return nc


def run_rwkv_mid(y0, y1, lat, ctx, inp):
    nc = build_rwkv_mid(2048, 32)
    common = {"r_k": vec_pc(inp["rwkv_r_k"][0].reshape(-1)), "ln_w": vec_pc(inp["rwkv_ln_w"][0]),
              "ln_b": vec_pc(inp["rwkv_ln_b"][0]), "blk": blockones()}
    maps = []
    for r in range(NCORES):
        ls = slice(r * 2048, (r + 1) * 2048)
        cs = slice(r * 32, (r + 1) * 32)
        arrs = []
        for (yl, yc) in ((y0[:, NCTX:], y0[:, :NCTX]), (y1[:, NCTX:], y1[:, :NCTX])):
            arrs.append(np.concatenate([yl[:, ls], yc[:, cs]], axis=1))
        for qi in (0, 3, 4, 1, 9):
            arrs.append(np.concatenate([lat[qi][:, ls], ctx[qi][:, cs]], axis=1))
        m = dict(common)
        m["inp"] = np.ascontiguousarray(np.stack(arrs))
        maps.append(m)
    res = run_bass_kernel_spmd(nc, maps, core_ids=list(range(NCORES)))
    os_ = [res.results[r]["oT"] for r in range(NCORES)]
    return (np.concatenate([o[:, :2048] for o in os_], axis=1), np.concatenate([o[:, 2048:] for o in os_], axis=1))


def kernel(**inp):
    inp = {k: np.asarray(v) for k, v in inp.items()}
    mod = run_stage_mod(inp)
    xl = to_fm(inp["x"][0])
    xc = to_fm(inp["ctx"][0])
    for li, j in ((0, 0),):
        lam_init = 0.8 - 0.6 * math.exp(-0.3 * li)
        qkv_l, qkv_c = run_pre_attn(xl, xc, mod[li], inp["norm_g"][li, 0], inp["attn_w_in"][j])
        o_l, o_c = run_attn_core(qkv_l, qkv_c, inp["attn_lambda"][j], inp["attn_subln"][j], lam_init, True)
        xl, xc = run_post(xl, xc, o_l, o_c, mod[li], inp, inp["attn_w_out"][j], inp["norm_g"][li, 1], li, False, False)
    li = 1
    o_l, o_c = run_gmlp(xl, xc, mod[li], inp["norm_g"][li, 0], inp)
    xl, xc = run_post(xl, xc, o_l, o_c, mod[li], inp, inp["gmlp_w_out"][0], inp["norm_g"][li, 1], li, True, False)
    li = 2
    lat, ctx = run_rwkv_pre(xl, xc, mod[li], inp["norm_g"][li, 0], inp)
    y0, y1 = run_rwkv_scan(lat, ctx)
    o_l, o_c = run_rwkv_mid(y0, y1, lat, ctx, inp)
    xl, xc = run_post(xl, xc, o_l, o_c, mod[li], inp, inp["rwkv_w_out"][0], inp["norm_g"][li, 1], li, False, False)
    li, j = 3, 1
    lam_init = 0.8 - 0.6 * math.exp(-0.3 * li)
    qkv_l, qkv_c = run_pre_attn(xl, xc, mod[li], inp["norm_g"][li, 0], inp["attn_w_in"][j])
    o_l, _ = run_attn_core(qkv_l, qkv_c, inp["attn_lambda"][j], inp["attn_subln"][j], lam_init, False)
    xl, _ = run_post(xl, None, o_l, None, mod[li], inp, inp["attn_w_out"][j], inp["norm_g"][li, 1], li, True, True)
    return np.ascontiguousarray(xl.T)[None].astype(np.float32)
```

```python
import math
import numpy as np
import ml_dtypes
import concourse.bass as bass
import concourse.mybir as mybir
from concourse.bass_utils import run_bass_kernel_spmd

F32 = mybir.dt.float32
BF16 = mybir.dt.bfloat16
AF = mybir.ActivationFunctionType
ALU = mybir.AluOpType
AX = mybir.AxisListType
NPBF = ml_dtypes.bfloat16

NCORES = 8
D = 1024
DC = 8
SEQ = 16384
NCTX = 256
FFN = 2816
FC = 22
NE = 8
EPS = 1e-6


class Buf:
    __slots__ = ("name", "w", "r", "sem_in", "sem_out", "n_in", "n_out")

    def __init__(self, name):
        self.name = name
        self.w = None
        self.r = {}
        self.sem_in = None
        self.sem_out = None
        self.n_in = 0
        self.n_out = 0


class Sched:
    def __init__(self, nc):
        self.nc = nc
        self.engs = {}
        for name, e in (("pe", nc.tensor), ("act", nc.scalar), ("dve", nc.vector),
                        ("pool", nc.gpsimd), ("sp", nc.sync)):
            self.engs[name] = dict(e=e, sem=nc.alloc_semaphore(name="sem_" + name), cnt=0, seen={})
        self.nsem = 5
        self.ninst = 0
        self.dma_toks = {}

    def buf(self, name):
        return Buf(name)

    def bufs(self, name, n):
        return [Buf(f"{name}{i}") for i in range(n)]

    def _wait(self, eng, toks):
        E = self.engs[eng]
        best = {}
        for t in toks:
            if t is None:
                continue
            s, v = t
            k = id(s)
            if E["seen"].get(k, 0) >= v:
                continue
            if k not in best or best[k][1] < v:
                best[k] = (s, v)
        for k, (s, v) in best.items():
            if s is E["sem"] and eng == "pe":
                E["seen"][k] = v
                continue
            E["e"].wait_ge(s, v)
            E["seen"][k] = v
            self.ninst += 1

    def _deps(self, reads, writes):
        toks = []
        for b in reads:
            toks.append(b.w)
        for b in writes:
            toks.append(b.w)
            toks.extend(b.r.values())
        return toks

    def op(self, eng, fn, reads=(), writes=()):
        E = self.engs[eng]
        self._wait(eng, self._deps(reads, writes))
        ins = fn()
        E["cnt"] += 1
        ins.then_inc(E["sem"], 1)
        tok = (E["sem"], E["cnt"])
        for b in reads:
            b.r[id(tok[0])] = tok
        for b in writes:
            b.w = tok
            b.r = {}
        self.ninst += 1
        return tok

    def dma(self, q, out, in_, reads=(), writes=(), **kw):
        E = self.engs[q]
        toks = []
        for b in reads:
            toks.append(b.w)
        for b in writes:
            toks.extend(b.r.values())
            if b.w is not None and (b.sem_in is None or b.w[0] is not b.sem_in):
                toks.append(b.w)
        self._wait(q, toks)
        if writes:
            own = writes[0]
            if own.sem_in is None:
                own.sem_in = self.nc.alloc_semaphore(name=f"di_{own.name}_{self.nsem}")
                self.nsem += 1
            own.n_in += 16
            sem, val = own.sem_in, own.n_in
        else:
            own = reads[0]
            if own.sem_out is None:
                own.sem_out = self.nc.alloc_semaphore(name=f"do_{own.name}_{self.nsem}")
                self.nsem += 1
            own.n_out += 16
            sem, val = own.sem_out, own.n_out
        E["e"].dma_start(out=out, in_=in_, **kw).then_inc(sem, 16)
        tok = (sem, val)
        self.dma_toks[id(sem)] = tok
        for b in reads:
            b.r[id(sem)] = tok
        for b in writes:
            b.w = tok
            b.r = {}
        self.ninst += 1
        return tok

    def barrier(self):
        toks = [(E["sem"], E["cnt"]) for E in self.engs.values() if E["cnt"] > 0]
        toks += list(self.dma_toks.values())
        for q in self.engs:
            self._wait(q, toks)

    def finish(self, bufs):
        toks = []
        for b in bufs:
            toks.append(b.w)
            toks.extend(b.r.values())
        self._wait("sp", toks)


def bcast(ap, shape):
    return ap.to_broadcast(shape)


def build_stage_mod():
    nc = bass.Bass("TRN2", target_bir_lowering=False)
    W = nc.dram_tensor("w", [D, 3072], F32, kind="ExternalInput").ap()
    Bv = nc.dram_tensor("b", [128, 24], F32, kind="ExternalInput").ap()
    S = nc.dram_tensor("s", [128, DC, 2], F32, kind="ExternalInput").ap()
    O = nc.dram_tensor("o", [128, 24, 2], F32, kind="ExternalOutput").ap()
    sc = Sched(nc)
    wt = nc.alloc_sbuf_tensor("wt", [128, DC, 3072], F32)
    bt = nc.alloc_sbuf_tensor("bt", [128, 24], F32)
    st = nc.alloc_sbuf_tensor("st", [128, DC, 2], F32)
    ot = nc.alloc_sbuf_tensor("ot", [128, 24, 2], F32)
    ps = nc.alloc_psum_tensor("ps", [128, 24, 2], F32)
    bw = sc.bufs("w", DC)
    bb, bs, bo, bp = sc.buf("b"), sc.buf("s"), sc.buf("o"), sc.buf("ps")
    Wv = W.rearrange("(k p) n -> p k n", p=128)
    sc.dma("sp", st[:], S, writes=[bs])
    sc.dma("sp", bt[:], Bv, writes=[bb])
    for k in range(DC):
        sc.dma("sp", wt[:, k, :], Wv[:, k, :], writes=[bw[k]])
    sc.op("act", lambda: nc.scalar.activation(out=st[:], in_=st[:], func=AF.Silu), reads=[bs], writes=[bs])
    for n in range(24):
        for k in range(DC):
            sc.op("pe", lambda: nc.tensor.matmul(ps[:, n, :], lhsT=wt[:, k, n * 128:(n + 1) * 128],
                                                 rhs=st[:, k, :], start=(k == 0), stop=(k == DC - 1)),
                  reads=[bw[k], bs], writes=[bp])
    sc.op("dve", lambda: nc.vector.tensor_tensor(out=ot[:], in0=ps[:], in1=bt[:].unsqueeze(2).to_broadcast([128, 24, 2]),
                                                 op=ALU.add), reads=[bp, bb], writes=[bo])
    sc.dma("sp", O, ot[:], reads=[bo])
    sc.finish([bo])
    return nc


def run_stage_mod(inp):
    nc = build_stage_mod()
    s = np.stack([inp["c"][0], inp["c_ctx"]], axis=-1)
    s = np.ascontiguousarray(s.reshape(DC, 128, 2).transpose(1, 0, 2))
    maps = []
    for r in range(NCORES):
        l, hf = r // 2, r % 2
        w = np.ascontiguousarray(inp["mod_w"][l][:, hf * 3072:(hf + 1) * 3072])
        b = np.ascontiguousarray(inp["mod_b"][l][hf * 3072:(hf + 1) * 3072].reshape(24, 128).T)
        maps.append({"w": w, "b": b, "s": s})
    res = run_bass_kernel_spmd(nc, maps, core_ids=list(range(NCORES)))
    mod = np.zeros((4, 128, 48, 2), np.float32)
    for r in range(NCORES):
        l, hf = r // 2, r % 2
        mod[l][:, hf * 24:(hf + 1) * 24, :] = res.results[r]["o"]
    return mod


def token_tiles(T_lat, T_ctx):
    tiles = []
    t = 0
    while t < T_lat:
        w = min(512, T_lat - t)
        tiles.append((t, w, 0))
        t += w
    if T_ctx:
        tiles.append((T_lat, T_ctx, 1))
    return tiles


class TL:
    def __init__(self, nc, sc):
        self.nc, self.sc = nc, sc
        self.ps = nc.alloc_psum_tensor("ps", [128, 8, 512], F32)
        self.pb = sc.bufs("psb", 8)
        self.ones = nc.alloc_sbuf_tensor("ones_bf", [128, 128], BF16)
        self.b_ones = sc.buf("ones")
        sc.op("dve", lambda: nc.vector.memset(self.ones[:], 1.0), writes=[self.b_ones])
        self.cnt = {}

    def rot(self, key, n):
        v = self.cnt.get(key, 0)
        self.cnt[key] = v + 1
        return v % n

    def load_vec(self, name, dram_ap, shape, q="sp"):
        t = self.nc.alloc_sbuf_tensor(name, shape, F32)
        b = self.sc.buf(name)
        self.sc.dma(q, t[:], dram_ap, writes=[b])
        return t, b

    def load_w_bf16(self, name, dram_ap_pkn, shape):
        t = self.nc.alloc_sbuf_tensor(name, shape, BF16)
        b = self.sc.buf(name)
        self.sc.dma("pool", t[:], dram_ap_pkn, writes=[b])
        return t, b


def emit_norm_mod(tl, x_ap, xb, w, geff, shift, col, h_out_ap, hb, tmp, tmpb, sq, sqb, rstd, rstdb,
                  h32_ap=None, h32b=None):
    nc, sc = tl.nc, tl.sc
    ps = tl.ps[:, 6, :w]
    sc.op("act", lambda: nc.scalar.activation(out=sq[:, :, :w], in_=x_ap, func=AF.Square), reads=[xb], writes=[sqb])
    for c in range(DC):
        sc.op("pe", lambda: nc.tensor.matmul(ps, lhsT=tl.ones[:], rhs=sq[:, c, :w], start=(c == 0), stop=(c == DC - 1)),
              reads=[sqb, tl.b_ones], writes=[tl.pb[6]])
    sc.op("act", lambda: nc.scalar.activation(out=rstd[:, :w], in_=ps, func=AF.Sqrt, bias=float(D * EPS), scale=1.0),
          reads=[tl.pb[6]], writes=[rstdb])
    sc.op("dve", lambda: nc.vector.reciprocal(out=rstd[:, :w], in_=rstd[:, :w]), reads=[rstdb], writes=[rstdb])
    sc.op("dve", lambda: nc.vector.tensor_tensor(out=tmp[:, :, :w], in0=x_ap,
                                                 in1=rstd[:, :w].unsqueeze(1).to_broadcast([128, DC, w]), op=ALU.mult),
          reads=[xb, rstdb], writes=[tmpb])
    for c in range(DC):
        sc.op("act", lambda: nc.scalar.activation(out=h_out_ap[:, c, :], in_=tmp[:, c, :w], func=AF.Identity,
                                                  bias=shift[:, c, col:col + 1], scale=geff[:, c, col:col + 1]),
              reads=[tmpb], writes=[hb])
        if h32_ap is not None:
            sc.op("act", lambda: nc.scalar.activation(out=h32_ap[:, c, :], in_=tmp[:, c, :w], func=AF.Identity,
                                                      bias=shift[:, c, col:col + 1], scale=geff[:, c, col:col + 1]),
                  reads=[tmpb], writes=[h32b])


def emit_geff(tl, name, mod_t, mod_b, g_t, g_b, j_scale, j_shift):
    nc, sc = tl.nc, tl.sc
    geff = nc.alloc_sbuf_tensor(name, [128, DC, 2], F32)
    gb = sc.buf(name)
    sc.op("dve", lambda: nc.vector.tensor_scalar(out=geff[:], in0=mod_t[:, j_scale * 8:(j_scale + 1) * 8, :],
                                                 scalar1=1.0, scalar2=float(math.sqrt(D)), op0=ALU.add, op1=ALU.mult),
          reads=[mod_b], writes=[gb])
    sc.op("dve", lambda: nc.vector.tensor_tensor(out=geff[:], in0=geff[:],
                                                 in1=g_t[:].unsqueeze(2).to_broadcast([128, DC, 2]), op=ALU.mult),
          reads=[gb, g_b], writes=[gb])
    return geff, gb, mod_t[:, j_shift * 8:(j_shift + 1) * 8, :]


FGROUPS = [(0, 4), (4, 4), (8, 4), (12, 4), (16, 4), (20, 2)]


def emit_ffn(tl, es, tiles, hT, hTb, xT, xTb, mod_t, mod_b, w1d, w3d, w2d, n_exp, gates=None):
    nc, sc = tl.nc, tl.sc
    A = lambda name, shape, dt: es.enter_context(nc.sbuf_tensor(name, shape, dt))
    w1t = [A(f"w1t{i}", [128, DC, 512], BF16) for i in range(2)]
    w3t = [A(f"w3t{i}", [128, DC, 512], BF16) for i in range(2)]
    w2t = [A(f"w2t{i}", [128, 4, D], BF16) for i in range(2)]
    wb = [sc.buf("wg0"), sc.buf("wg1")]
    s_sb = [A(f"s_sb{i}", [128, 512], F32) for i in range(2)]
    s_b = sc.bufs("s_b", 2)
    t_sb = [A(f"t_sb{i}", [128, 512], F32) for i in range(2)]
    t_b = sc.bufs("t_b", 2)
    g_sb = [A(f"g_sb{i}", [128, 4, 512], BF16) for i in range(2)]
    g_b = sc.bufs("g_b", 2)
    if gates is not None:
        gbc = [A(f"gbc{i}", [128, 512], F32) for i in range(2)]
        gbc_b = sc.bufs("gbc_b", 2)
    gi = 0
    for e in range(n_exp):
        for (f0, nf) in FGROUPS:
            slot = gi % 2
            gi += 1
            fw = nf * 128
            sc.dma("pool", w1t[slot][:, :, :fw],
                   w1d[e].rearrange("(k p) n -> p k n", p=128)[:, :, f0 * 128:f0 * 128 + fw], writes=[wb[slot]])
            sc.dma("pool", w3t[slot][:, :, :fw],
                   w3d[e].rearrange("(k p) n -> p k n", p=128)[:, :, f0 * 128:f0 * 128 + fw], writes=[wb[slot]])
            sc.dma("pool", w2t[slot][:, :nf, :],
                   w2d[e][f0 * 128:f0 * 128 + fw, :].rearrange("(k p) n -> p k n", p=128), writes=[wb[slot]])
            for (t0, w, ty) in tiles:
                gs = tl.rot("g", 2)
                if gates is not None:
                    bs_ = tl.rot("gbc", 2)
                    nblk = (w + 127) // 128
                    for bi in range(nblk):
                        bw_ = min(128, w - bi * 128)
                        blk = t0 // 128 + bi
                        sc.op("pe", lambda: nc.tensor.matmul(
                            tl.ps[:, 7, bi * 128:bi * 128 + bw_],
                            lhsT=gates["tm"][:bw_, blk, e:e + 1].to_broadcast([bw_, 128]),
                            rhs=gates["ident"][:bw_, :bw_], start=True, stop=True),
                            reads=[gates["b"], gates["ident_b"]], writes=[tl.pb[7]])
                    sc.op("act", lambda: nc.scalar.copy(out=gbc[bs_][:, :w], in_=tl.ps[:, 7, :w]),
                          reads=[tl.pb[7]], writes=[gbc_b[bs_]])
                for fi in range(nf):
                    b1 = 2 + tl.rot("h1", 2)
                    b3 = 4 + tl.rot("h3", 2)
                    for k in range(DC):
                        sc.op("pe", lambda: nc.tensor.matmul(tl.ps[:, b1, :w], lhsT=w1t[slot][:, k, fi * 128:(fi + 1) * 128],
                                                             rhs=hT[:, k, t0:t0 + w], start=(k == 0), stop=(k == DC - 1)),
                              reads=[wb[slot], hTb], writes=[tl.pb[b1]])
                    for k in range(DC):
                        sc.op("pe", lambda: nc.tensor.matmul(tl.ps[:, b3, :w], lhsT=w3t[slot][:, k, fi * 128:(fi + 1) * 128],
                                                             rhs=hT[:, k, t0:t0 + w], start=(k == 0), stop=(k == DC - 1)),
                              reads=[wb[slot], hTb], writes=[tl.pb[b3]])
                    ss = tl.rot("s", 2)
                    sc.op("act", lambda: nc.scalar.activation(out=s_sb[ss][:, :w], in_=tl.ps[:, b1, :w], func=AF.Silu),
                          reads=[tl.pb[b1]], writes=[s_b[ss]])
                    if gates is None:
                        sc.op("dve", lambda: nc.vector.tensor_tensor(out=g_sb[gs][:, fi, :w], in0=tl.ps[:, b3, :w],
                                                                     in1=s_sb[ss][:, :w], op=ALU.mult),
                              reads=[tl.pb[b3], s_b[ss]], writes=[g_b[gs]])
                    else:
                        ts = tl.rot("t", 2)
                        sc.op("dve", lambda: nc.vector.tensor_tensor(out=t_sb[ts][:, :w], in0=tl.ps[:, b3, :w],
                                                                     in1=s_sb[ss][:, :w], op=ALU.mult),
                              reads=[tl.pb[b3], s_b[ss]], writes=[t_b[ts]])
                        sc.op("pool", lambda: nc.gpsimd.tensor_tensor(out=g_sb[gs][:, fi, :w], in0=t_sb[ts][:, :w],
                                                                      in1=gbc[bs_][:, :w], op=ALU.mult),
                              reads=[t_b[ts], gbc_b[bs_]], writes=[g_b[gs]])
                for dc in range(DC):
                    bo = tl.rot("o", 2)
                    for fi in range(nf):
                        sc.op("pe", lambda: nc.tensor.matmul(tl.ps[:, bo, :w], lhsT=w2t[slot][:, fi, dc * 128:(dc + 1) * 128],
                                                             rhs=g_sb[gs][:, fi, :w], start=(fi == 0), stop=(fi == nf - 1)),
                              reads=[wb[slot], g_b[gs]], writes=[tl.pb[bo]])
                    sc.op("dve", lambda: nc.vector.scalar_tensor_tensor(
                        out=xT[:, dc, t0:t0 + w], in0=tl.ps[:, bo, :w], scalar=mod_t[:, 40 + dc, ty:ty + 1],
                        in1=xT[:, dc, t0:t0 + w], op0=ALU.mult, op1=ALU.add),
                        reads=[tl.pb[bo], mod_b, xTb[(t0, dc)]], writes=[xTb[(t0, dc)]])


def emit_gates(tl, tiles, h32, h32b, t0, w, router_t, router_b, gates):
    nc, sc = tl.nc, tl.sc
    nblk = (w + 127) // 128
    lg = gates["lg"]
    for bi in range(nblk):
        bw_ = min(128, w - bi * 128)
        blk = t0 // 128 + bi
        for k in range(DC):
            sc.op("pe", lambda: nc.tensor.matmul(tl.ps[:bw_, 7, 0:8], lhsT=h32[:, k, bi * 128:bi * 128 + bw_],
                                                 rhs=router_t[:, k, :], start=(k == 0), stop=(k == DC - 1)),
                  reads=[h32b, router_b], writes=[tl.pb[7]])
        sc.op("dve", lambda: nc.vector.tensor_copy(out=lg[:bw_, 0:8], in_=tl.ps[:bw_, 7, 0:8]),
              reads=[tl.pb[7]], writes=[gates["lgb"]])
        sc.op("dve", lambda: nc.vector.tensor_reduce(out=lg[:bw_, 8:9], in_=lg[:bw_, 0:8], axis=AX.X, op=ALU.max),
              reads=[gates["lgb"]], writes=[gates["lgb"]])
        sc.op("dve", lambda: nc.vector.tensor_scalar(out=lg[:bw_, 16:24], in0=lg[:bw_, 0:8], scalar1=lg[:bw_, 8:9],
                                                     scalar2=None, op0=ALU.is_equal),
              reads=[gates["lgb"]], writes=[gates["lgb"]])
        sc.op("dve", lambda: nc.vector.scalar_tensor_tensor(out=lg[:bw_, 24:32], in0=lg[:bw_, 16:24], scalar=-1e30,
                                                            in1=lg[:bw_, 0:8], op0=ALU.mult, op1=ALU.add),
              reads=[gates["lgb"]], writes=[gates["lgb"]])
        sc.op("dve", lambda: nc.vector.tensor_reduce(out=lg[:bw_, 9:10], in_=lg[:bw_, 24:32], axis=AX.X, op=ALU.max),
              reads=[gates["lgb"]], writes=[gates["lgb"]])
        sc.op("dve", lambda: nc.vector.tensor_scalar(out=lg[:bw_, 32:40], in0=lg[:bw_, 24:32], scalar1=lg[:bw_, 9:10],
                                                     scalar2=None, op0=ALU.is_equal),
              reads=[gates["lgb"]], writes=[gates["lgb"]])
        sc.op("dve", lambda: nc.vector.tensor_tensor(out=lg[:bw_, 10:11], in0=lg[:bw_, 9:10], in1=lg[:bw_, 8:9],
                                                     op=ALU.subtract),
              reads=[gates["lgb"]], writes=[gates["lgb"]])
        sc.op("act", lambda: nc.scalar.activation(out=lg[:bw_, 11:12], in_=lg[:bw_, 10:11], func=AF.Sigmoid),
              reads=[gates["lgb"]], writes=[gates["lgb"]])
        sc.op("dve", lambda: nc.vector.tensor_scalar(out=lg[:bw_, 12:13], in0=lg[:bw_, 11:12], scalar1=-1.0,
                                                     scalar2=1.0, op0=ALU.mult, op1=ALU.add),
              reads=[gates["lgb"]], writes=[gates["lgb"]])
        sc.op("dve", lambda: nc.vector.tensor_scalar(out=lg[:bw_, 32:40], in0=lg[:bw_, 32:40], scalar1=lg[:bw_, 11:12],
                                                     scalar2=None, op0=ALU.mult),
              reads=[gates["lgb"]], writes=[gates["lgb"]])
        sc.op("dve", lambda: nc.vector.scalar_tensor_tensor(out=gates["tm"][:bw_, blk, :], in0=lg[:bw_, 16:24],
                                                            scalar=lg[:bw_, 12:13], in1=lg[:bw_, 32:40],
                                                            op0=ALU.mult, op1=ALU.add),
              reads=[gates["lgb"]], writes=[gates["b"]])


def emit_norm_mod_multi(tl, x_ap, xbs, w, geff, shift, geff_b, mod_b, col, h_out_ap, hb, tmp, tmpb, sq, sqb, rstd, rstdb):
    nc, sc = tl.nc, tl.sc
    ps = tl.ps[:, 6, :w]
    sc.op("act", lambda: nc.scalar.activation(out=sq[:, :, :w], in_=x_ap, func=AF.Square), reads=xbs, writes=[sqb])
    for c in range(DC):
        sc.op("pe", lambda: nc.tensor.matmul(ps, lhsT=tl.ones[:], rhs=sq[:, c, :w], start=(c == 0), stop=(c == DC - 1)),
              reads=[sqb, tl.b_ones], writes=[tl.pb[6]])
    sc.op("act", lambda: nc.scalar.activation(out=rstd[:, :w], in_=ps, func=AF.Sqrt, bias=float(D * EPS), scale=1.0),
          reads=[tl.pb[6]], writes=[rstdb])
    sc.op("dve", lambda: nc.vector.reciprocal(out=rstd[:, :w], in_=rstd[:, :w]), reads=[rstdb], writes=[rstdb])
    sc.op("dve", lambda: nc.vector.tensor_tensor(out=tmp[:, :, :w], in0=x_ap,
                                                 in1=rstd[:, :w].unsqueeze(1).to_broadcast([128, DC, w]), op=ALU.mult),
          reads=list(xbs) + [rstdb], writes=[tmpb])
    for c in range(DC):
        sc.op("act", lambda: nc.scalar.activation(out=tmp[:, c, :w], in_=tmp[:, c, :w], func=AF.Identity,
                                                  bias=shift[:, c, col:col + 1], scale=geff[:, c, col:col + 1]),
              reads=[tmpb, geff_b, mod_b], writes=[tmpb])
    sc.op("pool", lambda: nc.gpsimd.tensor_copy(out=h_out_ap, in_=tmp[:, :, :w]), reads=[tmpb], writes=[hb])


def emit_final_norm(tl, x_ap, xbs, w, gf_t, gf_b, tmp, tmpb, sq, sqb, rstd, rstdb):
    nc, sc = tl.nc, tl.sc
    ps = tl.ps[:, 6, :w]
    sc.op("act", lambda: nc.scalar.activation(out=sq[:, :, :w], in_=x_ap, func=AF.Square), reads=xbs, writes=[sqb])
    for c in range(DC):
        sc.op("pe", lambda: nc.tensor.matmul(ps, lhsT=tl.ones[:], rhs=sq[:, c, :w], start=(c == 0), stop=(c == DC - 1)),
              reads=[sqb, tl.b_ones], writes=[tl.pb[6]])
    sc.op("act", lambda: nc.scalar.activation(out=rstd[:, :w], in_=ps, func=AF.Sqrt, bias=float(D * EPS), scale=1.0),
          reads=[tl.pb[6]], writes=[rstdb])
    sc.op("dve", lambda: nc.vector.reciprocal(out=rstd[:, :w], in_=rstd[:, :w]), reads=[rstdb], writes=[rstdb])
    sc.op("dve", lambda: nc.vector.tensor_tensor(out=tmp[:, :, :w], in0=x_ap,
                                                 in1=rstd[:, :w].unsqueeze(1).to_broadcast([128, DC, w]), op=ALU.mult),
          reads=list(xbs) + [rstdb], writes=[tmpb])
    for c in range(DC):
        sc.op("dve", lambda: nc.vector.tensor_scalar(out=tmp[:, c, :w], in0=tmp[:, c, :w], scalar1=gf_t[:, c:c + 1],
                                                     scalar2=float(math.sqrt(D)), op0=ALU.mult, op1=ALU.mult),
              reads=[tmpb, gf_b], writes=[tmpb])


def build_post(T_lat, T_ctx, moe, final_norm):
    import contextlib
    nc = bass.Bass("TRN2", target_bir_lowering=False)
    T = T_lat + T_ctx
    n_exp = NE if moe else 1
    Xd = nc.dram_tensor("xT", [D, T], F32, kind="ExternalInput").ap()
    Od = nc.dram_tensor("oT", [D, T], BF16, kind="ExternalInput").ap()
    Wod = nc.dram_tensor("w_out", [D, D], F32, kind="ExternalInput").ap()
    Md = nc.dram_tensor("mod", [128, 48, 2], F32, kind="ExternalInput").ap()
    Gd = nc.dram_tensor("g2", [128, DC], F32, kind="ExternalInput").ap()
    FGd = nc.dram_tensor("gf", [128, DC], F32, kind="ExternalInput").ap()
    W1d = nc.dram_tensor("w1", [n_exp, D, FFN], F32, kind="ExternalInput").ap()
    W3d = nc.dram_tensor("w3", [n_exp, D, FFN], F32, kind="ExternalInput").ap()
    W2d = nc.dram_tensor("w2", [n_exp, FFN, D], F32, kind="ExternalInput").ap()
    Rd = nc.dram_tensor("router", [D, NE], F32, kind="ExternalInput").ap()
    Idd = nc.dram_tensor("ident", [128, 128], F32, kind="ExternalInput").ap()
    Yd = nc.dram_tensor("yT", [D, T], F32, kind="ExternalOutput").ap()
    sc = Sched(nc)
    tl = TL(nc, sc)
    tiles = token_tiles(T_lat, T_ctx)
    xT = nc.alloc_sbuf_tensor("xT_sb", [128, DC, T], F32)
    xTb = {(t0, dc): sc.buf(f"x{t0}_{dc}") for (t0, w, ty) in tiles for dc in range(DC)}
    hT = nc.alloc_sbuf_tensor("hT_sb", [128, DC, T], BF16)
    hTb = sc.buf("hT")
    mod_t, mod_b = tl.load_vec("mod_sb", Md, [128, 48, 2])
    g2_t, g2_b = tl.load_vec("g2_sb", Gd, [128, DC])
    gf_t, gf_b = tl.load_vec("gf_sb", FGd, [128, DC])
    Xv = Xd.rearrange("(c p) t -> p c t", p=128)
    Yv = Yd.rearrange("(c p) t -> p c t", p=128)
    for (t0, w, ty) in tiles:
        sc.dma("sp", xT[:, :, t0:t0 + w], Xv[:, :, t0:t0 + w], writes=[xTb[(t0, dc)] for dc in range(DC)])
    geff, geff_b, shift = emit_geff(tl, "geff2", mod_t, mod_b, g2_t, g2_b, 4, 3)
    gates = None
    if moe:
        router_t, router_b = tl.load_vec("router_sb", Rd.rearrange("(k p) e -> p k e", p=128), [128, DC, NE])
        ident_t, ident_b = tl.load_vec("ident_sb", Idd, [128, 128])
        nblk_tot = (T + 127) // 128
        gates = dict(tm=nc.alloc_sbuf_tensor("gates_tm", [128, nblk_tot, NE], F32), b=sc.buf("gates"),
                     lg=nc.alloc_sbuf_tensor("lg_sb", [128, 40], F32), lgb=sc.buf("lg"),
                     ident=ident_t, ident_b=ident_b)
    with contextlib.ExitStack() as es:
        A = lambda name, shape, dt: es.enter_context(nc.sbuf_tensor(name, shape, dt))
        tmp, tmpb = A("tmp_sb", [128, DC, 512], F32), sc.buf("tmp")
        sq, sqb = A("sq_sb", [128, DC, 512], BF16), sc.buf("sq")
        rstd, rstdb = A("rstd_sb", [128, 512], F32), sc.buf("rstd")
        wo_t, wo_b = A("wo_sb", [128, DC, D], BF16), sc.buf("wo")
        sc.dma("pool", wo_t[:], Wod.rearrange("(k p) n -> p k n", p=128), writes=[wo_b])
        oT, oTb = A("oT_sb", [128, DC, 512], BF16), sc.buf("oT")
        Ov = Od.rearrange("(c p) t -> p c t", p=128)
        for (t0, w, ty) in tiles:
            sc.dma("sp", oT[:, :, :w], Ov[:, :, t0:t0 + w], writes=[oTb])
            for dc in range(DC):
                bo = tl.rot("o", 2)
                for k in range(DC):
                    sc.op("pe", lambda: nc.tensor.matmul(tl.ps[:, bo, :w], lhsT=wo_t[:, k, dc * 128:(dc + 1) * 128],
                                                         rhs=oT[:, k, :w], start=(k == 0), stop=(k == DC - 1)),
                          reads=[wo_b, oTb], writes=[tl.pb[bo]])
                sc.op("dve", lambda: nc.vector.scalar_tensor_tensor(
                    out=xT[:, dc, t0:t0 + w], in0=tl.ps[:, bo, :w], scalar=mod_t[:, 16 + dc, ty:ty + 1],
                    in1=xT[:, dc, t0:t0 + w], op0=ALU.mult, op1=ALU.add),
                    reads=[tl.pb[bo], mod_b, xTb[(t0, dc)]], writes=[xTb[(t0, dc)]])
            xbs = [xTb[(t0, dc)] for dc in range(DC)]
            emit_norm_mod_multi(tl, xT[:, :, t0:t0 + w], xbs, w, geff, shift, geff_b, mod_b, ty,
                                hT[:, :, t0:t0 + w], hTb, tmp, tmpb, sq, sqb, rstd, rstdb)
            if moe:
                emit_gates(tl, tiles, tmp, tmpb, t0, w, router_t, router_b, gates)
        sc.barrier()
    with contextlib.ExitStack() as es:
        emit_ffn(tl, es, tiles, hT, hTb, xT, xTb, mod_t, mod_b, W1d, W3d, W2d, n_exp, gates)
        sc.barrier()
    with contextlib.ExitStack() as es:
        A = lambda name, shape, dt: es.enter_context(nc.sbuf_tensor(name, shape, dt))
        outb = []
        if final_norm:
            tmps = [A(f"tmpf{i}", [128, DC, 512], F32) for i in range(2)]
            tmpbs = sc.bufs("tmpf", 2)
            sq, sqb = A("sqf_sb", [128, DC, 512], BF16), sc.buf("sqf")
            rstd, rstdb = A("rstdf_sb", [128, 512], F32), sc.buf("rstdf")
        for ti, (t0, w, ty) in enumerate(tiles):
            if final_norm and ty == 0:
                xbs = [xTb[(t0, dc)] for dc in range(DC)]
                tmp, tmpb = tmps[ti % 2], tmpbs[ti % 2]
                emit_final_norm(tl, xT[:, :, t0:t0 + w], xbs, w, gf_t, gf_b, tmp, tmpb, sq, sqb, rstd, rstdb)
                sc.dma("sp", Yv[:, :, t0:t0 + w], tmp[:, :, :w], reads=[tmpb])
                outb.append(tmpb)
            else:
                sc.dma("sp", Yv[:, :, t0:t0 + w], xT[:, :, t0:t0 + w], reads=[xTb[(t0, dc)] for dc in range(DC)])
                outb.extend(xTb[(t0, dc)] for dc in range(DC))
        sc.finish(outb)
    return nc


def to_fm(a):
    return np.ascontiguousarray(a.T)


def vec_pc(v):
    return np.ascontiguousarray(v.reshape(DC, 128).T)


def shard_tokens(lat_fm, ctx_fm, r, use_ctx=True):
    parts = [lat_fm[:, r * 2048:(r + 1) * 2048]]
    if use_ctx:
        parts.append(ctx_fm[:, r * 32:(r + 1) * 32])
    return np.ascontiguousarray(np.concatenate(parts, axis=1))


_prog_cache = {}


def get_prog(key, builder):
    if key not in _prog_cache:
        _prog_cache[key] = builder()
    return _prog_cache[key]


def run_post(xl_fm, xc_fm, ol_fm, oc_fm, mod_l, inp, w_out, g2, li, moe, final_norm):
    use_ctx = xc_fm is not None
    T_ctx = 32 if use_ctx else 0
    nc = build_post(2048, T_ctx, moe, final_norm)
    fi = li // 2
    if moe:
        w1, w3, w2 = inp["moe_w1"][fi], inp["moe_w3"][fi], inp["moe_w2"][fi]
        router = inp["moe_router"][fi]
    else:
        w1, w3, w2 = inp["ffn_w1"][fi][None], inp["ffn_w3"][fi][None], inp["ffn_w2"][fi][None]
        router = np.zeros((D, NE), np.float32)
    common = {"w_out": np.ascontiguousarray(w_out), "mod": np.ascontiguousarray(mod_l), "g2": vec_pc(g2),
              "gf": vec_pc(inp["final_g"]), "w1": np.ascontiguousarray(w1), "w3": np.ascontiguousarray(w3),
              "w2": np.ascontiguousarray(w2), "router": np.ascontiguousarray(router),
              "ident": np.eye(128, dtype=np.float32)}
    maps = []
    for r in range(NCORES):
        m = dict(common)
        m["xT"] = shard_tokens(xl_fm, xc_fm, r, use_ctx)
        m["oT"] = shard_tokens(ol_fm, oc_fm, r, use_ctx)
        maps.append(m)
    res = run_bass_kernel_spmd(nc, maps, core_ids=list(range(NCORES)))
    ys = [res.results[r]["yT"] for r in range(NCORES)]
    xl_new = np.concatenate([y[:, :2048] for y in ys], axis=1)
    xc_new = np.concatenate([y[:, 2048:] for y in ys], axis=1) if use_ctx else None
    return xl_new, xc_new


def build_pre_attn(T_lat, T_ctx):
    nc = bass.Bass("TRN2", target_bir_lowering=False)
    T = T_lat + T_ctx
    Xd = nc.dram_tensor("xT", [D, T], F32, kind="ExternalInput").ap()
    Wd = nc.dram_tensor("w_in", [D, 3 * D], F32, kind="ExternalInput").ap()
    Md = nc.dram_tensor("mod", [128, 48, 2], F32, kind="ExternalInput").ap()
    Gd = nc.dram_tensor("g1", [128, DC], F32, kind="ExternalInput").ap()
    Cd = nc.dram_tensor("cos", [128, T], F32, kind="ExternalInput").ap()
    Sd = nc.dram_tensor("sin", [128, T], F32, kind="ExternalInput").ap()
    Qd = nc.dram_tensor("qkvT", [3 * D, T], BF16, kind="ExternalOutput").ap()
    sc = Sched(nc)
    tl = TL(nc, sc)
    tiles = token_tiles(T_lat, T_ctx)
    mod_t, mod_b = tl.load_vec("mod_sb", Md, [128, 48, 2])
    g1_t, g1_b = tl.load_vec("g1_sb", Gd, [128, DC])
    cos_t, cos_b = tl.load_vec("cos_sb", Cd, [128, T])
    sin_t, sin_b = tl.load_vec("sin_sb", Sd, [128, T])
    geff, geff_b, shift = emit_geff(tl, "geff1", mod_t, mod_b, g1_t, g1_b, 1, 0)
    w_t, w_b = tl.load_w_bf16("w_sb", Wd.rearrange("(k p) n -> p k n", p=128), [128, DC, 3 * D])
    wr_t = nc.alloc_sbuf_tensor("wr_sb", [128, DC, 2 * D], BF16)
    wr_b = sc.buf("wr")
    wv = w_t[:, :, 0:2 * D].rearrange("p k (m q e) -> p k m q e", q=4, e=16)
    wrv = wr_t[:].rearrange("p k (m q e) -> p k m q e", q=4, e=16)
    for k in range(DC):
        for (dst, src, sgn) in ((0, 1, -1.0), (1, 0, 1.0), (2, 3, -1.0), (3, 2, 1.0)):
            sc.op("pool", lambda: nc.gpsimd.tensor_scalar(out=wrv[:, k, :, dst, :], in0=wv[:, k, :, src, :], scalar1=sgn,
                                                          scalar2=None, op0=ALU.mult), reads=[w_b], writes=[wr_b])
    xt = nc.alloc_sbuf_tensor("x_sb", [128, DC, 512], F32)
    xb = sc.buf("x")
    hT = nc.alloc_sbuf_tensor("hT_sb", [128, DC, 512], BF16)
    hTb = sc.buf("hT")
    tmp, tmpb = nc.alloc_sbuf_tensor("tmp_sb", [128, DC, 512], F32), sc.buf("tmp")
    sq, sqb = nc.alloc_sbuf_tensor("sq_sb", [128, DC, 512], BF16), sc.buf("sq")
    rstd, rstdb = nc.alloc_sbuf_tensor("rstd_sb", [128, 512], F32), sc.buf("rstd")
    t1 = [nc.alloc_sbuf_tensor(f"t1_{i}", [128, 512], F32) for i in range(2)]
    t1b = sc.bufs("t1b", 2)
    t2 = [nc.alloc_sbuf_tensor(f"t2_{i}", [128, 512], F32) for i in range(2)]
    t2b = sc.bufs("t2b", 2)
    ob = [nc.alloc_sbuf_tensor(f"ob_{i}", [128, 512], BF16) for i in range(4)]
    obb = sc.bufs("obb", 4)
    Xv = Xd.rearrange("(c p) t -> p c t", p=128)
    Qv = Qd.rearrange("(c p) t -> p c t", p=128)
    outs = []
    for (t0, w, ty) in tiles:
        sc.dma("sp", xt[:, :, :w], Xv[:, :, t0:t0 + w], writes=[xb])
        emit_norm_mod_multi(tl, xt[:, :, :w], [xb], w, geff, shift, geff_b, mod_b, ty, hT[:, :, :w], hTb,
                            tmp, tmpb, sq, sqb, rstd, rstdb)
        for mc in range(24):
            oi = tl.rot("ob", 4)
            ba = tl.rot("pa", 2)
            for k in range(DC):
                sc.op("pe", lambda: nc.tensor.matmul(tl.ps[:, ba, :w], lhsT=w_t[:, k, mc * 128:(mc + 1) * 128],
                                                     rhs=hT[:, k, :w], start=(k == 0), stop=(k == DC - 1)),
                      reads=[w_b, hTb], writes=[tl.pb[ba]])
            if mc < 16:
                bb_ = 2 + tl.rot("pb", 2)
                for k in range(DC):
                    sc.op("pe", lambda: nc.tensor.matmul(tl.ps[:, bb_, :w], lhsT=wr_t[:, k, mc * 128:(mc + 1) * 128],
                                                         rhs=hT[:, k, :w], start=(k == 0), stop=(k == DC - 1)),
                          reads=[wr_b, hTb], writes=[tl.pb[bb_]])
                ti = tl.rot("t12", 2)
                sc.op("dve", lambda: nc.vector.tensor_tensor(out=t1[ti][:, :w], in0=tl.ps[:, ba, :w], in1=cos_t[:, t0:t0 + w],
                                                             op=ALU.mult), reads=[tl.pb[ba], cos_b], writes=[t1b[ti]])
                sc.op("dve", lambda: nc.vector.tensor_tensor(out=t2[ti][:, :w], in0=tl.ps[:, bb_, :w], in1=sin_t[:, t0:t0 + w],
                                                             op=ALU.mult), reads=[tl.pb[bb_], sin_b], writes=[t2b[ti]])
                sc.op("pool", lambda: nc.gpsimd.tensor_tensor(out=ob[oi][:, :w], in0=t1[ti][:, :w], in1=t2[ti][:, :w],
                                                              op=ALU.add), reads=[t1b[ti], t2b[ti]], writes=[obb[oi]])
            else:
                sc.op("act", lambda: nc.scalar.copy(out=ob[oi][:, :w], in_=tl.ps[:, ba, :w]),
                      reads=[tl.pb[ba]], writes=[obb[oi]])
            sc.dma("sp", Qv[:, mc, t0:t0 + w], ob[oi][:, :w], reads=[obb[oi]])
    sc.finish(obb)
    return nc


def rope_tables():
    rows = SEQ // 64
    row = np.repeat(np.arange(rows, dtype=np.float32), 64)
    col = np.tile(np.arange(64, dtype=np.float32), rows)
    inv_freq = (10000.0 ** (-np.arange(0, 32, 2, dtype=np.float32) / 32)).astype(np.float32)
    ang_r = row[:, None] * inv_freq
    ang_c = col[:, None] * inv_freq
    ang = np.concatenate([ang_r, ang_r, ang_c, ang_c], axis=-1)
    return np.cos(ang).T.astype(np.float32), np.sin(ang).T.astype(np.float32)


def run_pre_attn(xl_fm, xc_fm, mod_l, g1, w_in):
    nc = build_pre_attn(2048, 32)
    cos, sin = rope_tables()
    cos2 = np.concatenate([cos, cos], axis=0)
    sin2 = np.concatenate([sin, sin], axis=0)
    maps = []
    for r in range(NCORES):
        c = np.concatenate([cos2[:, r * 2048:(r + 1) * 2048], np.ones((128, 32), np.float32)], axis=1)
        s = np.concatenate([sin2[:, r * 2048:(r + 1) * 2048], np.zeros((128, 32), np.float32)], axis=1)
        maps.append({"xT": shard_tokens(xl_fm, xc_fm, r), "w_in": np.ascontiguousarray(w_in),
                     "mod": np.ascontiguousarray(mod_l), "g1": vec_pc(g1),
                     "cos": np.ascontiguousarray(c), "sin": np.ascontiguousarray(s)})
    res = run_bass_kernel_spmd(nc, maps, core_ids=list(range(NCORES)))
    qs = [res.results[r]["qkvT"] for r in range(NCORES)]
    lat = np.concatenate([q[:, :2048] for q in qs], axis=1)
    ctx = np.concatenate([q[:, 2048:] for q in qs], axis=1)
    return lat, ctx


NK_ALL = NCTX + SEQ
NKC_ALL = NK_ALL // 128


def build_attn_core(TQ_lat, TQ_ctx, lam_init):
    nc = bass.Bass("TRN2", target_bir_lowering=False)
    TQ = TQ_lat + TQ_ctx
    Qd = nc.dram_tensor("qT", [D, TQ], BF16, kind="ExternalInput").ap()
    Kd = nc.dram_tensor("kT", [D, NK_ALL], BF16, kind="ExternalInput").ap()
    Vd = nc.dram_tensor("v", [8, 128, NKC_ALL, 128], BF16, kind="ExternalInput").ap()
    Ld = nc.dram_tensor("lam_p", [128, 4, 64], F32, kind="ExternalInput").ap()
    Sd = nc.dram_tensor("subln", [128, 128], F32, kind="ExternalInput").ap()
    Od = nc.dram_tensor("o", [TQ, D], BF16, kind="ExternalOutput").ap()
    sc = Sched(nc)
    tl = TL(nc, sc)
    tiles = token_tiles(TQ_lat, TQ_ctx)
    lp_t, lp_b = tl.load_vec("lp_sb", Ld, [128, 4, 64])
    sub_t, sub_b = tl.load_vec("sub_sb", Sd, [128, 128])
    lt = nc.alloc_sbuf_tensor("lt_sb", [128, 2, 64], F32)
    ls = nc.alloc_sbuf_tensor("ls_sb", [128, 4], F32)
    lb = sc.buf("lam")
    lpv = lp_t[:].rearrange("p (a b) d -> p a b d", b=2)
    sc.op("dve", lambda: nc.vector.tensor_tensor(out=lt[:], in0=lpv[:, :, 0, :], in1=lpv[:, :, 1, :], op=ALU.mult),
          reads=[lp_b], writes=[lb])
    sc.op("dve", lambda: nc.vector.tensor_reduce(out=ls[:, 0:2], in_=lt[:], axis=AX.X, op=ALU.add), reads=[lb], writes=[lb])
    sc.op("act", lambda: nc.scalar.activation(out=ls[:, 0:2], in_=ls[:, 0:2], func=AF.Exp), reads=[lb], writes=[lb])
    sc.op("dve", lambda: nc.vector.tensor_tensor(out=ls[:, 2:3], in0=ls[:, 1:2], in1=ls[:, 0:1], op=ALU.subtract),
          reads=[lb], writes=[lb])
    sc.op("dve", lambda: nc.vector.tensor_scalar(out=ls[:, 3:4], in0=ls[:, 2:3], scalar1=float(-lam_init), scalar2=None,
                                                 op0=ALU.add), reads=[lb], writes=[lb])
    neg_lam = ls[:, 3:4]
    sc.op("dve", lambda: nc.vector.tensor_scalar(out=sub_t[:], in0=sub_t[:], scalar1=float(1.0 - lam_init), scalar2=None,
                                                 op0=ALU.mult), reads=[sub_b], writes=[sub_b])
    kT = [nc.alloc_sbuf_tensor(f"kT{i}", [128, NK_ALL], BF16) for i in range(2)]
    kb = sc.bufs("kb", 2)
    vt = [nc.alloc_sbuf_tensor(f"vt{i}", [128, NKC_ALL, 129], BF16) for i in range(2)]
    vb = sc.bufs("vb", 2)
    qt = [nc.alloc_sbuf_tensor(f"qt{i}", [128, TQ], BF16) for i in range(2)]
    qb_ = sc.bufs("qb", 2)
    for i in range(2):
        sc.op("pool", lambda: nc.gpsimd.memset(vt[i][:, :, 128:129], 1.0), writes=[vb[i]])
    P = [nc.alloc_sbuf_tensor(f"P{i}", [128, 2, 512], BF16) for i in range(3)]
    Pb = sc.bufs("Pb", 3)
    rr = nc.alloc_sbuf_tensor("rr", [128, 8], F32)
    rrb = sc.buf("rr")
    d0 = nc.alloc_sbuf_tensor("d0", [128, 128], F32)
    d1 = nc.alloc_sbuf_tensor("d1", [128, 128], F32)
    junk = nc.alloc_sbuf_tensor("junk", [128, 128], F32)
    db = sc.buf("d")
    osb = [nc.alloc_sbuf_tensor(f"osb{i}", [128, 4, 128], BF16) for i in range(2)]
    osbb = sc.bufs("osbb", 2)
    ps = tl.ps

    def emit_qk(slot, t0, w, kc, sbk):
        for m in range(2):
            sc.op("pe", lambda: nc.tensor.matmul(ps[:, 2 * sbk + m, :w], lhsT=kT[slot][m * 64:(m + 1) * 64, kc * 128:(kc + 1) * 128],
                                                 rhs=qt[slot][m * 64:(m + 1) * 64, t0:t0 + w], start=True, stop=True),
                  reads=[kb[slot], qb_[slot]], writes=[tl.pb[2 * sbk + m]])

    for h in range(8):
        slot = h % 2
        sc.dma("sp", kT[slot][:], Kd[h * 128:(h + 1) * 128, :], writes=[kb[slot]])
        sc.dma("sp", vt[slot][:, :, 0:128], Vd[h], writes=[vb[slot]])
        sc.dma("sp", qt[slot][:], Qd[h * 128:(h + 1) * 128, :], writes=[qb_[slot]])
        for (t0, w, ty) in tiles:
            nkc = NKC_ALL if ty == 0 else NCTX // 128
            nqb = (w + 127) // 128
            sbk = tl.rot("S", 2)
            emit_qk(slot, t0, w, 0, sbk)
            for kc in range(nkc):
                cur = sbk
                if kc + 1 < nkc:
                    sbk = tl.rot("S", 2)
                    emit_qk(slot, t0, w, kc + 1, sbk)
                pi = tl.rot("P", 3)
                sc.op("act", lambda: nc.scalar.activation(out=P[pi][:, :, :w], in_=ps[:, 2 * cur:2 * cur + 2, :w], func=AF.Exp,
                                                          scale=0.125),
                      reads=[tl.pb[2 * cur], tl.pb[2 * cur + 1]], writes=[Pb[pi]])
                seen_bank = set()
                for m in range(2):
                    for qi in range(nqb):
                        bw_ = min(128, w - qi * 128)
                        so = m * 4 + qi
                        bank, off = 4 + so // 3, (so % 3) * 129
                        first = (kc == 0) and (bank not in seen_bank)
                        seen_bank.add(bank)
                        sc.op("pe", lambda: nc.tensor.matmul(ps[:bw_, bank, off:off + 129], lhsT=P[pi][:, m, qi * 128:qi * 128 + bw_],
                                                             rhs=vt[slot][:, kc, :], start=first, stop=(kc == nkc - 1),
                                                             skip_group_check=True),
                              reads=[Pb[pi], vb[slot]], writes=[tl.pb[bank]])
            oi = tl.rot("osb", 2)
            for qi in range(nqb):
                bw_ = min(128, w - qi * 128)
                b0, o0 = 4 + qi // 3, (qi % 3) * 129
                b1, o1 = 4 + (4 + qi) // 3, ((4 + qi) % 3) * 129
                O0 = ps[:bw_, b0, o0:o0 + 129]
                O1 = ps[:bw_, b1, o1:o1 + 129]
                sc.op("dve", lambda: nc.vector.reciprocal(out=rr[:bw_, 0:1], in_=O0[:, 128:129]), reads=[tl.pb[b0]], writes=[rrb])
                sc.op("dve", lambda: nc.vector.reciprocal(out=rr[:bw_, 1:2], in_=O1[:, 128:129]), reads=[tl.pb[b1]], writes=[rrb])
                sc.op("dve", lambda: nc.vector.tensor_tensor(out=rr[:bw_, 2:3], in0=rr[:bw_, 1:2], in1=neg_lam[:bw_, :], op=ALU.mult),
                      reads=[rrb, lb], writes=[rrb])
                sc.op("dve", lambda: nc.vector.tensor_scalar(out=d0[:bw_, :], in0=O0[:, 0:128], scalar1=rr[:bw_, 0:1], scalar2=None,
                                                             op0=ALU.mult), reads=[tl.pb[b0], rrb], writes=[db])
                sc.op("dve", lambda: nc.vector.scalar_tensor_tensor(out=d1[:bw_, :], in0=O1[:, 0:128], scalar=rr[:bw_, 2:3],
                                                                    in1=d0[:bw_, :], op0=ALU.mult, op1=ALU.add),
                      reads=[tl.pb[b1], rrb, db], writes=[db])
                sc.op("act", lambda: nc.scalar.activation(out=junk[:bw_, :], in_=d1[:bw_, :], func=AF.Square), reads=[db], writes=[db])
                sc.op("dve", lambda: nc.vector.tensor_reduce(out=rr[:bw_, 3:4], in_=junk[:bw_, :], axis=AX.X, op=ALU.add),
                      reads=[db], writes=[rrb])
                sc.op("act", lambda: nc.scalar.activation(out=rr[:bw_, 4:5], in_=rr[:bw_, 3:4], func=AF.Sqrt, bias=1e-5,
                                                          scale=1.0 / 128), reads=[rrb], writes=[rrb])
                sc.op("dve", lambda: nc.vector.reciprocal(out=rr[:bw_, 5:6], in_=rr[:bw_, 4:5]), reads=[rrb], writes=[rrb])
                sc.op("dve", lambda: nc.vector.scalar_tensor_tensor(out=osb[oi][:bw_, qi, :], in0=d1[:bw_, :], scalar=rr[:bw_, 5:6],
                                                                    in1=sub_t[:bw_, :], op0=ALU.mult, op1=ALU.mult),
                      reads=[db, rrb, sub_b], writes=[osbb[oi]])
            if w % 128 == 0:
                sc.dma("sp", Od[t0:t0 + w, h * 128:(h + 1) * 128].rearrange("(q p) e -> p q e", p=128), osb[oi][:, :nqb, :],
                       reads=[osbb[oi]])
            else:
                sc.dma("sp", Od[t0:t0 + w, h * 128:(h + 1) * 128], osb[oi][:w, 0, :], reads=[osbb[oi]])
    sc.finish(osbb)
    return nc


def run_attn_core(qkv_lat, qkv_ctx, lam_p, subln, lam_init, with_ctx_q):
    nc = build_attn_core(2048, 32 if with_ctx_q else 0, lam_init)
    kT = np.ascontiguousarray(np.concatenate([qkv_ctx[D:2 * D], qkv_lat[D:2 * D]], axis=1))
    vT = np.concatenate([qkv_ctx[2 * D:], qkv_lat[2 * D:]], axis=1)
    v = np.ascontiguousarray(vT.reshape(8, 128, NKC_ALL, 128).transpose(0, 3, 2, 1))
    lam_b = np.ascontiguousarray(np.broadcast_to(lam_p[None], (128, 4, 64)))
    sub_b = np.ascontiguousarray(np.broadcast_to(subln[None], (128, 128)))
    maps = []
    for r in range(NCORES):
        parts = [qkv_lat[:D, r * 2048:(r + 1) * 2048]]
        if with_ctx_q:
            parts.append(qkv_ctx[:D, r * 32:(r + 1) * 32])
        maps.append({"qT": np.ascontiguousarray(np.concatenate(parts, axis=1)), "kT": kT, "v": v,
                     "lam_p": lam_b, "subln": sub_b})
    res = run_bass_kernel_spmd(nc, maps, core_ids=list(range(NCORES)))
    os_ = [res.results[r]["o"] for r in range(NCORES)]
    o_lat = np.concatenate([o[:2048] for o in os_], axis=0)
    o_ctx = np.concatenate([o[2048:] for o in os_], axis=0) if with_ctx_q else None
    return to_fm(o_lat), (to_fm(o_ctx) if with_ctx_q else None)


def build_gmlp(T_lat, T_ctx):
    nc = bass.Bass("TRN2", target_bir_lowering=False)
    T = T_lat + T_ctx
    Xd = nc.dram_tensor("xT", [D, T], F32, kind="ExternalInput").ap()
    Wd = nc.dram_tensor("w_in", [D, 2 * D], F32, kind="ExternalInput").ap()
    Md = nc.dram_tensor("mod", [128, 48, 2], F32, kind="ExternalInput").ap()
    Gd = nc.dram_tensor("g1", [128, DC], F32, kind="ExternalInput").ap()
    LGd = nc.dram_tensor("ln_g", [128, D], F32, kind="ExternalInput").ap()
    LBd = nc.dram_tensor("ln_b", [128, D], F32, kind="ExternalInput").ap()
    WSd = nc.dram_tensor("w_sT", [128, 8, 128], F32, kind="ExternalInput").ap()
    BSd = nc.dram_tensor("b_s", [128, 8, 128], F32, kind="ExternalInput").ap()
    Od = nc.dram_tensor("oT", [D, T], BF16, kind="ExternalOutput").ap()
    sc = Sched(nc)
    tl = TL(nc, sc)
    tiles = token_tiles(T_lat, T_ctx)
    mod_t, mod_b = tl.load_vec("mod_sb", Md, [128, 48, 2])
    g1_t, g1_b = tl.load_vec("g1_sb", Gd, [128, DC])
    lng_t, lng_b = tl.load_vec("lng_sb", LGd, [128, D])
    lnb_t, lnb_b = tl.load_vec("lnb_sb", LBd, [128, D])
    bs_t, bs_b = tl.load_vec("bs_sb", BSd, [128, 8, 128])
    geff, geff_b, shift = emit_geff(tl, "geff1", mod_t, mod_b, g1_t, g1_b, 1, 0)
    w_t, w_b = tl.load_w_bf16("w_sb", Wd.rearrange("(k p) n -> p k n", p=128), [128, DC, 2 * D])
    ws_t, ws_b = tl.load_w_bf16("ws_sb", WSd, [128, 8, 128])
    xt, xb = nc.alloc_sbuf_tensor("x_sb", [128, DC, 512], F32), sc.buf("x")
    hT, hTb = nc.alloc_sbuf_tensor("hT_sb", [128, DC, 512], BF16), sc.buf("hT")
    tmp, tmpb = nc.alloc_sbuf_tensor("tmp_sb", [128, DC, 512], F32), sc.buf("tmp")
    sq, sqb = nc.alloc_sbuf_tensor("sq_sb", [128, DC, 512], BF16), sc.buf("sq")
    rstd, rstdb = nc.alloc_sbuf_tensor("rstd_sb", [128, 512], F32), sc.buf("rstd")
    uT, uTb = nc.alloc_sbuf_tensor("uT_sb", [128, DC, 512], F32), sc.buf("uT")
    vs = [nc.alloc_sbuf_tensor(f"v_sb{i}", [128, D], F32) for i in range(2)]
    vsb = sc.bufs("vsb", 2)
    st = nc.alloc_sbuf_tensor("st_sb", [128, 8], F32)
    stb = sc.buf("st")
    vn, vnb = nc.alloc_sbuf_tensor("vn_sb", [128, 4, D], BF16), sc.buf("vn")
    ts_ = [nc.alloc_sbuf_tensor(f"ts_sb{i}", [128, 512], F32) for i in range(2)]
    tsb = sc.bufs("tsb", 2)
    ob = [nc.alloc_sbuf_tensor(f"ob_{i}", [128, 512], BF16) for i in range(3)]
    obb = sc.bufs("obb", 3)
    Xv = Xd.rearrange("(c p) t -> p c t", p=128)
    Ov = Od.rearrange("(c p) t -> p c t", p=128)
    ps = tl.ps
    for (t0, w, ty) in tiles:
        nch = w // 128
        sc.dma("sp", xt[:, :, :w], Xv[:, :, t0:t0 + w], writes=[xb])
        emit_norm_mod_multi(tl, xt[:, :, :w], [xb], w, geff, shift, geff_b, mod_b, ty, hT[:, :, :w], hTb,
                            tmp, tmpb, sq, sqb, rstd, rstdb)
        for mc in range(DC):
            ba = tl.rot("pa", 2)
            for k in range(DC):
                sc.op("pe", lambda: nc.tensor.matmul(ps[:, ba, :w], lhsT=w_t[:, k, mc * 128:(mc + 1) * 128], rhs=hT[:, k, :w],
                                                     start=(k == 0), stop=(k == DC - 1)), reads=[w_b, hTb], writes=[tl.pb[ba]])
            sc.op("act", lambda: nc.scalar.activation(out=uT[:, mc, :w], in_=ps[:, ba, :w], func=AF.Gelu),
                  reads=[tl.pb[ba]], writes=[uTb])
        for ci in range(nch):
            vi = tl.rot("v", 2)
            v_ = vs[vi]
            for half in range(2):
                bb_ = 2 + tl.rot("pb", 2)
                for k in range(DC):
                    sc.op("pe", lambda: nc.tensor.matmul(ps[:, bb_, :], lhsT=hT[:, k, ci * 128:(ci + 1) * 128],
                                                         rhs=w_t[:, k, D + half * 512:D + (half + 1) * 512],
                                                         start=(k == 0), stop=(k == DC - 1)), reads=[w_b, hTb], writes=[tl.pb[bb_]])
                sc.op("act", lambda: nc.scalar.activation(out=v_[:, half * 512:(half + 1) * 512], in_=ps[:, bb_, :], func=AF.Gelu),
                      reads=[tl.pb[bb_]], writes=[vsb[vi]])
            sc.op("dve", lambda: nc.vector.tensor_reduce(out=st[:, 0:1], in_=v_[:], axis=AX.X, op=ALU.add), reads=[vsb[vi]], writes=[stb])
            sc.op("dve", lambda: nc.vector.tensor_scalar(out=st[:, 1:2], in0=st[:, 0:1], scalar1=float(-1.0 / D), scalar2=None,
                                                         op0=ALU.mult), reads=[stb], writes=[stb])
            sc.op("dve", lambda: nc.vector.tensor_scalar(out=v_[:], in0=v_[:], scalar1=st[:, 1:2], scalar2=None, op0=ALU.add),
                  reads=[vsb[vi], stb], writes=[vsb[vi]])
            sc.op("act", lambda: nc.scalar.activation(out=tmp[:, 0:2, :].rearrange("p a b -> p (a b)"), in_=v_[:], func=AF.Square),
                  reads=[vsb[vi]], writes=[tmpb])
            sc.op("dve", lambda: nc.vector.tensor_reduce(out=st[:, 2:3], in_=tmp[:, 0:2, :].rearrange("p a b -> p (a b)"), axis=AX.X,
                                                         op=ALU.add), reads=[tmpb], writes=[stb])
            sc.op("act", lambda: nc.scalar.activation(out=st[:, 3:4], in_=st[:, 2:3], func=AF.Sqrt, bias=1e-5, scale=1.0 / D),
                  reads=[stb], writes=[stb])
            sc.op("dve", lambda: nc.vector.reciprocal(out=st[:, 4:5], in_=st[:, 3:4]), reads=[stb], writes=[stb])
            sc.op("dve", lambda: nc.vector.scalar_tensor_tensor(out=v_[:], in0=v_[:], scalar=st[:, 4:5], in1=lng_t[:],
                                                                op0=ALU.mult, op1=ALU.mult), reads=[vsb[vi], stb, lng_b], writes=[vsb[vi]])
            sc.op("pool", lambda: nc.gpsimd.tensor_tensor(out=vn[:, ci, :], in0=v_[:], in1=lnb_t[:], op=ALU.add),
                  reads=[vsb[vi], lnb_b], writes=[vnb])
        for g in range(8):
            bc_ = 4 + tl.rot("pc", 2)
            for ci in range(nch):
                sc.op("pe", lambda: nc.tensor.matmul(ps[:, bc_, ci * 128:(ci + 1) * 128], lhsT=vn[:, ci, g * 128:(g + 1) * 128],
                                                     rhs=ws_t[:, g, :], start=True, stop=True), reads=[vnb, ws_b], writes=[tl.pb[bc_]])
            ti = tl.rot("ts", 2)
            oi = tl.rot("ob", 3)
            sc.op("dve", lambda: nc.vector.tensor_tensor(out=ts_[ti][:, :w].rearrange("p (c q) -> p c q", q=128),
                                                         in0=ps[:, bc_, :w].rearrange("p (c q) -> p c q", q=128),
                                                         in1=bs_t[:, g, :].unsqueeze(1).to_broadcast([128, nch, 128]), op=ALU.add),
                  reads=[tl.pb[bc_], bs_b], writes=[tsb[ti]])
            sc.op("pool", lambda: nc.gpsimd.tensor_tensor(out=ob[oi][:, :w], in0=ts_[ti][:, :w], in1=uT[:, g, :w], op=ALU.mult),
                  reads=[tsb[ti], uTb], writes=[obb[oi]])
            sc.dma("sp", Ov[:, g, t0:t0 + w], ob[oi][:, :w], reads=[obb[oi]])
    sc.finish(obb)
    return nc


def run_gmlp(xl_fm, xc_fm, mod_l, g1, inp):
    nc = build_gmlp(2048, 128)
    w_sT = np.ascontiguousarray(inp["gmlp_w_s"][0].transpose(2, 0, 1))
    b_s = np.ascontiguousarray(np.broadcast_to(inp["gmlp_b_s"][0][None], (128, 8, 128)))
    maps = []
    for r in range(NCORES):
        xs = np.concatenate([xl_fm[:, r * 2048:(r + 1) * 2048], xc_fm[:, (r % 2) * 128:(r % 2 + 1) * 128]], axis=1)
        maps.append({"xT": np.ascontiguousarray(xs), "w_in": np.ascontiguousarray(inp["gmlp_w_in"][0]),
                     "mod": np.ascontiguousarray(mod_l), "g1": vec_pc(g1),
                     "ln_g": np.ascontiguousarray(np.broadcast_to(inp["gmlp_ln_g"][0][None], (128, D))),
                     "ln_b": np.ascontiguousarray(np.broadcast_to(inp["gmlp_ln_b"][0][None], (128, D))),
                     "w_sT": w_sT, "b_s": b_s})
    res = run_bass_kernel_spmd(nc, maps, core_ids=list(range(NCORES)))
    os_ = [res.results[r]["oT"] for r in range(NCORES)]
    o_lat = np.concatenate([o[:, :2048] for o in os_], axis=1)
    o_ctx = np.concatenate([os_[0][:, 2048:], os_[1][:, 2048:]], axis=1)
    return o_lat, o_ctx


RW_TW = 256


def build_rwkv_pre():
    nc = bass.Bass("TRN2", target_bir_lowering=False)
    TX = 2050 + 34
    TO = 2048 + 32
    Xd = nc.dram_tensor("xT", [D, TX], F32, kind="ExternalInput").ap()
    Md = nc.dram_tensor("mod", [128, 48, 2], F32, kind="ExternalInput").ap()
    Gd = nc.dram_tensor("g1n", [128, DC], F32, kind="ExternalInput").ap()
    Ed = nc.dram_tensor("edge", [128, 4], F32, kind="ExternalInput").ap()
    MXd = nc.dram_tensor("mix", [128, 6, DC], F32, kind="ExternalInput").ap()
    Wd = nc.dram_tensor("w_rkv", [3, D, D], F32, kind="ExternalInput").ap()
    W0d = nc.dram_tensor("w0", [128, 2, DC], F32, kind="ExternalInput").ap()
    W1d = nc.dram_tensor("w1", [2, D, 64], F32, kind="ExternalInput").ap()
    W2d = nc.dram_tensor("w2", [2, 64, D], F32, kind="ExternalInput").ap()
    A0d = nc.dram_tensor("a0", [128, 2, DC], F32, kind="ExternalInput").ap()
    A1d = nc.dram_tensor("a1", [2, D, 64], F32, kind="ExternalInput").ap()
    A2d = nc.dram_tensor("a2", [2, 64, D], F32, kind="ExternalInput").ap()
    G1d = nc.dram_tensor("gw1", [D, 160], F32, kind="ExternalInput").ap()
    G2d = nc.dram_tensor("gw2", [160, D], F32, kind="ExternalInput").ap()
    KKd = nc.dram_tensor("k_k", [128, DC], F32, kind="ExternalInput").ap()
    KAd = nc.dram_tensor("k_a", [128, DC], F32, kind="ExternalInput").ap()
    BOd = nc.dram_tensor("blk", [128, 128], F32, kind="ExternalInput").ap()
    Od = nc.dram_tensor("out", [10, D, TO], F32, kind="ExternalOutput").ap()
    sc = Sched(nc)
    tl = TL(nc, sc)
    ps = tl.ps
    mod_t, mod_b = tl.load_vec("mod_sb", Md, [128, 48, 2])
    g1_t, g1_b = tl.load_vec("g1_sb", Gd, [128, DC])
    ed_t, ed_b = tl.load_vec("ed_sb", Ed, [128, 4])
    mx_t, mx_b = tl.load_vec("mx_sb", MXd, [128, 6, DC])
    w0_t, w0_b = tl.load_vec("w0_sb", W0d, [128, 2, DC])
    a0_t, a0_b = tl.load_vec("a0_sb", A0d, [128, 2, DC])
    kk_t, kk_b = tl.load_vec("kk_sb", KKd, [128, DC])
    ka_t, ka_b = tl.load_vec("ka_sb", KAd, [128, DC])
    blk_t, blk_b = tl.load_vec("blk_sb", BOd, [128, 128])
    omk_t = nc.alloc_sbuf_tensor("omk_sb", [128, DC], F32)
    omk_b = sc.buf("omk")
    sc.op("dve", lambda: nc.vector.tensor_scalar(out=omk_t[:], in0=ka_t[:], scalar1=-1.0, scalar2=1.0, op0=ALU.mult, op1=ALU.add),
          reads=[ka_b], writes=[omk_b])
    geff, geff_b, shift = emit_geff(tl, "geff1", mod_t, mod_b, g1_t, g1_b, 1, 0)
    wr = []
    for i in range(3):
        wr.append(tl.load_w_bf16(f"wrkv{i}", Wd[i].rearrange("(k p) n -> p k n", p=128), [128, DC, D]))
    w1_t, w1_b = tl.load_w_bf16("w1_sb", W1d.rearrange("z (k p) l -> p z k l", p=128), [128, 2, DC, 64])
    a1_t, a1_b = tl.load_w_bf16("a1_sb", A1d.rearrange("z (k p) l -> p z k l", p=128), [128, 2, DC, 64])
    w2_t, w2_b = tl.load_w_bf16("w2_sb", W2d.rearrange("z l n -> l z n"), [64, 2, D])
    a2_t, a2_b = tl.load_w_bf16("a2_sb", A2d.rearrange("z l n -> l z n"), [64, 2, D])
    gw1_t, gw1_b = tl.load_w_bf16("gw1_sb", G1d.rearrange("(k p) l -> p k l", p=128), [128, DC, 160])
    gw2_t = nc.alloc_sbuf_tensor("gw2_sb", [128, 2, D], BF16)
    gw2_b = sc.buf("gw2")
    sc.dma("pool", gw2_t[:, 0, :], G2d[0:128, :], writes=[gw2_b])
    sc.dma("pool", gw2_t[0:32, 1, :], G2d[128:160, :], writes=[gw2_b])
    TWX = RW_TW + 2
    xt, xb = nc.alloc_sbuf_tensor("x_sb", [128, DC, TWX], F32), sc.buf("x")
    hx, hxb = nc.alloc_sbuf_tensor("hx_sb", [128, DC, TWX], F32), sc.buf("hx")
    tmp, tmpb = nc.alloc_sbuf_tensor("tmp_sb", [128, DC, TWX], F32), sc.buf("tmp")
    sq, sqb = nc.alloc_sbuf_tensor("sq_sb", [128, DC, TWX], BF16), sc.buf("sq")
    rstd, rstdb = nc.alloc_sbuf_tensor("rstd_sb", [128, TWX], F32), sc.buf("rstd")
    ts_, tsb = nc.alloc_sbuf_tensor("ts_sb", [128, DC, RW_TW], F32), sc.buf("ts")
    xm = [nc.alloc_sbuf_tensor(f"xm{j}", [128, DC, RW_TW], BF16) for j in range(6)]
    xmb = sc.bufs("xmb", 6)
    NS = 16
    stg = [nc.alloc_sbuf_tensor(f"stg{i}", [128, RW_TW], F32) for i in range(NS)]
    stgb = sc.bufs("stgb", NS)
    lt = [nc.alloc_sbuf_tensor(f"lt{i}", [128, 2, RW_TW], BF16) for i in range(2)]
    ltb = sc.bufs("ltb", 2)
    gsb, gsbb = nc.alloc_sbuf_tensor("gsb", [128, 2, RW_TW], BF16), sc.buf("gsbb")
    Xv = Xd.rearrange("(c p) t -> p c t", p=128)

    def stage():
        i = tl.rot("stg", NS)
        return stg[i], stgb[i]

    def out_dma(qi, c, o0, w, t_, b_):
        sc.dma("sp", Od[qi, c * 128:(c + 1) * 128, o0:o0 + w], t_[:, :w], reads=[b_])

    def proj(wt, wb_, xj, mc, w, bank):
        for k in range(DC):
            sc.op("pe", lambda: nc.tensor.matmul(ps[:, bank, :w], lhsT=wt[:, k, mc * 128:(mc + 1) * 128], rhs=xm[xj][:, k, :w],
                                                 start=(k == 0), stop=(k == DC - 1)), reads=[wb_, xmb[xj]], writes=[tl.pb[bank]])

    tiles = [(k * RW_TW, k * RW_TW, RW_TW, 0) for k in range(2048 // RW_TW)] + [(2050, 2048, 32, 1)]
    for ti, (x0, o0, w, ty) in enumerate(tiles):
        wx = w + 2
        sc.dma("sp", xt[:, :, :wx], Xv[:, :, x0:x0 + wx], writes=[xb])
        emit_norm_mod_multi(tl, xt[:, :, :wx], [xb], wx, geff, shift, geff_b, mod_b, ty, hx[:, :, :wx], hxb,
                            tmp, tmpb, sq, sqb, rstd, rstdb)
        edges = []
        if ty == 1:
            edges = [(0, 2), (wx - 1, 3)]
        elif ti == 0:
            edges = [(0, 0)]
        elif ti == 2048 // RW_TW - 1:
            edges = [(wx - 1, 1)]
        for (colx, ei) in edges:
            sc.op("dve", lambda: nc.vector.tensor_scalar(out=hx[:, :, colx:colx + 1], in0=hx[:, :, colx:colx + 1],
                                                         scalar1=ed_t[:, ei:ei + 1], scalar2=None, op0=ALU.mult),
                  reads=[hxb, ed_b], writes=[hxb])
        hc = hx[:, :, 1:1 + w]
        sc.op("dve", lambda: nc.vector.tensor_tensor(out=ts_[:, :, :w], in0=hx[:, :, 0:w], in1=hx[:, :, 2:2 + w], op=ALU.add),
              reads=[hxb], writes=[tsb])
        sc.op("dve", lambda: nc.vector.scalar_tensor_tensor(out=ts_[:, :, :w], in0=ts_[:, :, :w], scalar=0.5, in1=hc,
                                                            op0=ALU.mult, op1=ALU.subtract), reads=[tsb, hxb], writes=[tsb])
        for j in range(6):
            for c in range(DC):
                eng = "dve"
                e_ = nc.vector
                sc.op(eng, lambda: e_.scalar_tensor_tensor(out=xm[j][:, c, :w], in0=ts_[:, c, :w], scalar=mx_t[:, j, c:c + 1],
                                                           in1=hx[:, c, 1:1 + w], op0=ALU.mult, op1=ALU.add),
                      reads=[tsb, hxb, mx_b], writes=[xmb[j]])
        for z in range(2):
            bk = tl.rot("pl", 2)
            for k in range(DC):
                sc.op("pe", lambda: nc.tensor.matmul(ps[:64, bk, :w], lhsT=w1_t[:, z, k, :], rhs=xm[1][:, k, :w],
                                                     start=(k == 0), stop=(k == DC - 1)), reads=[w1_b, xmb[1]], writes=[tl.pb[bk]])
            sc.op("act", lambda: nc.scalar.activation(out=lt[0][:64, z, :w], in_=ps[:64, bk, :w], func=AF.Tanh),
                  reads=[tl.pb[bk]], writes=[ltb[0]])
            bk = tl.rot("pl", 2)
            for k in range(DC):
                sc.op("pe", lambda: nc.tensor.matmul(ps[:64, bk, :w], lhsT=a1_t[:, z, k, :], rhs=xm[4][:, k, :w],
                                                     start=(k == 0), stop=(k == DC - 1)), reads=[a1_b, xmb[4]], writes=[tl.pb[bk]])
            sc.op("act", lambda: nc.scalar.copy(out=lt[1][:64, z, :w], in_=ps[:64, bk, :w]), reads=[tl.pb[bk]], writes=[ltb[1]])
        for (m0, mw, gi) in ((0, 128, 0), (128, 32, 1)):
            bk = tl.rot("pl", 2)
            for k in range(DC):
                sc.op("pe", lambda: nc.tensor.matmul(ps[:mw, bk, :w], lhsT=gw1_t[:, k, m0:m0 + mw], rhs=xm[5][:, k, :w],
                                                     start=(k == 0), stop=(k == DC - 1)), reads=[gw1_b, xmb[5]], writes=[tl.pb[bk]])
            sc.op("act", lambda: nc.scalar.activation(out=gsb[:mw, gi, :w], in_=ps[:mw, bk, :w], func=AF.Sigmoid),
                  reads=[tl.pb[bk]], writes=[gsbb])
        for c in range(DC):
            bk = 2 + tl.rot("pm", 4)
            proj(wr[0][0], wr[0][1], 0, c, w, bk)
            s_, sb_ = stage()
            sc.op("act", lambda: nc.scalar.copy(out=s_[:, :w], in_=ps[:, bk, :w]), reads=[tl.pb[bk]], writes=[sb_])
            out_dma(0, c, o0, w, s_, sb_)
            bk = 2 + tl.rot("pm", 4)
            proj(wr[2][0], wr[2][1], 3, c, w, bk)
            s_, sb_ = stage()
            sc.op("act", lambda: nc.scalar.copy(out=s_[:, :w], in_=ps[:, bk, :w]), reads=[tl.pb[bk]], writes=[sb_])
            out_dma(1, c, o0, w, s_, sb_)
            bk = 2 + tl.rot("pm", 4)
            sc.op("pe", lambda: nc.tensor.matmul(ps[:, bk, :w], lhsT=gw2_t[:, 0, c * 128:(c + 1) * 128], rhs=gsb[:, 0, :w],
                                                 start=True, stop=False), reads=[gw2_b, gsbb], writes=[tl.pb[bk]])
            sc.op("pe", lambda: nc.tensor.matmul(ps[:, bk, :w], lhsT=gw2_t[0:32, 1, c * 128:(c + 1) * 128], rhs=gsb[0:32, 1, :w],
                                                 start=False, stop=True), reads=[gw2_b, gsbb], writes=[tl.pb[bk]])
            s_, sb_ = stage()
            sc.op("act", lambda: nc.scalar.copy(out=s_[:, :w], in_=ps[:, bk, :w]), reads=[tl.pb[bk]], writes=[sb_])
            out_dma(9, c, o0, w, s_, sb_)
            bk = 2 + tl.rot("pm", 4)
            proj(wr[1][0], wr[1][1], 2, c, w, bk)
            k_s, k_b = stage()
            sc.op("act", lambda: nc.scalar.copy(out=k_s[:, :w], in_=ps[:, bk, :w]), reads=[tl.pb[bk]], writes=[k_b])
            kk_s, kk_sb = stage()
            sc.op("dve", lambda: nc.vector.tensor_scalar(out=kk_s[:, :w], in0=k_s[:, :w], scalar1=kk_t[:, c:c + 1], scalar2=None,
                                                         op0=ALU.mult), reads=[k_b, kk_b], writes=[kk_sb])
            q_s, q_b = stage()
            sc.op("pool", lambda: nc.gpsimd.tensor_tensor(out=q_s[:, :w], in0=kk_s[:, :w], in1=kk_s[:, :w], op=ALU.mult),
                  reads=[kk_sb], writes=[q_b])
            bk2 = tl.rot("pl", 2)
            sc.op("pe", lambda: nc.tensor.matmul(ps[:, bk2, :w], lhsT=blk_t[:], rhs=q_s[:, :w], start=True, stop=True),
                  reads=[blk_b, q_b], writes=[tl.pb[bk2]])
            n_s, n_b = stage()
            sc.op("act", lambda: nc.scalar.activation(out=n_s[:, :w], in_=ps[:, bk2, :w], func=AF.Sqrt), reads=[tl.pb[bk2]], writes=[n_b])
            sc.op("dve", lambda: nc.vector.tensor_scalar(out=n_s[:, :w], in0=n_s[:, :w], scalar1=1e-12, scalar2=None, op0=ALU.max),
                  reads=[n_b], writes=[n_b])
            sc.op("dve", lambda: nc.vector.reciprocal(out=n_s[:, :w], in_=n_s[:, :w]), reads=[n_b], writes=[n_b])
            kap_s, kap_b = stage()
            sc.op("dve", lambda: nc.vector.tensor_tensor(out=kap_s[:, :w], in0=kk_s[:, :w], in1=n_s[:, :w], op=ALU.mult),
                  reads=[kk_sb, n_b], writes=[kap_b])
            out_dma(2, c, o0, w, kap_s, kap_b)
            for z in range(2):
                bk = 2 + tl.rot("pm", 4)
                sc.op("pe", lambda: nc.tensor.matmul(ps[:, bk, :w], lhsT=a2_t[:, z, c * 128:(c + 1) * 128], rhs=lt[1][:64, z, :w],
                                                     start=True, stop=True), reads=[a2_b, ltb[1]], writes=[tl.pb[bk]])
                a_s, a_b = stage()
                sc.op("act", lambda: nc.scalar.activation(out=a_s[:, :w], in_=ps[:, bk, :w], func=AF.Sigmoid,
                                                          bias=a0_t[:, z, c:c + 1], scale=1.0), reads=[tl.pb[bk], a0_b], writes=[a_b])
                t_s, t_b = stage()
                sc.op("dve", lambda: nc.vector.tensor_scalar(out=t_s[:, :w], in0=a_s[:, :w], scalar1=ka_t[:, c:c + 1],
                                                             scalar2=omk_t[:, c:c + 1], op0=ALU.mult, op1=ALU.add),
                      reads=[a_b, ka_b, omk_b], writes=[t_b])
                sc.op("pool", lambda: nc.gpsimd.tensor_tensor(out=t_s[:, :w], in0=t_s[:, :w], in1=k_s[:, :w], op=ALU.mult),
                      reads=[t_b, k_b], writes=[t_b])
                out_dma(3 + z, c, o0, w, t_s, t_b)
                sc.op("pool", lambda: nc.gpsimd.tensor_tensor(out=a_s[:, :w], in0=a_s[:, :w], in1=kap_s[:, :w], op=ALU.mult),
                      reads=[a_b, kap_b], writes=[a_b])
                out_dma(7 + z, c, o0, w, a_s, a_b)
                bk = 2 + tl.rot("pm", 4)
                sc.op("pe", lambda: nc.tensor.matmul(ps[:, bk, :w], lhsT=w2_t[:, z, c * 128:(c + 1) * 128], rhs=lt[0][:64, z, :w],
                                                     start=True, stop=True), reads=[w2_b, ltb[0]], writes=[tl.pb[bk]])
                l_s, l_b = stage()
                sc.op("act", lambda: nc.scalar.activation(out=l_s[:, :w], in_=ps[:, bk, :w], func=AF.Sigmoid,
                                                          bias=w0_t[:, z, c:c + 1], scale=1.0), reads=[tl.pb[bk], w0_b], writes=[l_b])
                sc.op("dve", lambda: nc.vector.tensor_scalar(out=l_s[:, :w], in0=l_s[:, :w], scalar1=float(-math.exp(-0.5)),
                                                             scalar2=None, op0=ALU.mult), reads=[l_b], writes=[l_b])
                out_dma(5 + z, c, o0, w, l_s, l_b)
    sc.finish(stgb)
    return nc


def blockones():
    b = np.zeros((128, 128), np.float32)
    b[:64, :64] = 1.0
    b[64:, 64:] = 1.0
    return b


def vec_pzc(v):
    return np.ascontiguousarray(v.reshape(v.shape[0], DC, 128).transpose(2, 0, 1))


def run_rwkv_pre(xl_fm, xc_fm, mod_l, g1, inp):
    nc = build_rwkv_pre()
    zl = np.zeros((D, 1), np.float32)
    maps = []
    common = {"mod": np.ascontiguousarray(mod_l), "g1n": vec_pc(g1), "mix": vec_pzc(inp["rwkv_mix"][0]),
              "w_rkv": np.ascontiguousarray(inp["rwkv_w_rkv"][0]), "w0": vec_pzc(inp["rwkv_w0"][0]),
              "w1": np.ascontiguousarray(inp["rwkv_w1"][0]), "w2": np.ascontiguousarray(inp["rwkv_w2"][0]),
              "a0": vec_pzc(inp["rwkv_a0"][0]), "a1": np.ascontiguousarray(inp["rwkv_a1"][0]),
              "a2": np.ascontiguousarray(inp["rwkv_a2"][0]), "gw1": np.ascontiguousarray(inp["rwkv_g1"][0]),
              "gw2": np.ascontiguousarray(inp["rwkv_g2"][0]), "k_k": vec_pc(inp["rwkv_k_k"][0]),
              "k_a": vec_pc(inp["rwkv_k_a"][0]), "blk": blockones()}
    xlp = np.concatenate([zl, xl_fm, zl], axis=1)
    xcp = np.concatenate([zl, xc_fm, zl], axis=1)
    for r in range(NCORES):
        xs = np.concatenate([xlp[:, r * 2048:r * 2048 + 2050], xcp[:, r * 32:r * 32 + 34]], axis=1)
        edge = np.ones((128, 4), np.float32)
        if r == 0:
            edge[:, 0] = 0.0
            edge[:, 2] = 0.0
        if r == NCORES - 1:
            edge[:, 1] = 0.0
            edge[:, 3] = 0.0
        m = dict(common)
        m["xT"] = np.ascontiguousarray(xs)
        m["edge"] = edge
        maps.append(m)
    res = run_bass_kernel_spmd(nc, maps, core_ids=list(range(NCORES)))
    outs = [res.results[r]["out"] for r in range(NCORES)]
    lat = np.concatenate([o[:, :, :2048] for o in outs], axis=2)
    ctx = np.concatenate([o[:, :, 2048:] for o in outs], axis=2)
    return lat, ctx


RW_N = NCTX + SEQ
RW_NCH = RW_N // 64
SEG = 8


def build_rwkv_scan(n_pairs=4, n_chunks=RW_NCH):
    import itertools
    nc = bass.Bass("TRN2", target_bir_lowering=False)
    N = n_chunks * 64
    Fd = nc.dram_tensor("F", [n_pairs, 4, 128, N], F32, kind="ExternalInput").ap()
    Td = nc.dram_tensor("Tm", [n_pairs, 4, 128, n_chunks, 64], F32, kind="ExternalInput").ap()
    Cd = nc.dram_tensor("C", [128, 4, 64], F32, kind="ExternalInput").ap()
    Yd = nc.dram_tensor("Y", [n_pairs, 2, N, 64], F32, kind="ExternalOutput").ap()
    sc = Sched(nc)
    ps = nc.alloc_psum_tensor("ps", [128, 8, 512], F32)
    pb = sc.bufs("psb", 8)
    zb, ub, hb_ = sc.buf("zps"), sc.buf("ups"), sc.buf("hps")
    ct = nc.alloc_sbuf_tensor("ct", [128, 4, 64], F32)
    ctb = nc.alloc_sbuf_tensor("ctb", [128, 4, 64], BF16)
    cb = sc.buf("c")
    sc.dma("sp", ct[:], Cd, writes=[cb])
    sc.op("dve", lambda: nc.vector.tensor_copy(out=ctb[:], in_=ct[:]), reads=[cb], writes=[cb])
    TRI, TRS, TRST, IDN = ct[:, 0, :], ct[:, 1, :], ct[:, 2, :], ct[:, 3, :]
    TRIb, TRSb, IDNb = ctb[:, 0, :], ctb[:, 1, :], ctb[:, 3, :]

    def T_(name, shape, dt=F32):
        return nc.alloc_sbuf_tensor(name, shape, dt)

    S = []
    for i in range(2):
        d = dict(
            fm=T_(f"fm{i}", [128, 4, SEG * 64]), fmb=sc.buf(f"fm{i}"),
            tm=T_(f"tm{i}", [128, 4, SEG, 64]), tmb=sc.buf(f"tm{i}"),
            ein=T_(f"ein{i}", [128, SEG * 64]), einb=sc.buf(f"ein{i}"),
            eex=T_(f"eex{i}", [128, SEG * 64]), eexb=sc.buf(f"eex{i}"),
            eng=T_(f"eng{i}", [128, SEG * 64]), engb=sc.buf(f"eng{i}"),
            ent=T_(f"ent{i}", [128, SEG * 64]), entb=sc.buf(f"ent{i}"),
            ar=T_(f"ar{i}", [128, SEG, 128], BF16), arb=sc.buf(f"ar{i}"),
            bt=T_(f"bt{i}", [128, SEG * 64], BF16), btb=sc.buf(f"bt{i}"),
            kt=T_(f"kt{i}", [128, SEG * 64], BF16), ktb=sc.buf(f"kt{i}"),
            btm=T_(f"btm{i}", [128, SEG, 64], BF16), btmb=sc.buf(f"btm{i}"),
            ktm=T_(f"ktm{i}", [128, SEG, 64], BF16), ktmb=sc.buf(f"ktm{i}"),
            x=[T_(f"x{i}_{q}", [128, SEG, 64], BF16) for q in range(2)], xb=sc.bufs(f"x{i}_", 2),
            xt=[T_(f"xt{i}_{q}", [128, SEG, 64], BF16) for q in range(2)], xtb=sc.bufs(f"xt{i}_", 2),
            tt=T_(f"tt{i}", [128, SEG, 64], BF16), ttb=sc.buf(f"tt{i}"),
            qbt=T_(f"qbt{i}", [128, SEG, 64], BF16), qbtb=sc.buf(f"qbt{i}"),
            aak=T_(f"aak{i}", [128, SEG, 64], BF16), aakb=sc.buf(f"aak{i}"),
            qkt=T_(f"qkt{i}", [128, SEG, 64], BF16), qktb=sc.buf(f"qkt{i}"),
            ysb=T_(f"ysb{i}", [128, SEG * 64]), ysbb=sc.buf(f"ysb{i}"),
            lwb=T_(f"lwb{i}", [128, SEG, 64], BF16), lwbb=sc.buf(f"lwb{i}"),
            vb=T_(f"vb{i}", [128, SEG, 64], BF16), vbb=sc.buf(f"vb{i}"),
        )
        S.append(d)
    Hs = [T_(f"H{i}", [128, 64], BF16) for i in range(3)]
    Hb = sc.bufs("H", 3)
    Zs = [T_(f"Z{i}", [128, 64], BF16) for i in range(2)]
    Zb = sc.bufs("Z", 2)
    Us = [T_(f"U{i}", [128, 64], BF16) for i in range(2)]
    Ub = sc.bufs("U", 2)
    cnt = {}

    def rot(key, n):
        v = cnt.get(key, 0)
        cnt[key] = v + 1
        return v % n

    import os
    PRELOAD = False
    REP = 2
    ISL = int(os.environ.get("SCAN_ISL", "48"))

    def mm(out, lhsT, rhs, start, stop, reads, writes, pre_=False):
        for l in range(2):
            sl = slice(l * 64, (l + 1) * 64)
            sc.op("pe", lambda: nc.tensor.matmul(out[sl], lhsT=lhsT[sl], rhs=rhs[sl], start=start, stop=stop),
                  reads=reads, writes=writes)

    def bc(ap, n):
        return ap.unsqueeze(1).to_broadcast([128, n, 64])

    def pre(pair, g, d):
        c0 = g * SEG
        n = min(SEG, n_chunks - c0)
        w = n * 64
        sc.dma("sp", d["fm"][:, :, :w], Fd[pair, :, :, c0 * 64:c0 * 64 + w].rearrange("q j t -> j q t"), writes=[d["fmb"]])
        sc.dma("sp", d["tm"][:, :, :n, :], Td[pair, :, :, c0:c0 + n, :].rearrange("q s c j -> s q c j"), writes=[d["tmb"]])
        yield
        fm, tm = d["fm"], d["tm"]
        sc.op("act", lambda: nc.scalar.copy(out=d["lwb"][:, :n, :], in_=tm[:, 0, :n, :]), reads=[d["tmb"]], writes=[d["lwbb"]])
        sc.op("act", lambda: nc.scalar.copy(out=d["vb"][:, :n, :], in_=tm[:, 3, :n, :]), reads=[d["tmb"]], writes=[d["vbb"]])
        yield
        for c in range(n):
            mm(ps[:, 0, c * 64:(c + 1) * 64], d["lwb"][:, c, :], TRIb, True, True, [d["lwbb"], cb], [pb[0]])
            mm(ps[:, 1, c * 64:(c + 1) * 64], d["lwb"][:, c, :], TRSb, True, True, [d["lwbb"], cb], [pb[1]])
            yield
        mm(ps[:, 2, :w], TRIb, d["lwb"][:, :n, :].rearrange("p c j -> p (c j)"), True, True, [d["lwbb"], cb], [pb[2]])
        sc.op("act", lambda: nc.scalar.activation(out=d["ein"][:, :w], in_=ps[:, 0, :w], func=AF.Exp), reads=[pb[0]], writes=[d["einb"]])
        sc.op("act", lambda: nc.scalar.activation(out=d["eng"][:, :w], in_=ps[:, 0, :w], func=AF.Exp, scale=-1.0),
              reads=[pb[0]], writes=[d["engb"]])
        yield
        sc.op("act", lambda: nc.scalar.activation(out=d["eex"][:, :w], in_=ps[:, 1, :w], func=AF.Exp), reads=[pb[1]], writes=[d["eexb"]])
        sc.op("act", lambda: nc.scalar.activation(out=d["ent"][:, :w], in_=ps[:, 2, :w], func=AF.Exp, scale=-1.0),
              reads=[pb[2]], writes=[d["entb"]])
        yield
        v3 = lambda t_: t_[:, :w].rearrange("p (c t) -> p c t", t=64)
        sc.op("dve", lambda: nc.vector.scalar_tensor_tensor(out=d["ar"][:, :n, 0:64], in0=v3(fm[:, 2, :]), scalar=-1.0, in1=v3(d["eex"]),
                                                            op0=ALU.mult, op1=ALU.mult), reads=[d["fmb"], d["eexb"]], writes=[d["arb"]])
        sc.op("dve", lambda: nc.vector.tensor_tensor(out=d["ar"][:, :n, 64:128], in0=v3(fm[:, 0, :]), in1=v3(d["ein"]), op=ALU.mult),
              reads=[d["fmb"], d["einb"]], writes=[d["arb"]])
        yield
        sc.op("pool", lambda: nc.gpsimd.tensor_tensor(out=d["bt"][:, :w], in0=fm[:, 3, :w], in1=d["eng"][:, :w], op=ALU.mult),
              reads=[d["fmb"], d["engb"]], writes=[d["btb"]])
        sc.op("pool", lambda: nc.gpsimd.tensor_tensor(out=d["kt"][:, :w], in0=fm[:, 1, :w], in1=d["eng"][:, :w], op=ALU.mult),
              reads=[d["fmb"], d["engb"]], writes=[d["ktb"]])
        yield
        sc.op("pool", lambda: nc.gpsimd.tensor_tensor(out=d["btm"][:, :n, :], in0=tm[:, 2, :n, :], in1=v3(d["ent"]).rearrange("p c t -> p c t"),
                                                      op=ALU.mult), reads=[d["tmb"], d["entb"]], writes=[d["btmb"]])
        sc.op("pool", lambda: nc.gpsimd.tensor_tensor(out=d["ktm"][:, :n, :], in0=tm[:, 1, :n, :], in1=v3(d["ent"]), op=ALU.mult),
              reads=[d["tmb"], d["entb"]], writes=[d["ktmb"]])
        yield
        for c in range(n):
            bk, off = c // 4, (c % 4) * 128
            mm(ps[:, 0 + bk, off:off + 128], d["bt"][:, c * 64:(c + 1) * 64], d["ar"][:, c, :], True, True, [d["btb"], d["arb"]], [pb[0 + bk]])
            mm(ps[:, 2 + bk, off:off + 128], d["kt"][:, c * 64:(c + 1) * 64], d["ar"][:, c, :], True, True, [d["ktb"], d["arb"]], [pb[2 + bk]])
            mm(ps[:, 4, c * 64:(c + 1) * 64], d["ar"][:, c, 0:64], d["bt"][:, c * 64:(c + 1) * 64], True, True, [d["btb"], d["arb"]], [pb[4]])
            yield
        nb = (n + 3) // 4
        for bk in range(nb):
            cc = min(4, n - bk * 4)
            o1 = ps[:, 0 + bk, :cc * 128].rearrange("p (c x) -> p c x", x=128)
            o2 = ps[:, 2 + bk, :cc * 128].rearrange("p (c x) -> p c x", x=128)
            sl = slice(bk * 4, bk * 4 + cc)
            sc.op("dve", lambda: nc.vector.tensor_tensor(out=d["x"][0][:, sl, :], in0=o1[:, :, 0:64], in1=bc(TRS, cc), op=ALU.mult),
                  reads=[pb[0 + bk], cb], writes=[d["xb"][0]])
            sc.op("dve", lambda: nc.vector.tensor_tensor(out=d["qbt"][:, sl, :], in0=o1[:, :, 64:128], in1=bc(TRI, cc), op=ALU.mult),
                  reads=[pb[0 + bk], cb], writes=[d["qbtb"]])
            yield
            sc.op("dve", lambda: nc.vector.tensor_tensor(out=d["aak"][:, sl, :], in0=o2[:, :, 0:64], in1=bc(TRS, cc), op=ALU.mult),
                  reads=[pb[2 + bk], cb], writes=[d["aakb"]])
            sc.op("dve", lambda: nc.vector.tensor_tensor(out=d["qkt"][:, sl, :], in0=o2[:, :, 64:128], in1=bc(TRI, cc), op=ALU.mult),
                  reads=[pb[2 + bk], cb], writes=[d["qktb"]])
            yield
        p4 = ps[:, 4, :w].rearrange("p (c t) -> p c t", t=64)
        sc.op("dve", lambda: nc.vector.tensor_tensor(out=d["xt"][0][:, :n, :], in0=p4, in1=bc(TRST, n), op=ALU.mult),
              reads=[pb[4], cb], writes=[d["xtb"][0]])
        sc.op("pool", lambda: nc.gpsimd.tensor_tensor(out=d["tt"][:, :n, :], in0=d["x"][0][:, :n, :], in1=bc(IDN, n), op=ALU.add),
              reads=[d["xb"][0], cb], writes=[d["ttb"]])
        yield
        cur = 0
        for lvl in range(1, 6):
            nxt = 1 - cur
            X, XT = d["x"][cur], d["xt"][cur]
            if lvl < 5:
                for c in range(n):
                    mm(ps[:, 0, c * 64:(c + 1) * 64], XT[:, c, :], X[:, c, :], True, True, [d["xb"][cur], d["xtb"][cur]], [pb[0]])
                    yield
            for c in range(n):
                mm(ps[:, 1, c * 64:(c + 1) * 64], X[:, c, :], XT[:, c, :], True, True, [d["xb"][cur], d["xtb"][cur]], [pb[1]])
                yield
            if lvl < 5:
                sc.op("act", lambda: nc.scalar.copy(out=d["x"][nxt][:, :n, :], in_=ps[:, 0, :w].rearrange("p (c t) -> p c t", t=64)),
                      reads=[pb[0]], writes=[d["xb"][nxt]])
            sc.op("dve", lambda: nc.vector.tensor_copy(out=d["xt"][nxt][:, :n, :], in_=ps[:, 1, :w].rearrange("p (c t) -> p c t", t=64)),
                  reads=[pb[1]], writes=[d["xtb"][nxt]])
            yield
            for c in range(n):
                mm(ps[:, 2, c * 64:(c + 1) * 64], d["xt"][nxt][:, c, :], d["tt"][:, c, :], True, True, [d["xtb"][nxt], d["ttb"]], [pb[2]])
                yield
            sc.op("dve", lambda: nc.vector.tensor_tensor(out=d["tt"][:, :n, :], in0=ps[:, 2, :w].rearrange("p (c t) -> p c t", t=64),
                                                         in1=d["tt"][:, :n, :], op=ALU.add), reads=[pb[2], d["ttb"]], writes=[d["ttb"]])
            yield
            cur = nxt

    n_seg = (n_chunks + SEG - 1) // SEG
    work = [(p_, g) for p_ in range(n_pairs) for g in range(n_seg)]
    hc = 0
    import os
    DBG = os.environ.get("SCAN_DBG", "")
    gen = pre(work[0][0], work[0][1], S[0])
    for _ in gen:
        pass
    if "b" in DBG:
        sc.barrier()
    for wi, (pair, g) in enumerate(work):
        d = S[wi % 2]
        gen = pre(work[wi + 1][0], work[wi + 1][1], S[(wi + 1) % 2]) if wi + 1 < len(work) else iter(())
        c0 = g * SEG
        n = min(SEG, n_chunks - c0)
        w = n * 64
        if g == 0:
            hc = rot("H", 3)
            sc.op("pool", lambda: nc.gpsimd.memset(Hs[hc][:], 0.0), writes=[Hb[hc]])
        yb = 6
        for c in range(n):
            vtm = d["vb"][:, c, :]
            zi, ui = rot("Z", 2), rot("U", 2)
            hn = rot("H", 3)
            mm(ps[:, 7, 0:64], d["ar"][:, c, 0:64], Hs[hc][:], True, False, [d["arb"], Hb[hc]], [zb])
            mm(ps[:, 7, 0:64], d["aak"][:, c, :], vtm, False, True, [d["aakb"], d["vbb"]], [zb])
            for _rep in range(REP):
                sc.op("act", lambda: nc.scalar.copy(out=Zs[zi][:], in_=ps[:, 7, 0:64]), reads=[zb], writes=[Zb[zi]])
            mm(ps[:, 7, 64:128], d["tt"][:, c, :], Zs[zi][:], True, True, [d["ttb"], Zb[zi]], [ub])
            for _rep in range(REP):
                sc.op("dve", lambda: nc.vector.tensor_copy(out=Us[ui][:], in_=ps[:, 7, 64:128]), reads=[ub], writes=[Ub[ui]])
            mm(ps[:, 7, 128:192], IDNb, Hs[hc][:], True, False, [cb, Hb[hc]], [hb_])
            mm(ps[:, 7, 128:192], d["btm"][:, c, :], Us[ui][:], False, False, [d["btmb"], Ub[ui]], [hb_])
            mm(ps[:, 7, 128:192], d["ktm"][:, c, :], vtm, False, True, [d["ktmb"], d["vbb"]], [hb_])
            for _rep in range(REP):
                sc.op("dve", lambda: nc.vector.tensor_scalar(out=Hs[hn][:], in0=ps[:, 7, 128:192],
                                                             scalar1=d["ein"][:, c * 64 + 63:c * 64 + 64], scalar2=None, op0=ALU.mult),
                      reads=[hb_, d["einb"]], writes=[Hb[hn]])
            mm(ps[:, yb, c * 64:(c + 1) * 64], d["ar"][:, c, 64:128], Hs[hc][:], True, False, [Hb[hc], d["arb"]], [pb[yb]])
            mm(ps[:, yb, c * 64:(c + 1) * 64], d["qbt"][:, c, :], Us[ui][:], False, True, [Ub[ui], d["qbtb"]], [pb[yb]])
            mm(ps[:, 5, c * 64:(c + 1) * 64], d["qkt"][:, c, :], vtm, True, True, [d["vbb"], d["qktb"]], [pb[5]])
            hc = hn
            if "n" not in DBG:
                for _ in itertools.islice(gen, ISL):
                    pass
            if "c" in DBG:
                sc.barrier()
        sc.op("act", lambda: nc.scalar.copy(out=d["ysb"][:, :w], in_=ps[:, 5, :w]), reads=[pb[5]], writes=[d["ysbb"]])
        sc.op("dve", lambda: nc.vector.tensor_tensor(out=d["ysb"][:, :w], in0=ps[:, yb, :w], in1=d["ysb"][:, :w], op=ALU.add),
              reads=[pb[yb], d["ysbb"]], writes=[d["ysbb"]])
        for l in range(2):
            sc.dma("sp", Yd[pair, l, c0 * 64:c0 * 64 + w, :].rearrange("(c t) i -> t c i", t=64),
                   d["ysb"][l * 64:(l + 1) * 64, :w].rearrange("p (c i) -> p c i", i=64), reads=[d["ysbb"]])
        if "b" in DBG:
            sc.barrier()
        for _ in gen:
            pass
        if "b" in DBG:
            sc.barrier()
    sc.finish([S[0]["ysbb"], S[1]["ysbb"]])
    return nc


def scan_consts():
    s = np.arange(64)[:, None]
    t = np.arange(64)[None, :]
    tri = (s <= t).astype(np.float32)
    trs = (s < t).astype(np.float32)
    c = np.stack([tri, trs, trs.T, np.eye(64, dtype=np.float32)], axis=1)
    return np.ascontiguousarray(np.concatenate([c, c], axis=0))


RW_NCH_PAD = ((RW_NCH + SEG - 1) // SEG) * SEG


def run_rwkv_scan(lat, ctx):
    nc = build_rwkv_scan(2, RW_NCH_PAD)
    NP = RW_NCH_PAD * 64
    pad = np.zeros((10, D, NP - RW_N), np.float32)
    full = np.concatenate([ctx, lat, pad], axis=2)
    rev = np.concatenate([ctx[:, :, ::-1], lat[:, :, ::-1], pad], axis=2)
    C = scan_consts()
    maps = []
    for r in range(NCORES):
        Fs, Ts = [], []
        sl = slice(r * 128, (r + 1) * 128)
        for z in range(2):
            src = full if z == 0 else rev
            rr_, vv, kap, kz, lw, bz = src[0, sl], src[1, sl], src[2, sl], src[3 + z, sl], src[5 + z, sl], src[7 + z, sl]
            Fs.append(np.stack([rr_, kz, kap, bz]))
            tmaj = lambda a: a.reshape(2, 64, RW_NCH_PAD, 64).transpose(0, 3, 2, 1).reshape(128, RW_NCH_PAD, 64)
            Ts.append(np.stack([tmaj(lw), tmaj(kz), tmaj(bz), tmaj(vv)]))
        maps.append({"F": np.ascontiguousarray(np.stack(Fs)), "Tm": np.ascontiguousarray(np.stack(Ts)), "C": C})
    res = run_bass_kernel_spmd(nc, maps, core_ids=list(range(NCORES)))
    y0 = np.zeros((D, RW_N), np.float32)
    y1 = np.zeros((D, RW_N), np.float32)
    for r in range(NCORES):
        Y = res.results[r]["Y"]
        for hh in range(2):
            h = 2 * r + hh
            y0[h * 64:(h + 1) * 64] = Y[0, hh, :RW_N].T
            yr = Y[1, hh, :RW_N].T
            y1[h * 64:(h + 1) * 64] = np.concatenate([yr[:, :NCTX][:, ::-1], yr[:, NCTX:][:, ::-1]], axis=1)
    return y0, y1


def build_rwkv_mid(T_lat, T_ctx):
    nc = bass.Bass("TRN2", target_bir_lowering=False)
    T = T_lat + T_ctx
    Id = nc.dram_tensor("inp", [7, D, T], F32, kind="ExternalInput").ap()
    RKd = nc.dram_tensor("r_k", [128, DC], F32, kind="ExternalInput").ap()
    LWd = nc.dram_tensor("ln_w", [128, DC], F32, kind="ExternalInput").ap()
    LBd = nc.dram_tensor("ln_b", [128, DC], F32, kind="ExternalInput").ap()
    BOd = nc.dram_tensor("blk", [128, 128], F32, kind="ExternalInput").ap()
    Od = nc.dram_tensor("oT", [D, T], BF16, kind="ExternalOutput").ap()
    sc = Sched(nc)
    tl = TL(nc, sc)
    ps = tl.ps
    rk_t, rk_b = tl.load_vec("rk_sb", RKd, [128, DC])
    lw_t, lw_b = tl.load_vec("lw_sb", LWd, [128, DC])
    lb_t, lb_b = tl.load_vec("lb_sb", LBd, [128, DC])
    blk_t, blk_b = tl.load_vec("blk_sb", BOd, [128, 128])
    MW = 256
    tiles = [(t0, MW, 0) for t0 in range(0, T_lat, MW)] + ([(T_lat, T_ctx, 1)] if T_ctx else [])
    NI = 2
    it = [[nc.alloc_sbuf_tensor(f"in{q}_{i}", [128, DC, MW], F32) for i in range(NI)] for q in range(7)]
    itb = [[sc.buf(f"in{q}_{i}") for i in range(NI)] for q in range(7)]
    Iv = Id.rearrange("q (c p) t -> q p c t", p=128)
    NS = 8
    stg = [nc.alloc_sbuf_tensor(f"stg{i}", [128, MW], F32) for i in range(NS)]
    stgb = sc.bufs("stgb", NS)
    ob = [nc.alloc_sbuf_tensor(f"ob{i}", [128, MW], BF16) for i in range(2)]
    obb = sc.bufs("obb", 2)

    def stage():
        i = tl.rot("stg", NS)
        return stg[i], stgb[i]

    for (t0, w, ty) in tiles:
        sl = tl.rot("in", NI)
        for q in range(7):
            sc.dma("sp", it[q][sl][:, :, :w], Iv[q, :, :, t0:t0 + w], writes=[itb[q][sl]])
        for c in range(DC):
            X = [(it[q][sl][:, c, :], itb[q][sl]) for q in range(7)]
            (y0, y0b), (y1, y1b), (r_, rb), (k0, k0b), (k1, k1b), (v_, vb_), (g_, gb) = X
            y, yb = stage()
            sc.op("pool", lambda: nc.gpsimd.tensor_tensor(out=y[:, :w], in0=y0[:, :w], in1=y1[:, :w], op=ALU.add),
                  reads=[y0b, y1b], writes=[yb])
            b1 = tl.rot("pa", 2)
            sc.op("pe", lambda: nc.tensor.matmul(ps[:, b1, :w], lhsT=blk_t[:], rhs=y[:, :w], start=True, stop=True),
                  reads=[blk_b, yb], writes=[tl.pb[b1]])
            yc, ycb = stage()
            sc.op("dve", lambda: nc.vector.scalar_tensor_tensor(out=yc[:, :w], in0=ps[:, b1, :w], scalar=float(-1.0 / 64), in1=y[:, :w],
                                                                op0=ALU.mult, op1=ALU.add), reads=[tl.pb[b1], yb], writes=[ycb])
            q2, q2b = stage()
            sc.op("pool", lambda: nc.gpsimd.tensor_tensor(out=q2[:, :w], in0=yc[:, :w], in1=yc[:, :w], op=ALU.mult),
                  reads=[ycb], writes=[q2b])
            b2 = 2 + tl.rot("pb", 2)
            sc.op("pe", lambda: nc.tensor.matmul(ps[:, b2, :w], lhsT=blk_t[:], rhs=q2[:, :w], start=True, stop=True),
                  reads=[blk_b, q2b], writes=[tl.pb[b2]])
            rs, rsb = stage()
            sc.op("act", lambda: nc.scalar.activation(out=rs[:, :w], in_=ps[:, b2, :w], func=AF.Sqrt, bias=64e-5, scale=1.0 / 64),
                  reads=[tl.pb[b2]], writes=[rsb])
            sc.op("dve", lambda: nc.vector.reciprocal(out=rs[:, :w], in_=rs[:, :w]), reads=[rsb], writes=[rsb])
            sc.op("dve", lambda: nc.vector.tensor_tensor(out=yc[:, :w], in0=yc[:, :w], in1=rs[:, :w], op=ALU.mult),
                  reads=[ycb, rsb], writes=[ycb])
            sc.op("act", lambda: nc.scalar.activation(out=yc[:, :w], in_=yc[:, :w], func=AF.Identity, bias=lb_t[:, c:c + 1],
                                                      scale=lw_t[:, c:c + 1]), reads=[ycb, lw_b, lb_b], writes=[ycb])
            kk_, kkb = stage()
            sc.op("pool", lambda: nc.gpsimd.tensor_tensor(out=kk_[:, :w], in0=k0[:, :w], in1=k1[:, :w], op=ALU.add),
                  reads=[k0b, k1b], writes=[kkb])
            sc.op("dve", lambda: nc.vector.scalar_tensor_tensor(out=kk_[:, :w], in0=kk_[:, :w], scalar=rk_t[:, c:c + 1], in1=r_[:, :w],
                                                                op0=ALU.mult, op1=ALU.mult), reads=[kkb, rk_b, rb], writes=[kkb])
            b3 = 4 + tl.rot("pc", 2)
            sc.op("pe", lambda: nc.tensor.matmul(ps[:, b3, :w], lhsT=blk_t[:], rhs=kk_[:, :w], start=True, stop=True),
                  reads=[blk_b, kkb], writes=[tl.pb[b3]])
            bn, bnb = stage()
            sc.op("dve", lambda: nc.vector.tensor_tensor(out=bn[:, :w], in0=ps[:, b3, :w], in1=v_[:, :w], op=ALU.mult),
                  reads=[tl.pb[b3], vb_], writes=[bnb])
            sc.op("pool", lambda: nc.gpsimd.tensor_tensor(out=bn[:, :w], in0=bn[:, :w], in1=yc[:, :w], op=ALU.add),
                  reads=[bnb, ycb], writes=[bnb])
            oi = tl.rot("ob", 2)
            sc.op("pool", lambda: nc.gpsimd.tensor_tensor(out=ob[oi][:, :w], in0=bn[:, :w], in1=g_[:, :w], op=ALU.mult),
                  reads=[bnb, gb], writes=[obb[oi]])
            sc.dma("sp", Od[c * 128:(c + 1) * 128, t0:t0 + w], ob[oi][:, :w], reads=[obb[oi]])
    sc.finish(obb)
    return nc


def run_rwkv_mid(y0, y1, lat, ctx, inp):
    nc = build_rwkv_mid(2048, 32)
    common = {"r_k": vec_pc(inp["rwkv_r_k"][0].reshape(-1)), "ln_w": vec_pc(inp["rwkv_ln_w"][0]),
              "ln_b": vec_pc(inp["rwkv_ln_b"][0]), "blk": blockones()}
    maps = []
    for r in range(NCORES):
        ls = slice(r * 2048, (r + 1) * 2048)
        cs = slice(r * 32, (r + 1) * 32)
        arrs = []
        for (yl, yc) in ((y0[:, NCTX:], y0[:, :NCTX]), (y1[:, NCTX:], y1[:, :NCTX])):
            arrs.append(np.concatenate([yl[:, ls], yc[:, cs]], axis=1))
        for qi in (0, 3, 4, 1, 9):
            arrs.append(np.concatenate([lat[qi][:, ls], ctx[qi][:, cs]], axis=1))
        m = dict(common)
        m["inp"] = np.ascontiguousarray(np.stack(arrs))
        maps.append(m)
    res = run_bass_kernel_spmd(nc, maps, core_ids=list(range(NCORES)))
    os_ = [res.results[r]["oT"] for r in range(NCORES)]
    return (np.concatenate([o[:, :2048] for o in os_], axis=1), np.concatenate([o[:, 2048:] for o in os_], axis=1))


def kernel(**inp):
    inp = {k: np.asarray(v) for k, v in inp.items()}
    mod = run_stage_mod(inp)
    xl = to_fm(inp["x"][0])
    xc = to_fm(inp["ctx"][0])
    for li, j in ((0, 0),):
        lam_init = 0.8 - 0.6 * math.exp(-0.3 * li)
        qkv_l, qkv_c = run_pre_attn(xl, xc, mod[li], inp["norm_g"][li, 0], inp["attn_w_in"][j])
        o_l, o_c = run_attn_core(qkv_l, qkv_c, inp["attn_lambda"][j], inp["attn_subln"][j], lam_init, True)
        xl, xc = run_post(xl, xc, o_l, o_c, mod[li], inp, inp["attn_w_out"][j], inp["norm_g"][li, 1], li, False, False)
    li = 1
    o_l, o_c = run_gmlp(xl, xc, mod[li], inp["norm_g"][li, 0], inp)
    xl, xc = run_post(xl, xc, o_l, o_c, mod[li], inp, inp["gmlp_w_out"][0], inp["norm_g"][li, 1], li, True, False)
    li = 2
    lat, ctx = run_rwkv_pre(xl, xc, mod[li], inp["norm_g"][li, 0], inp)
    y0, y1 = run_rwkv_scan(lat, ctx)
    o_l, o_c = run_rwkv_mid(y0, y1, lat, ctx, inp)
    xl, xc = run_post(xl, xc, o_l, o_c, mod[li], inp, inp["rwkv_w_out"][0], inp["norm_g"][li, 1], li, False, False)
    li, j = 3, 1
    lam_init = 0.8 - 0.6 * math.exp(-0.3 * li)
    qkv_l, qkv_c = run_pre_attn(xl, xc, mod[li], inp["norm_g"][li, 0], inp["attn_w_in"][j])
    o_l, _ = run_attn_core(qkv_l, qkv_c, inp["attn_lambda"][j], inp["attn_subln"][j], lam_init, False)
    xl, _ = run_post(xl, None, o_l, None, mod[li], inp, inp["attn_w_out"][j], inp["norm_g"][li, 1], li, True, True)
    return np.ascontiguousarray(xl.T)[None].astype(np.float32)
```
